# Optimizing a Trainium2 kernel written in Bass

```python
import math
import jax, jax.numpy as jnp
from jax import lax
import numpy as np

D_MODEL = 1024
BATCH = 16
SEQ = 2048
DEPTH = 1

MLA_HEADS = 8
MLA_Q_RANK = 256
MLA_KV_RANK = 128
MLA_NOPE_DIM = 64
MLA_ROPE_DIM = 32
MLA_V_DIM = 64
MLA_QK_DIM = MLA_NOPE_DIM + MLA_ROPE_DIM
DIFF_HEADS = 4
DIFF_HEAD_DIM = 64
DIFF_V_DIM = 2 * DIFF_HEAD_DIM
DIFF_QK_COLS = DIFF_HEADS * 2 * DIFF_HEAD_DIM
DIFF_V_COLS = DIFF_HEADS * DIFF_V_DIM
MIX_WIDTH = MLA_HEADS * MLA_V_DIM + DIFF_HEADS * DIFF_V_DIM
IN_SIZES = (MLA_Q_RANK, MLA_KV_RANK, MLA_ROPE_DIM, DIFF_QK_COLS, DIFF_QK_COLS, DIFF_V_COLS)
IN_COLS = sum(IN_SIZES)
IN_SPLITS = tuple(int(s) for s in np.cumsum(IN_SIZES)[:-1])
N_EXPERTS = 32
TOP_K = 4
D_EXPERT = D_MODEL
SWIGLU_LIMIT = 7.0
SWIGLU_ALPHA = 1.702
MOE_BLOCK = 256
ROPE_THETA = 10000.0
NORM_EPS = 1e-6
SUBLN_EPS = 1e-5
Q_BLOCK = 128
N_MOD = 6
MASK_VALUE = -1e30

kernel_name = "hymba_mla_diffattn_moe_adaln_layer"


def rmsnorm(x, g, eps=NORM_EPS):
    xf = x.astype(jnp.float32)
    y = xf * lax.rsqrt(jnp.mean(xf * xf, axis=-1, keepdims=True) + eps)
    return y.astype(x.dtype) * g


def rope_tables(positions, dim):
    inv_freq = ROPE_THETA ** (-jnp.arange(0, dim, 2, dtype=jnp.float32) / dim)
    ang = positions.astype(jnp.float32)[..., None] * inv_freq
    return jnp.cos(ang), jnp.sin(ang)


def apply_rope(x, cos, sin):
    shape = cos.shape[:2] + (1,) * (x.ndim - 3) + cos.shape[2:]
    cs = cos.reshape(shape).astype(x.dtype)
    sn = sin.reshape(shape).astype(x.dtype)
    x1, x2 = jnp.split(x, 2, axis=-1)
    return jnp.concatenate([x1 * cs - x2 * sn, x2 * cs + x1 * sn], axis=-1)


def to_query_blocks(q):
    b, s = q.shape[:2]
    return jnp.swapaxes(q.reshape((b, s // Q_BLOCK, Q_BLOCK) + q.shape[2:]), 0, 1)


def from_query_blocks(o):
    o = jnp.swapaxes(o, 0, 1)
    return o.reshape((o.shape[0], -1) + o.shape[3:])


def causal_mask(i, seq):
    q_pos = i * Q_BLOCK + jnp.arange(Q_BLOCK)
    return jnp.arange(seq)[None, :] <= q_pos[:, None]


def masked_softmax(s, mask):
    return jax.nn.softmax(jnp.where(mask, s.astype(jnp.float32), MASK_VALUE), axis=-1)


def mla_attention(c_q, c_kv, k_rope, g_q_a, w_q_b, g_kv_a, w_kv_b, cos, sin):
    b, s, _ = c_q.shape
    q = (rmsnorm(c_q, g_q_a) @ w_q_b).reshape(b, s, MLA_HEADS, MLA_QK_DIM)
    q = jnp.concatenate([q[..., :MLA_NOPE_DIM], apply_rope(q[..., MLA_NOPE_DIM:], cos, sin)], axis=-1)
    kv = (rmsnorm(c_kv, g_kv_a) @ w_kv_b).reshape(b, s, MLA_HEADS, MLA_NOPE_DIM + MLA_V_DIM)
    k_r = apply_rope(k_rope, cos, sin)
    k = jnp.concatenate(
        [kv[..., :MLA_NOPE_DIM], jnp.broadcast_to(k_r[:, :, None, :], (b, s, MLA_HEADS, MLA_ROPE_DIM))], axis=-1)
    v = kv[..., MLA_NOPE_DIM:]
    scale = MLA_QK_DIM ** -0.5

    def block(args):
        q_blk, i = args
        sc = jnp.einsum('bqhd,bkhd->bhqk', q_blk, k) * scale
        p = masked_softmax(sc, causal_mask(i, s))
        return jnp.einsum('bhqk,bkhd->bqhd', p.astype(v.dtype), v)

    o = lax.map(block, (to_query_blocks(q), jnp.arange(s // Q_BLOCK)))
    return from_query_blocks(o).reshape(b, s, MLA_HEADS * MLA_V_DIM)


def diff_attention(q_d, k_d, v_d, lambda_q1, lambda_k1, lambda_q2, lambda_k2, g_subln, lambda_init, cos, sin):
    b, s, _ = q_d.shape
    q = apply_rope(q_d.reshape(b, s, DIFF_HEADS, 2, DIFF_HEAD_DIM), cos, sin)
    k = apply_rope(k_d.reshape(b, s, DIFF_HEADS, 2, DIFF_HEAD_DIM), cos, sin)
    v = v_d.reshape(b, s, DIFF_HEADS, DIFF_V_DIM)
    lam = (jnp.exp(jnp.sum(lambda_q1.astype(jnp.float32) * lambda_k1.astype(jnp.float32)))
           - jnp.exp(jnp.sum(lambda_q2.astype(jnp.float32) * lambda_k2.astype(jnp.float32)))
           + lambda_init)
    scale = DIFF_HEAD_DIM ** -0.5

    def block(args):
        q_blk, i = args
        sc = jnp.einsum('bqhjd,bkhjd->bhjqk', q_blk, k) * scale
        p = masked_softmax(sc, causal_mask(i, s))
        a = p[:, :, 0] - lam * p[:, :, 1]
        return jnp.einsum('bhqk,bkhe->bqhe', a.astype(v.dtype), v)

    o = from_query_blocks(lax.map(block, (to_query_blocks(q), jnp.arange(s // Q_BLOCK))))
    o = rmsnorm(o, g_subln, SUBLN_EPS) * (1.0 - lambda_init)
    return o.reshape(b, s, DIFF_HEADS * DIFF_V_DIM)


def moe_ffn(h, w_router, b_router, w_gate_up, b_gate_up, w_down, b_down):
    b, s, d = h.shape
    n_tok = b * s
    xt = h.reshape(n_tok, d)
    logits = (xt @ w_router + b_router).astype(jnp.float32)
    top_val, top_idx = lax.top_k(logits, TOP_K)
    gates = jax.nn.softmax(top_val, axis=-1).astype(h.dtype)

    n_assign = n_tok * TOP_K
    n_blocks = -(-n_assign // MOE_BLOCK) + N_EXPERTS
    n_rows = n_blocks * MOE_BLOCK
    flat_e = top_idx.reshape(-1)
    flat_tok = jnp.arange(n_assign, dtype=jnp.int32) // TOP_K
    order = jnp.argsort(flat_e)
    sorted_e = flat_e[order]
    counts = jnp.bincount(flat_e, length=N_EXPERTS)
    padded = ((counts + MOE_BLOCK - 1) // MOE_BLOCK) * MOE_BLOCK
    start_sorted = jnp.cumsum(counts) - counts
    end_padded = jnp.cumsum(padded)
    start_padded = end_padded - padded
    dest = start_padded[sorted_e] + jnp.arange(n_assign) - start_sorted[sorted_e]
    row_tok = jnp.full((n_rows,), n_tok, dtype=jnp.int32).at[dest].set(flat_tok[order])
    row_w = jnp.zeros((n_rows,), h.dtype).at[dest].set(gates.reshape(-1)[order])
    blk_start = jnp.arange(n_blocks) * MOE_BLOCK
    blk_e = jnp.minimum(jnp.sum(blk_start[:, None] >= end_padded[None, :], axis=-1), N_EXPERTS - 1)
    x_rows = jnp.concatenate([xt, jnp.zeros((1, d), xt.dtype)], axis=0)[row_tok]
    x_rows = x_rows.reshape(n_blocks, MOE_BLOCK, d)

    def expert_block(args):
        xb, e = args
        gu = xb @ w_gate_up[e] + b_gate_up[e]
        g, u = jnp.split(gu, 2, axis=-1)
        g = jnp.minimum(g, SWIGLU_LIMIT)
        u = jnp.clip(u, -SWIGLU_LIMIT, SWIGLU_LIMIT)
        act = g * jax.nn.sigmoid(SWIGLU_ALPHA * g) * (u + 1.0)
        return act @ w_down[e] + b_down[e]

    y_rows = lax.map(expert_block, (x_rows, blk_e)).reshape(n_rows, d) * row_w[:, None]
    y = jax.ops.segment_sum(y_rows, row_tok, num_segments=n_tok + 1)[:n_tok]
    return y.reshape(b, s, d)


def setup_inputs(seed: int = 0) -> dict:
    key = jax.random.key(seed)
    ks = jax.random.split(key, 28)

    def nrm(k, shape, scale):
        return jax.random.normal(k, shape, jnp.float32) * scale

    def gain(k, shape):
        return 1.0 + 0.02 * jax.random.normal(k, shape, jnp.float32)

    L = DEPTH
    x = nrm(ks[0], (BATCH, SEQ, D_MODEL), 1.0)
    c = nrm(ks[1], (BATCH, D_MODEL), 1.0)
    offset = jax.random.randint(ks[2], (BATCH, 1), 0, 4096, dtype=jnp.int32)
    positions = offset + jnp.arange(SEQ, dtype=jnp.int32)[None, :]
    return {
        "x": x,
        "c": c,
        "positions": positions,
        "w_ada": nrm(ks[3], (L, D_MODEL, N_MOD * D_MODEL), 0.5 * D_MODEL ** -0.5),
        "b_ada": nrm(ks[4], (L, N_MOD * D_MODEL), 0.02),
        "g_mix_pre": gain(ks[5], (L, D_MODEL)),
        "g_mix_post": gain(ks[6], (L, D_MODEL)),
        "w_in": nrm(ks[7], (L, D_MODEL, IN_COLS), D_MODEL ** -0.5),
        "g_q_a": gain(ks[8], (L, MLA_Q_RANK)),
        "w_q_b": nrm(ks[9], (L, MLA_Q_RANK, MLA_HEADS * MLA_QK_DIM), MLA_Q_RANK ** -0.5),
        "g_kv_a": gain(ks[10], (L, MLA_KV_RANK)),
        "w_kv_b": nrm(ks[11], (L, MLA_KV_RANK, MLA_HEADS * (MLA_NOPE_DIM + MLA_V_DIM)), MLA_KV_RANK ** -0.5),
        "lambda_q1": nrm(ks[12], (L, DIFF_HEAD_DIM), 0.1),
        "lambda_k1": nrm(ks[13], (L, DIFF_HEAD_DIM), 0.1),
        "lambda_q2": nrm(ks[14], (L, DIFF_HEAD_DIM), 0.1),
        "lambda_k2": nrm(ks[15], (L, DIFF_HEAD_DIM), 0.1),
        "g_subln": gain(ks[16], (L, DIFF_V_DIM)),
        "w_o": nrm(ks[17], (L, MIX_WIDTH, D_MODEL), MIX_WIDTH ** -0.5),
        "g_ffn_pre": gain(ks[18], (L, D_MODEL)),
        "g_ffn_post": gain(ks[19], (L, D_MODEL)),
        "w_router": nrm(ks[20], (L, D_MODEL, N_EXPERTS), D_MODEL ** -0.5),
        "b_router": nrm(ks[21], (L, N_EXPERTS), 0.01),
        "w_gate_up": nrm(ks[22], (L, N_EXPERTS, D_MODEL, 2 * D_EXPERT), D_MODEL ** -0.5),
        "b_gate_up": nrm(ks[23], (L, N_EXPERTS, 2 * D_EXPERT), 0.02),
        "w_down": nrm(ks[24], (L, N_EXPERTS, D_EXPERT, D_MODEL), D_EXPERT ** -0.5),
        "b_down": nrm(ks[25], (L, N_EXPERTS, D_MODEL), 0.02),
    }


def reference(x, c, positions, w_ada, b_ada, g_mix_pre, g_mix_post, w_in, g_q_a, w_q_b, g_kv_a, w_kv_b,
              lambda_q1, lambda_k1, lambda_q2, lambda_k2, g_subln, w_o, g_ffn_pre, g_ffn_post,
              w_router, b_router, w_gate_up, b_gate_up, w_down, b_down):
    b, s, _ = x.shape
    cos_mla, sin_mla = rope_tables(positions, MLA_ROPE_DIM)
    cos_diff, sin_diff = rope_tables(positions, DIFF_HEAD_DIM)
    c_act = jax.nn.silu(c)
    for l in range(DEPTH):
        lambda_init = 0.8 - 0.6 * math.exp(-0.3 * l)
        mod = c_act @ w_ada[l] + b_ada[l]
        shift_m, scale_m, gate_m, shift_f, scale_f, gate_f = jnp.split(mod[:, None, :], N_MOD, axis=-1)

        h = rmsnorm(x, g_mix_pre[l]) * (1.0 + scale_m) + shift_m
        proj = h @ w_in[l]
        c_q, c_kv, k_rope, q_d, k_d, v_d = jnp.split(proj, IN_SPLITS, axis=-1)
        o_mla = mla_attention(c_q, c_kv, k_rope, g_q_a[l], w_q_b[l], g_kv_a[l], w_kv_b[l], cos_mla, sin_mla)
        o_diff = diff_attention(q_d, k_d, v_d, lambda_q1[l], lambda_k1[l], lambda_q2[l], lambda_k2[l],
                                g_subln[l], lambda_init, cos_diff, sin_diff)
        y = jnp.concatenate([o_mla, o_diff], axis=-1) @ w_o[l]
        x = x + gate_m * rmsnorm(y, g_mix_post[l])

        h = rmsnorm(x, g_ffn_pre[l]) * (1.0 + scale_f) + shift_f
        y = moe_ffn(h, w_router[l], b_router[l], w_gate_up[l], b_gate_up[l], w_down[l], b_down[l])
        x = x + gate_f * rmsnorm(y, g_ffn_post[l])
    return x
```

```python
import math
import numpy as np
import concourse.bass as bass
import concourse.mybir as mybir
from concourse.bass_utils import run_bass_kernel_spmd

F32 = mybir.dt.float32
BF16 = mybir.dt.bfloat16
I32 = mybir.dt.int32
ALU = mybir.AluOpType
AF = mybir.ActivationFunctionType
AX = mybir.AxisListType

NCORES = 8
D = 1024
S = 2048
NB = 2
NT = NB * S
NE = 32
PI = math.pi
CW1 = 6.28125
CW2 = 2 * math.pi - 6.28125
PI_LO = 3.1415925


class Buf:
    __slots__ = ("name", "last_w", "readers", "dsem", "dcnt")

    def __init__(self, name):
        self.name = name
        self.last_w = None
        self.readers = []
        self.dsem = None
        self.dcnt = 0


class Ctx:
    ENGS = ("pe", "act", "dve", "pool", "sp")

    def __init__(self, nc):
        self.nc = nc
        self.eng = {"pe": nc.tensor, "act": nc.scalar, "dve": nc.vector,
                    "pool": nc.gpsimd, "sp": nc.sync}
        self.sems = {}
        self.cnt = {}
        for e in self.ENGS:
            self.sems[e] = nc.alloc_semaphore("s_" + e)
            self.cnt[e] = 0
        self.waited = {}
        self.n_dsem = 0
        self.n_wait = 0
        self.n_ins = 0

    def sb(self, name, shape, dt):
        return self.nc.alloc_sbuf_tensor("sb_" + name, list(shape), dt)

    def ps(self, name, shape, dt=F32):
        return self.nc.alloc_psum_tensor("ps_" + name, list(shape), dt)

    def _need(self, eng, deps):
        best = {}
        for d in deps:
            if d is None:
                continue
            k, v = d
            if k == "pe" and eng == "pe":
                continue
            if best.get(k, 0) < v:
                best[k] = v
        for k, v in best.items():
            if self.waited.get((eng, k), 0) >= v:
                continue
            self.eng[eng].wait_ge(self.sems[k], v)
            self.waited[(eng, k)] = v
            self.n_wait += 1

    @staticmethod
    def _deps(reads, writes):
        deps = []
        for b in reads:
            deps.append(b.last_w)
        for b in writes:
            deps.append(b.last_w)
            deps.extend(b.readers)
        return deps

    def op(self, eng, fn, reads=(), writes=()):
        self._need(eng, self._deps(reads, writes))
        ins = fn(self.eng[eng])
        self.cnt[eng] += 1
        ins.then_inc(self.sems[eng], 1)
        self.n_ins += 1
        tok = (eng, self.cnt[eng])
        for b in reads:
            b.readers.append(tok)
            if len(b.readers) > 10:
                best = {}
                for k, v in b.readers:
                    if best.get(k, 0) < v:
                        best[k] = v
                b.readers = list(best.items())
        for b in writes:
            b.last_w = tok
            b.readers = []
        return ins

    def _dsem(self, b):
        if b.dsem is None:
            b.dsem = "d%d_%s" % (self.n_dsem, b.name)
            self.n_dsem += 1
            self.sems[b.dsem] = self.nc.alloc_semaphore(b.dsem)
        return b.dsem

    def dma(self, eng, fn, reads=(), writes=(), owner=None):
        self._need(eng, self._deps(reads, writes))
        own = owner if owner is not None else writes[0]
        k = self._dsem(own)
        res = fn(self.eng[eng])
        if not isinstance(res, (list, tuple)):
            res = [res]
        for ins in res:
            ins.then_inc(self.sems[k], 16)
            own.dcnt += 16
            self.n_ins += 1
        tok = (k, own.dcnt)
        for b in reads:
            b.readers.append(tok)
        for b in writes:
            b.last_w = tok
            b.readers = []
        return tok

    def wait_all(self, eng, bufs):
        self._need(eng, [b.last_w for b in bufs])


def _col(v, p=128):
    return np.ascontiguousarray(v.reshape(-1, p).T)


def _rope_perm(n_half):
    i = np.arange(2 * n_half)
    return (i + n_half) % (2 * n_half)


def host_prep(inp):
    f = np.float32
    x = inp["x"]; c = inp["c"]; pos = inp["positions"]
    w_ada = np.ascontiguousarray(inp["w_ada"][0]); b_ada = inp["b_ada"][0]
    w_in = inp["w_in"][0]
    cq = w_in[:, 0:256]; ckv = w_in[:, 256:384]; kr = w_in[:, 384:416]
    qd = w_in[:, 416:928]; kd = w_in[:, 928:1440]; vd = w_in[:, 1440:1952]
    pm = _rope_perm(16)
    z64 = np.zeros((D, 64), f); z32 = np.zeros((D, 32), f)
    groups = [cq[:, 0:128], cq[:, 128:256], ckv,
              np.concatenate([z64, kr, z32], 1), np.concatenate([z64, kr[:, pm], z32], 1)]
    pd = _rope_perm(32)
    def permd(w):
        w4 = w.reshape(D, 4, 2, 64)
        return np.ascontiguousarray(w4[:, :, :, pd]).reshape(D, 512)
    qdp = permd(qd); kdp = permd(kd)
    for h in range(4):
        groups.append(qd[:, h * 128:(h + 1) * 128])
    for h in range(4):
        groups.append(qdp[:, h * 128:(h + 1) * 128])
    for h in range(4):
        groups.append(kd[:, h * 128:(h + 1) * 128])
    for h in range(4):
        groups.append(kdp[:, h * 128:(h + 1) * 128])
    w_in_g = np.ascontiguousarray(np.stack(groups, 0))
    wq = inp["w_q_b"][0].reshape(256, 8, 96)
    wqA = np.ascontiguousarray(wq)
    wqB = np.zeros_like(wq)
    wqB[:, :, 64:96] = wq[:, :, 64:96][:, :, pm]
    wkv = inp["w_kv_b"][0].reshape(128, 8, 128)
    wkvk = np.ascontiguousarray(wkv[:, :, 0:64])
    wkvv = np.ascontiguousarray(wkv[:, :, 64:128]).reshape(128, 512)
    invf_d = (10000.0 ** (-np.arange(0, 64, 2, dtype=f) / f(64))).astype(f)
    invf_m = (10000.0 ** (-np.arange(0, 32, 2, dtype=f) / f(32))).astype(f)
    cst = np.zeros((128, 8), f)
    p = np.arange(128)
    cst[:, 0] = invf_d[p % 32]
    cst[:, 1] = np.where((p % 64) < 32, -1.0, 1.0)
    cst[64:96, 2] = invf_m[(p[64:96] - 64) % 16]
    cst[:, 3] = 1.0
    cst[64:80, 3] = -1.0
    cst[:, 4] = -PI * cst[:, 1]
    cst[:, 5] = -PI * cst[:, 3]
    cst[:, 6] = -PI
    ident = np.eye(128, dtype=f)
    maskT = (p[None, :] >= p[:, None]).astype(f)
    lam = np.stack([inp["lambda_q1"][0], inp["lambda_k1"][0], inp["lambda_q2"][0], inp["lambda_k2"][0]], 0)
    lam_rep = np.ascontiguousarray(np.broadcast_to(lam[None], (128, 4, 64))).astype(f)
    shared = dict(
        w_ada=w_ada, bada_col=_col(b_ada),
        bada_rep=np.ascontiguousarray(np.broadcast_to(
            np.concatenate([b_ada[2048:3072], b_ada[5120:6144]])[None], (128, 2048))).astype(f),
        gpre_col=_col(inp["g_mix_pre"][0]), gfpre_col=_col(inp["g_ffn_pre"][0]),
        gpost_rep=np.ascontiguousarray(np.broadcast_to(np.concatenate(
            [inp["g_mix_post"][0], inp["g_ffn_post"][0]])[None], (128, 2048))).astype(f),
        w_in_g=w_in_g, w_vd=np.ascontiguousarray(vd),
        wqA=wqA, wqB=wqB, gqa_col=_col(inp["g_q_a"][0]),
        wkvk=wkvk, wkvv=wkvv, gkva_col=_col(inp["g_kv_a"][0]),
        lam_rep=lam_rep, gsub_col=_col(inp["g_subln"][0], 64),
        w_o=np.ascontiguousarray(inp["w_o"][0]),
        w_r=np.ascontiguousarray(inp["w_router"][0]),
        br_rep=np.ascontiguousarray(np.broadcast_to(inp["b_router"][0][None], (128, NE))).astype(f),
        w_gu=np.ascontiguousarray(inp["w_gate_up"][0]),
        bgu_col=np.ascontiguousarray(inp["b_gate_up"][0].reshape(NE, 16, 128).transpose(2, 0, 1)),
        w_dn=np.ascontiguousarray(inp["w_down"][0]),
        b_dn=np.ascontiguousarray(inp["b_down"][0]),
        cst=cst, ident=ident, maskT=maskT,
    )
    maps = []
    for i in range(NCORES):
        bs = slice(i * NB, (i + 1) * NB)
        m = dict(shared)
        m["x"] = np.ascontiguousarray(x[bs].reshape(NT, D))
        cc = c[bs]
        m["ccol"] = np.ascontiguousarray(cc.reshape(NB, 8, 128).transpose(2, 1, 0))
        m["crep"] = np.ascontiguousarray(np.broadcast_to(
            m["ccol"][:, :, :, None], (128, 8, NB, 128))).astype(f)
        m["posr"] = np.ascontiguousarray(np.broadcast_to(pos[bs][:, None, :], (NB, 128, S))).astype(np.int32)
        maps.append(m)
    return maps


def build(stage="full"):
    nc = bass.Bass("TRN2", target_bir_lowering=False)
    c = Ctx(nc)

    names = []

    def din(name, shape, dt=F32):
        names.append(name)
        return nc.dram_tensor(name, list(shape), dt, kind="ExternalInput").ap()

    x_d = din("x", [NT, D]); ccol_d = din("ccol", [128, 8, NB]); crep_d = din("crep", [128, 8, NB, 128])
    posr_d = din("posr", [NB, 128, S], I32)
    wada_d = din("w_ada", [D, 6 * D]); badac_d = din("bada_col", [128, 48]); badar_d = din("bada_rep", [128, 2048])
    gpre_d = din("gpre_col", [128, 8]); gfpre_d = din("gfpre_col", [128, 8]); gpost_d = din("gpost_rep", [128, 2048])
    wing_d = din("w_in_g", [21, D, 128]); wvd_d = din("w_vd", [D, 512])
    wqA_d = din("wqA", [256, 8, 96]); wqB_d = din("wqB", [256, 8, 96]); gqa_d = din("gqa_col", [128, 2])
    wkvk_d = din("wkvk", [128, 8, 64]); wkvv_d = din("wkvv", [128, 512]); gkva_d = din("gkva_col", [128, 1])
    lam_d = din("lam_rep", [128, 4, 64]); gsub_d = din("gsub_col", [64, 2])
    wo_d = din("w_o", [D, D]); wr_d = din("w_r", [D, NE]); brr_d = din("br_rep", [128, NE])
    if stage == "full":
        wgu_d = din("w_gu", [NE, D, 2 * D]); bguc_d = din("bgu_col", [128, NE, 16])
        wdn_d = din("w_dn", [NE, D, D]); bdn_d = din("b_dn", [NE, D])
    nc._in_names = names
    cst_d = din("cst", [128, 8]); ident_d = din("ident", [128, 128]); maskT_d = din("maskT", [128, 128])
    out_d = nc.dram_tensor("out", [NT, D], F32, kind="ExternalOutput").ap()
    xmid_d = nc.dram_tensor("xmid", [NT, D], F32, kind="Internal").ap()
    h2T_d = nc.dram_tensor("h2T", [8, 128, NT], BF16, kind="Internal").ap()
    GT_d = nc.dram_tensor("GTd", [NE, NT], F32, kind="Internal").ap()
    gf_d = nc.dram_tensor("gfd", [128, NB, D], F32, kind="Internal").ap()
    b_gfd = Buf("gfd")

    op = c.op; dma = c.dma

    cst = c.sb("cst", [128, 8], F32); b_cst = Buf("cst")
    ident_f = c.sb("ident_f", [128, 128], F32); ident_b = c.sb("ident_b", [128, 128], BF16); b_id = Buf("ident")
    maskT = c.sb("maskT", [128, 128], BF16); b_mask = Buf("maskT")
    ones_b = c.sb("ones_b", [128, 128], BF16); ones_f = c.sb("ones_f", [128, 128], F32); b_ones = Buf("ones")
    dma("sp", lambda e: e.dma_start(out=cst[:], in_=cst_d), writes=[b_cst])
    dma("sp", lambda e: e.dma_start(out=ident_f[:], in_=ident_d), writes=[b_id])
    dma("pool", lambda e: [e.dma_start(out=ident_b[:], in_=ident_d)], writes=[b_id])
    dma("pool", lambda e: e.dma_start(out=maskT[:], in_=maskT_d), writes=[b_mask])
    op("dve", lambda e: e.memset(ones_b[:], 1.0), writes=[b_ones])
    op("dve", lambda e: e.memset(ones_f[:], 1.0), writes=[b_ones])

    from contextlib import ExitStack
    uid = [0]

    class Scope:
        def __init__(self):
            self.es = ExitStack(); self.bufs = []
        def T(self, name, shape, dt):
            uid[0] += 1
            return self.es.enter_context(nc.sbuf_tensor("t_%s_%d" % (name, uid[0]), list(shape), dt))
        def B(self, name):
            b = Buf(name); self.bufs.append(b); return b
        def close(self):
            fence(c, self.bufs)
            self.es.close()

    def bf(ap):
        return ap.bitcast(BF16)

    MS = c.sb("MS", [128, NB, 8], F32); MH = c.sb("MH", [128, NB, 8], F32)
    FS = c.sb("FS", [128, NB, 8], F32); FH = c.sb("FH", [128, NB, 8], F32)
    b_mod = Buf("mod")
    lamc = c.sb("lamc", [128, 4], F32); b_lam = Buf("lam")
    gqa = c.sb("gqa", [128, 2], F32); gkva = c.sb("gkva", [128, 1], F32); gsub = c.sb("gsub", [64, 2], F32)
    b_g = Buf("gains")
    dma("sp", lambda e: [e.dma_start(out=gqa[:], in_=gqa_d), e.dma_start(out=gkva[:], in_=gkva_d),
                         e.dma_start(out=gsub[:], in_=gsub_d)], writes=[b_g])

    P0 = Scope()
    GM = P0.T("GM", [128, NB, D], F32)
    PB = [c.ps("pb%d" % i, [128, 512], F32) for i in range(8)]
    b_pb = [Buf("pb%d" % i) for i in range(8)]

    with nc.sbuf_tensor("t_ccol", [128, 8, NB], F32) as ccol, \
         nc.sbuf_tensor("t_crep", [128, 8, NB, 128], F32) as crep, \
         nc.sbuf_tensor("t_wa0", [128, 8, 512], F32) as wa0, \
         nc.sbuf_tensor("t_wa1", [128, 8, 512], F32) as wa1, \
         nc.sbuf_tensor("t_badac", [128, 48], F32) as badac, \
         nc.sbuf_tensor("t_badar", [128, 2048], F32) as badar, \
         nc.sbuf_tensor("t_gpre", [128, 8], F32) as gpre, \
         nc.sbuf_tensor("t_gfpre", [128, 8], F32) as gfpre, \
         nc.sbuf_tensor("t_gpost", [128, 2048], F32) as gpost, \
         nc.sbuf_tensor("t_modc", [128, 48, NB], F32) as modc, \
         nc.sbuf_tensor("t_lamt", [128, 4, 64], F32) as lamt, \
         nc.sbuf_tensor("t_lamj", [128, 64], F32) as lamj, \
         nc.sbuf_tensor("t_GF", [128, NB, D], F32) as GF:
        b_cc = Buf("cc"); b_wa = [Buf("wa0"), Buf("wa1")]; b_misc = Buf("misc0"); b_modc = Buf("modc")
        wa = [wa0, wa1]
        dma("sp", lambda e: [e.dma_start(out=ccol[:], in_=ccol_d), e.dma_start(out=crep[:], in_=crep_d)],
            writes=[b_cc])
        dma("sp", lambda e: [e.dma_start(out=badac[:], in_=badac_d), e.dma_start(out=badar[:], in_=badar_d),
                             e.dma_start(out=gpre[:], in_=gpre_d), e.dma_start(out=gfpre[:], in_=gfpre_d),
                             e.dma_start(out=gpost[:], in_=gpost_d), e.dma_start(out=lamt[:], in_=lam_d)],
            writes=[b_misc])
        op("act", lambda e: e.activation(out=ccol[:], in_=ccol[:], func=AF.Silu), reads=[b_cc], writes=[b_cc])
        op("act", lambda e: e.activation(out=crep[:], in_=crep[:], func=AF.Silu), reads=[b_cc], writes=[b_cc])
        op("dve", lambda e: e.memset(lamc[:], 0.0), writes=[b_lam])
        for j in range(2):
            op("dve", lambda e: e.scalar_tensor_tensor(out=lamj[:], in0=lamt[:, 2 * j, :], scalar=1.0,
                                                       in1=lamt[:, 2 * j + 1, :], op0=ALU.mult, op1=ALU.mult,
                                                       accum_out=lamc[:, j:j + 1]),
               reads=[b_misc], writes=[b_lam, b_misc])
        op("act", lambda e: e.activation(out=lamc[:, 0:2], in_=lamc[:, 0:2], func=AF.Exp), reads=[b_lam], writes=[b_lam])
        op("dve", lambda e: e.tensor_tensor(out=lamc[:, 2:3], in0=lamc[:, 0:1], in1=lamc[:, 1:2], op=ALU.subtract),
           reads=[b_lam], writes=[b_lam])
        op("dve", lambda e: e.tensor_scalar(out=lamc[:, 3:4], in0=lamc[:, 2:3], scalar1=0.2, scalar2=-1.0,
                                            op0=ALU.add, op1=ALU.mult), reads=[b_lam], writes=[b_lam])
        for sl in range(12):
            w = wa[sl % 2]; bw = b_wa[sl % 2]
            dma("sp", lambda e: e.dma_start(out=w[:], in_=wada_d[:, sl * 512:(sl + 1) * 512]
                                            .rearrange("(k p) n -> p k n", p=128)), writes=[bw])
            if sl in (4, 5, 10, 11):
                gi = 0 if sl < 6 else 1
                half = sl % 2 if sl < 6 else (sl - 10)
                dst = GM if gi == 0 else GF
                for b in range(NB):
                    pbk = PB[b]; bpb = b_pb[b]
                    for kc in range(8):
                        op("pe", lambda e: e.matmul(pbk[:], lhsT=crep[:, kc, b, :], rhs=w[:, kc, :],
                                                    start=(kc == 0), stop=(kc == 7)),
                           reads=[b_cc, bw], writes=[bpb])
                    cs = slice(half * 512, (half + 1) * 512)
                    rs = slice(gi * 1024 + half * 512, gi * 1024 + (half + 1) * 512)
                    op("dve", lambda e: e.tensor_tensor(out=dst[:, b, cs], in0=pbk[:], in1=badar[:, rs], op=ALU.add),
                       reads=[bpb, b_misc], writes=[b_mod])
                    op("dve", lambda e: e.tensor_tensor(out=dst[:, b, cs], in0=dst[:, b, cs], in1=gpost[:, rs],
                                                        op=ALU.mult), reads=[b_mod, b_misc], writes=[b_mod])
            else:
                pbk = PB[2 + sl % 2]; bpb = b_pb[2 + sl % 2]
                for jj in range(4):
                    for kc in range(8):
                        op("pe", lambda e: e.matmul(pbk[:, jj * NB:(jj + 1) * NB], lhsT=w[:, kc, jj * 128:(jj + 1) * 128],
                                                    rhs=ccol[:, kc, :], start=(kc == 0), stop=(kc == 7)),
                           reads=[b_cc, bw], writes=[bpb])
                op("dve", lambda e: e.tensor_copy(out=modc[:, sl * 4:(sl + 1) * 4, :],
                                                  in_=pbk[:, 0:4 * NB].rearrange("p (j b) -> p j b", b=NB)),
                   reads=[bpb], writes=[b_modc])
        for b in range(NB):
            op("dve", lambda e: e.tensor_tensor(out=MH[:, b, :], in0=modc[:, 0:8, b], in1=badac[:, 0:8], op=ALU.add),
               reads=[b_modc, b_misc], writes=[b_mod])
            op("dve", lambda e: e.tensor_tensor(out=FH[:, b, :], in0=modc[:, 24:32, b], in1=badac[:, 24:32], op=ALU.add),
               reads=[b_modc, b_misc], writes=[b_mod])
            for (dst, j0, gg) in ((MS, 8, gpre), (FS, 32, gfpre)):
                op("dve", lambda e: e.scalar_tensor_tensor(out=dst[:, b, :], in0=modc[:, j0:j0 + 8, b], scalar=1.0,
                                                           in1=badac[:, j0:j0 + 8], op0=ALU.add, op1=ALU.add),
                   reads=[b_modc, b_misc], writes=[b_mod])
                op("dve", lambda e: e.tensor_tensor(out=dst[:, b, :], in0=dst[:, b, :], in1=gg[:], op=ALU.mult),
                   reads=[b_mod, b_misc], writes=[b_mod])
        b_gft = Buf("gft")
        op("dve", lambda e: e.tensor_copy(out=GF[:, :, 0:1], in_=GF[:, :, 0:1]), reads=[b_mod], writes=[b_gft])
        dma("sp", lambda e: e.dma_start(out=gf_d, in_=GF[:]), reads=[b_gft, b_mod], writes=[b_gfd])
        fence(c, [b_cc, b_misc, b_modc, b_gft, b_gfd] + b_wa)


    wqA = P0.T("wqA", [128, 2, 8, 96], BF16); wqB = P0.T("wqB", [128, 2, 8, 96], BF16)
    wkvk = P0.T("wkvk", [128, 8, 64], BF16); wkvv = P0.T("wkvv", [128, 512], BF16)
    wr = P0.T("wr", [128, 8, NE], F32); brr = P0.T("brr", [128, NE], F32)
    b_w = P0.B("attw")
    dma("pool", lambda e: [e.dma_start(out=wqA[:], in_=wqA_d.rearrange("(k p) h n -> p k h n", p=128)),
                           e.dma_start(out=wqB[:], in_=wqB_d.rearrange("(k p) h n -> p k h n", p=128)),
                           e.dma_start(out=wkvk[:], in_=wkvk_d), e.dma_start(out=wkvv[:], in_=wkvv_d)],
        writes=[b_w])
    dma("sp", lambda e: [e.dma_start(out=wr[:], in_=wr_d.rearrange("(k p) n -> p k n", p=128)),
                         e.dma_start(out=brr[:], in_=brr_d)], writes=[b_w])
    EPS = 1e-6
    SC_M = 96.0 ** -0.5
    SC_D = 64.0 ** -0.5

    def rstd_col(eng_sq, src, ss, n, eps, reads, bss, junk, bjunk):
        op("dve", lambda e: e.memset(ss[:, 0:1], 0.0), writes=[bss])
        op("act", lambda e: e.activation(out=junk, in_=src, func=AF.Square, accum_out=ss[:, 0:1]),
           reads=reads + [bss], writes=[bss, bjunk])
        op("dve", lambda e: e.tensor_scalar(out=ss[:, 1:2], in0=ss[:, 0:1], scalar1=1.0 / n, scalar2=eps,
                                            op0=ALU.mult, op1=ALU.add), reads=[bss], writes=[bss])
        op("dve", lambda e: e.reciprocal(out=ss[:, 2:3], in_=ss[:, 1:2]), reads=[bss], writes=[bss])
        op("act", lambda e: e.activation(out=ss[:, 3:4], in_=ss[:, 2:3], func=AF.Sqrt), reads=[bss], writes=[bss])
        return ss[:, 3:4]

    def rstd_bc(pbank, bpbank, n, eps, dst, bdst, rows=128):
        op("dve", lambda e: e.tensor_scalar(out=dst[0:rows, :], in0=pbank[0:rows, :], scalar1=1.0 / n, scalar2=eps,
                                            op0=ALU.mult, op1=ALU.add), reads=[bpbank], writes=[bdst])
        op("dve", lambda e: e.reciprocal(out=dst[0:rows, :], in_=dst[0:rows, :]), reads=[bdst], writes=[bdst])
        op("act", lambda e: e.activation(out=dst[0:rows, :], in_=dst[0:rows, :], func=AF.Sqrt), reads=[bdst], writes=[bdst])

    for b in range(NB):
        tok0 = b * S
        AB = Scope()
        cqnT = AB.T("cqnT", [128, 2, S], BF16); b_cqn = AB.B("cqn")
        ckvnT = AB.T("ckvnT", [128, S], BF16); b_ckvn = AB.B("ckvn")
        krT = AB.T("krT", [96, S], BF16); b_kr = AB.B("kr")
        vm = AB.T("vm", [128, 16, 8, 65], BF16); b_vm = AB.B("vm")
        qdT = AB.T("qdT", [128, 4, S], BF16); b_qd = AB.B("qd")
        kdT = AB.T("kdT", [128, 4, S], BF16); b_kd = AB.B("kd")
        vd = AB.T("vd", [128, 16, 4, 2, 65], BF16); b_vd = AB.B("vd")
        Cm = AB.T("Cm", [96, S], BF16); Sm = AB.T("Sm", [96, S], BF16); b_tm = AB.B("tabm")
        op("pool", lambda e: e.memset(vm[:], 1.0), writes=[b_vm])
        op("pool", lambda e: e.memset(vd[:], 1.0), writes=[b_vd])
        op("pool", lambda e: e.memset(krT[:], 0.0), writes=[b_kr])

        A = Scope()
        hT = A.T("hT", [128, 8, S], BF16); b_hT = A.B("hT")
        wing_m = A.T("wing_m", [128, 8, 640], BF16); b_wm = A.B("wing_m")
        wing_p = [A.T("wing_p%d" % i, [128, 8, 256], BF16) for i in range(2)]; b_wp = [A.B("wp0"), A.B("wp1")]
        wvd = A.T("wvd", [128, 8, 512], BF16); b_wvd = A.B("wvd")
        Cd = A.T("Cd", [128, S], BF16); Sd = A.T("Sd", [128, S], BF16); b_td = A.B("tabd")
        posi = A.T("posi", [128, 512], I32); posf = A.T("posf", [128, 512], F32); angt = A.T("angt", [128, 512], F32)
        angi = A.T("angi", [128, 512], I32); angk = A.T("angk", [128, 512], F32)
        b_pos = A.B("pos"); b_ang = A.B("ang"); b_angi = A.B("angi"); b_angk = A.B("angk")
        xt = [A.T("xt%d" % i, [128, D], F32) for i in range(2)]; b_xt = [A.B("xt0"), A.B("xt1")]
        xn = [A.T("xn%d" % i, [128, D], BF16) for i in range(2)]; b_xn = [A.B("xn0"), A.B("xn1")]
        ssA = A.T("ssA", [128, 8], F32); b_ssA = A.B("ssA")
        tb = [A.T("tb%d" % i, [128, 512], BF16) for i in range(2)]; b_tb = [A.B("tb0"), A.B("tb1")]
        tf = [A.T("tf%d" % i, [128, 512], F32) for i in range(3)]; b_tf = [A.B("tf%d" % i) for i in range(3)]

        for g in range(5):
            dma("pool", lambda e: e.dma_start(out=wing_m[:, :, g * 128:(g + 1) * 128],
                                              in_=wing_d[g].rearrange("(k p) n -> p k n", p=128)), writes=[b_wm])
        dma("pool", lambda e: e.dma_start(out=wvd[:], in_=wvd_d.rearrange("(k p) n -> p k n", p=128)), writes=[b_wvd])
        for ch in range(4):
            cs = slice(ch * 512, (ch + 1) * 512)
            dma("sp", lambda e: e.dma_start(out=posi[:], in_=posr_d[b][:, cs]), writes=[b_pos])
            op("dve", lambda e: e.tensor_copy(out=posf[:], in_=posi[:]), reads=[b_pos], writes=[b_pos])
            for (Ct, St, rows, ci, si, bt) in ((Cd, Sd, 128, 0, 1, b_td), (Cm, Sm, 96, 2, 3, b_tm)):
                for (dstT, shift, use_sgn) in ((Ct, 0.5 * PI, False), (St, 0.0, True)):
                    R = slice(0, rows)
                    op("dve", lambda e: e.tensor_scalar(out=angt[R, :], in0=posf[R, :], scalar1=cst[R, ci:ci + 1],
                                                        scalar2=shift, op0=ALU.mult, op1=ALU.add),
                       reads=[b_pos, b_cst], writes=[b_ang])
                    op("dve", lambda e: e.tensor_scalar(out=angi[R, :], in0=angt[R, :], scalar1=1.0 / (2 * PI), scalar2=0.0,
                                                        op0=ALU.mult, op1=ALU.add), reads=[b_ang], writes=[b_angi])
                    op("dve", lambda e: e.tensor_copy(out=angk[R, :], in_=angi[R, :]), reads=[b_angi], writes=[b_angk])
                    op("dve", lambda e: e.scalar_tensor_tensor(out=angt[R, :], in0=angk[R, :], scalar=-CW1, in1=angt[R, :],
                                                               op0=ALU.mult, op1=ALU.add), reads=[b_angk, b_ang], writes=[b_ang])
                    op("dve", lambda e: e.scalar_tensor_tensor(out=angt[R, :], in0=angk[R, :], scalar=-CW2, in1=angt[R, :],
                                                               op0=ALU.mult, op1=ALU.add), reads=[b_angk, b_ang], writes=[b_ang])
                    op("dve", lambda e: e.tensor_scalar(out=angk[R, :], in0=angt[R, :], scalar1=PI, scalar2=2 * PI,
                                                        op0=ALU.is_gt, op1=ALU.mult), reads=[b_ang], writes=[b_angk])
                    op("dve", lambda e: e.tensor_tensor(out=angt[R, :], in0=angt[R, :], in1=angk[R, :], op=ALU.subtract),
                       reads=[b_ang, b_angk], writes=[b_ang])
                    op("dve", lambda e: e.tensor_scalar(out=angt[R, :], in0=angt[R, :], scalar1=-PI_LO, scalar2=PI_LO,
                                                        op0=ALU.max, op1=ALU.min), reads=[b_ang], writes=[b_ang])
                    if use_sgn:
                        op("act", lambda e: e.activation(out=dstT[R, cs], in_=angt[R, :], func=AF.Sin, scale=cst[R, si:si + 1]),
                           reads=[b_ang, b_cst], writes=[bt])
                    else:
                        op("act", lambda e: e.activation(out=dstT[R, cs], in_=angt[R, :], func=AF.Sin),
                           reads=[b_ang, b_cst], writes=[bt])
        for t in range(16):
            xi = xt[t % 2]; bxi = b_xt[t % 2]; xni = xn[t % 2]; bxni = b_xn[t % 2]
            dma("sp", lambda e: e.dma_start(out=xi[:], in_=x_d[tok0 + t * 128: tok0 + (t + 1) * 128, :]), writes=[bxi])
            r = rstd_col("act", xi[:], ssA, D, EPS, [bxi], b_ssA, xni[:], bxni)
            op("act", lambda e: e.activation(out=xni[:], in_=xi[:], func=AF.Copy, scale=r), reads=[bxi, b_ssA], writes=[bxni])
            pT = bf(PB[6][:]).rearrange("p (k t) -> p k t", t=128)
            for kc in range(8):
                op("pe", lambda e: e.transpose(out=pT[:, kc, :], in_=xni[:, kc * 128:(kc + 1) * 128], identity=ident_b[:]),
                   reads=[bxni, b_id], writes=[b_pb[6]])
            for kc in range(8):
                dst = hT[:, kc, t * 128:(t + 1) * 128]
                if kc % 2 == 0:
                    op("dve", lambda e: e.tensor_scalar(out=dst, in0=pT[:, kc, :], scalar1=MS[:, b, kc:kc + 1],
                                                        scalar2=MH[:, b, kc:kc + 1], op0=ALU.mult, op1=ALU.add),
                       reads=[b_pb[6], b_mod], writes=[b_hT])
                else:
                    op("act", lambda e: e.activation(out=dst, in_=pT[:, kc, :], func=AF.Identity,
                                                     bias=MH[:, b, kc:kc + 1], scale=MS[:, b, kc:kc + 1]),
                       reads=[b_pb[6], b_mod], writes=[b_hT])
        for ch in range(4):
            cs = slice(ch * 512, (ch + 1) * 512)
            for g in range(5):
                M = 128 if g < 3 else 96
                for kc in range(8):
                    op("pe", lambda e: e.matmul(PB[g][0:M, :], lhsT=wing_m[:, kc, g * 128:g * 128 + M], rhs=hT[:, kc, cs],
                                                start=(kc == 0), stop=(kc == 7)),
                       reads=[b_wm, b_hT], writes=[b_pb[g]])
            for i in range(2):
                op("act", lambda e: e.activation(out=tb[i][:], in_=PB[i][:], func=AF.Square), reads=[b_pb[i]], writes=[b_tb[i]])
            for i in range(2):
                op("pe", lambda e: e.matmul(PB[5][:], lhsT=ones_b[:], rhs=tb[i][:], start=(i == 0), stop=(i == 1)),
                   reads=[b_ones, b_tb[i]], writes=[b_pb[5]])
            rstd_bc(PB[5], b_pb[5], 256.0, EPS, tf[0], b_tf[0])
            for i in range(2):
                op("dve", lambda e: e.scalar_tensor_tensor(out=cqnT[:, i, cs], in0=PB[i][:], scalar=gqa[:, i:i + 1],
                                                           in1=tf[0][:], op0=ALU.mult, op1=ALU.mult),
                   reads=[b_pb[i], b_g, b_tf[0]], writes=[b_cqn])
            op("act", lambda e: e.activation(out=tb[0][:], in_=PB[2][:], func=AF.Square), reads=[b_pb[2]], writes=[b_tb[0]])
            op("pe", lambda e: e.matmul(PB[5][:], lhsT=ones_b[:], rhs=tb[0][:], start=True, stop=True),
               reads=[b_ones, b_tb[0]], writes=[b_pb[5]])
            rstd_bc(PB[5], b_pb[5], 128.0, EPS, tf[1], b_tf[1])
            op("dve", lambda e: e.scalar_tensor_tensor(out=ckvnT[:, cs], in0=PB[2][:], scalar=gkva[:, 0:1],
                                                       in1=tf[1][:], op0=ALU.mult, op1=ALU.mult),
               reads=[b_pb[2], b_g, b_tf[1]], writes=[b_ckvn])
            op("dve", lambda e: e.tensor_tensor(out=tf[2][64:96, :], in0=PB[3][64:96, :], in1=Cm[64:96, cs], op=ALU.mult),
               reads=[b_pb[3], b_tm], writes=[b_tf[2]])
            op("dve", lambda e: e.tensor_tensor(out=tf[0][64:96, :], in0=PB[4][64:96, :], in1=Sm[64:96, cs], op=ALU.mult),
               reads=[b_pb[4], b_tm], writes=[b_tf[0]])
            op("pool", lambda e: e.tensor_tensor(out=krT[64:96, cs], in0=tf[2][64:96, :], in1=tf[0][64:96, :], op=ALU.add),
               reads=[b_tf[2], b_tf[0]], writes=[b_kr])
        for t in range(16):
            pb = PB[6 + t % 2]; bpb = b_pb[6 + t % 2]
            op("pe", lambda e: e.matmul(pb[:], lhsT=ckvnT[:, t * 128:(t + 1) * 128], rhs=wkvv[:], start=True, stop=True),
               reads=[b_ckvn, b_w], writes=[bpb])
            op("act" if t % 2 else "dve",
               (lambda e: e.activation(out=vm[:, t, :, 0:64], in_=pb[:].rearrange("p (h d) -> p h d", d=64), func=AF.Copy))
               if t % 2 else
               (lambda e: e.tensor_copy(out=vm[:, t, :, 0:64], in_=pb[:].rearrange("p (h d) -> p h d", d=64))),
               reads=[bpb], writes=[b_vm])
        pi = 0
        for kind in range(2):
            dstT = qdT if kind == 0 else kdT; bdst = b_qd if kind == 0 else b_kd
            for h in range(4):
                wp = wing_p[pi % 2]; bwp = b_wp[pi % 2]; pi += 1
                gA = 5 + 8 * kind + h; gB = gA + 4
                dma("pool", lambda e: [e.dma_start(out=wp[:, :, 0:128], in_=wing_d[gA].rearrange("(k p) n -> p k n", p=128)),
                                       e.dma_start(out=wp[:, :, 128:256], in_=wing_d[gB].rearrange("(k p) n -> p k n", p=128))],
                    writes=[bwp])
                for ch in range(4):
                    cs = slice(ch * 512, (ch + 1) * 512)
                    pa = PB[2 * (ch % 2)]; bpa = b_pb[2 * (ch % 2)]; pbb = PB[2 * (ch % 2) + 1]; bpbb = b_pb[2 * (ch % 2) + 1]
                    for kc in range(8):
                        op("pe", lambda e: e.matmul(pa[:], lhsT=wp[:, kc, 0:128], rhs=hT[:, kc, cs], start=(kc == 0), stop=(kc == 7)),
                           reads=[bwp, b_hT], writes=[bpa])
                    for kc in range(8):
                        op("pe", lambda e: e.matmul(pbb[:], lhsT=wp[:, kc, 128:256], rhs=hT[:, kc, cs], start=(kc == 0), stop=(kc == 7)),
                           reads=[bwp, b_hT], writes=[bpbb])
                    op("dve", lambda e: e.tensor_tensor(out=tf[0][:], in0=pa[:], in1=Cd[:, cs], op=ALU.mult),
                       reads=[bpa, b_td], writes=[b_tf[0]])
                    op("dve", lambda e: e.tensor_tensor(out=tf[1][:], in0=pbb[:], in1=Sd[:, cs], op=ALU.mult),
                       reads=[bpbb, b_td], writes=[b_tf[1]])
                    op("pool", lambda e: e.tensor_tensor(out=dstT[:, h, cs], in0=tf[0][:], in1=tf[1][:], op=ALU.add),
                       reads=[b_tf[0], b_tf[1]], writes=[bdst])
        for t in range(16):
            pb = PB[6 + t % 2]; bpb = b_pb[6 + t % 2]
            for kc in range(8):
                op("pe", lambda e: e.matmul(pb[:], lhsT=hT[:, kc, t * 128:(t + 1) * 128], rhs=wvd[:, kc, :],
                                            start=(kc == 0), stop=(kc == 7)), reads=[b_hT, b_wvd], writes=[bpb])
            src = pb[:].rearrange("p (h j d) -> p h j d", h=4, j=2)
            if t % 2:
                op("act", lambda e: e.activation(out=vd[:, t, :, :, 0:64], in_=src, func=AF.Copy), reads=[bpb], writes=[b_vd])
            else:
                op("dve", lambda e: e.tensor_copy(out=vd[:, t, :, :, 0:64], in_=src), reads=[bpb], writes=[b_vd])
        A.close()

        Bs = Scope()
        wo = Bs.T("wo", [64, 16, D], BF16); b_wo = Bs.B("wo")
        dma("pool", lambda e: e.dma_start(out=wo[:], in_=wo_d.rearrange("(i p) n -> p i n", p=64)), writes=[b_wo])
        mixT = Bs.T("mixT", [64, 16, 512], BF16); b_mix = Bs.B("mix")
        kTh = [Bs.T("kTh%d" % i, [96, S], BF16) for i in range(2)]; b_kTh = [Bs.B("kTh0"), Bs.B("kTh1")]
        pt = [Bs.T("pt%d" % i, [128, 512], BF16) for i in range(3)]; b_pt = [Bs.B("pt%d" % i) for i in range(3)]
        qh = [Bs.T("qh%d" % i, [96, 512], BF16) for i in range(2)]; b_qh = [Bs.B("qh0"), Bs.B("qh1")]
        tg = [Bs.T("tg%d" % i, [128, 512], F32) for i in range(5)]; b_tg = [Bs.B("tg%d" % i) for i in range(5)]
        rd = Bs.T("rd", [128, 512], F32); b_rd = Bs.B("rd")
        sqb = [Bs.T("sqb%d" % i, [64, 512], BF16) for i in range(2)]; b_sqb = [Bs.B("sqb0"), Bs.B("sqb1")]
        xr = Bs.T("xr", [128, D], F32); b_xr = Bs.B("xr")
        ty = Bs.T("ty", [128, D], F32); b_ty = Bs.B("ty")
        xm = Bs.T("xm", [128, D], F32); b_xm = Bs.B("xm")
        ssB = Bs.T("ssB", [128, 8], F32); b_ssB = Bs.B("ssB")
        h2f = Bs.T("h2f", [128, 8, 128], F32); b_h2f = Bs.B("h2f")
        junkB = h2f[:].rearrange("p k t -> p (k t)"); b_junkB = b_h2f
        h2b = Bs.T("h2b", [128, 8, 128], BF16); b_h2b = Bs.B("h2b")
        rt = Bs.T("rt", [128, 6, NE], F32); b_rt = Bs.B("rt")
        gts = Bs.T("gts", [32, 128], F32); b_gts = Bs.B("gts")
        b_xmid = Bs.B("xmid_d"); b_h2d = Bs.B("h2T_d"); b_gtd = Bs.B("GT_d")
        pti = [0]; sbi = [0]

        def attn_map(kT_ap_fn, q_ap_fn, scale, pv_list, c_, kreads, qreads, vreads):
            nkt = 4 * c_ + 4

            def issue_S(kt):
                lo = max(0, kt * 128 - c_ * 512); n = 512 - lo
                sb_ = PB[sbi[0] % 2]; bsb = b_pb[sbi[0] % 2]; sbi[0] += 1
                op("pe", lambda e: e.matmul(sb_[:, 0:n], lhsT=kT_ap_fn(kt), rhs=q_ap_fn(lo, 512), start=True, stop=True),
                   reads=kreads + qreads, writes=[bsb])
                p_ = pt[pti[0] % 3]; bp_ = b_pt[pti[0] % 3]; pti[0] += 1
                op("act", lambda e: e.activation(out=p_[:, 0:n], in_=sb_[:, 0:n], func=AF.Exp, scale=scale),
                   reads=[bsb], writes=[bp_])
                if kt >= 4 * c_:
                    op("pool", lambda e: e.tensor_tensor(out=p_[:, 0:128], in0=p_[:, 0:128], in1=maskT[:], op=ALU.mult),
                       reads=[bp_, b_mask], writes=[bp_])
                return (p_, bp_, lo, n)

            pend = issue_S(0)
            for kt in range(nkt):
                nxt = issue_S(kt + 1) if kt + 1 < nkt else None
                p_, bp_, lo, n = pend
                for (acc, bacc, v_fn) in pv_list:
                    op("pe", lambda e: e.matmul(acc[:, lo:512], lhsT=v_fn(kt), rhs=p_[:, 0:n],
                                                start=(kt == 0), stop=(kt == nkt - 1)),
                       reads=[bp_] + vreads, writes=[bacc])
                pend = nxt

        def normalize(acc, bacc, dst, bdst, tmp, btmp):
            op("dve", lambda e: e.reciprocal(out=rd[64:65, :], in_=acc[64:65, :]), reads=[bacc], writes=[b_rd])
            op("pe", lambda e: e.matmul(PB[7][0:64, :], lhsT=ones_f[64:65, 0:64], rhs=rd[64:65, :], start=True, stop=True),
               reads=[b_rd, b_ones], writes=[b_pb[7]])
            op("act", lambda e: e.activation(out=tmp[0:64, :], in_=PB[7][0:64, :], func=AF.Copy), reads=[b_pb[7]], writes=[btmp])
            op("dve", lambda e: e.tensor_tensor(out=dst, in0=acc[0:64, :], in1=tmp[0:64, :], op=ALU.mult),
               reads=[bacc, btmp], writes=[bdst])

        for c_ in range(4):
            q0 = c_ * 512
            qs = slice(q0, q0 + 512)
            nk = (c_ + 1) * 512
            for h in range(8):
                kt_ = kTh[h % 2]; bkt = b_kTh[h % 2]
                for k2 in range(c_ + 1):
                    op("pe", lambda e: e.matmul(PB[6][0:64, :], lhsT=wkvk[:, h, :], rhs=ckvnT[:, k2 * 512:(k2 + 1) * 512],
                                                start=True, stop=True), reads=[b_w, b_ckvn], writes=[b_pb[6]])
                    op("dve", lambda e: e.tensor_copy(out=kt_[0:64, k2 * 512:(k2 + 1) * 512], in_=PB[6][0:64, :]),
                       reads=[b_pb[6]], writes=[bkt])
                op("pool", lambda e: e.tensor_copy(out=kt_[64:96, 0:nk], in_=krT[64:96, 0:nk]), reads=[b_kr], writes=[bkt])
                for (wq_, pbi) in ((wqA, 6), (wqB, 7)):
                    for kc in range(2):
                        op("pe", lambda e: e.matmul(PB[pbi][0:96, :], lhsT=wq_[:, kc, h, :], rhs=cqnT[:, kc, qs],
                                                    start=(kc == 0), stop=(kc == 1)), reads=[b_w, b_cqn], writes=[b_pb[pbi]])
                q_ = qh[h % 2]; bq_ = b_qh[h % 2]
                op("dve", lambda e: e.tensor_tensor(out=tg[0][0:96, :], in0=PB[6][0:96, :], in1=Cm[:, qs], op=ALU.mult),
                   reads=[b_pb[6], b_tm], writes=[b_tg[0]])
                op("dve", lambda e: e.tensor_tensor(out=tg[1][0:96, :], in0=PB[7][0:96, :], in1=Sm[:, qs], op=ALU.mult),
                   reads=[b_pb[7], b_tm], writes=[b_tg[1]])
                op("pool", lambda e: e.tensor_tensor(out=q_[:], in0=tg[0][0:96, :], in1=tg[1][0:96, :], op=ALU.add),
                   reads=[b_tg[0], b_tg[1]], writes=[bq_])
                acc = PB[2 + h % 2]; bacc = b_pb[2 + h % 2]
                attn_map(lambda kt: kt_[:, kt * 128:(kt + 1) * 128], lambda lo, hi: q_[:, lo:hi], SC_M,
                         [(acc[0:65, :], bacc, lambda kt: vm[:, kt, h, :])], c_, [bkt], [bq_], [b_vm])
                normalize(acc, bacc, mixT[:, h, :], b_mix, tg[2], b_tg[2])
            for h in range(4):
                for j in range(2):
                    js = slice(j * 64, (j + 1) * 64)
                    accs = [(PB[2 + 2 * j + hf][0:65, :], b_pb[2 + 2 * j + hf], (lambda kt, hf=hf: vd[:, kt, h, hf, :]))
                            for hf in range(2)]
                    attn_map(lambda kt: kdT[js, h, kt * 128:(kt + 1) * 128], lambda lo, hi: qdT[js, h, q0 + lo:q0 + hi], SC_D,
                             accs, c_, [b_kd], [b_qd], [b_vd])
                for j in range(2):
                    for hf in range(2):
                        normalize(PB[2 + 2 * j + hf], b_pb[2 + 2 * j + hf], tg[2 * j + hf][0:64, :], b_tg[2 * j + hf],
                                  tg[4], b_tg[4])
                for hf in range(2):
                    op("dve", lambda e: e.scalar_tensor_tensor(out=tg[hf][0:64, :], in0=tg[2 + hf][0:64, :], scalar=lamc[0:64, 3:4],
                                                               in1=tg[hf][0:64, :], op0=ALU.mult, op1=ALU.add),
                       reads=[b_tg[2 + hf], b_tg[hf], b_lam], writes=[b_tg[hf]])
                    op("act", lambda e: e.activation(out=sqb[hf][:], in_=tg[hf][0:64, :], func=AF.Square),
                       reads=[b_tg[hf]], writes=[b_sqb[hf]])
                for hf in range(2):
                    op("pe", lambda e: e.matmul(PB[7][0:64, :], lhsT=ones_b[0:64, 0:64], rhs=sqb[hf][:], start=(hf == 0), stop=(hf == 1)),
                       reads=[b_ones, b_sqb[hf]], writes=[b_pb[7]])
                rstd_bc(PB[7], b_pb[7], 128.0, 1e-5, tg[4], b_tg[4], rows=64)
                for hf in range(2):
                    op("dve", lambda e: e.tensor_scalar(out=tg[2 + hf][0:64, :], in0=tg[hf][0:64, :], scalar1=gsub[:, hf:hf + 1],
                                                        scalar2=0.8, op0=ALU.mult, op1=ALU.mult),
                       reads=[b_tg[hf], b_g], writes=[b_tg[2 + hf]])
                    op("dve", lambda e: e.tensor_tensor(out=mixT[:, 8 + 2 * h + hf, :], in0=tg[2 + hf][0:64, :], in1=tg[4][0:64, :],
                                                        op=ALU.mult), reads=[b_tg[2 + hf], b_tg[4]], writes=[b_mix])
            for tt in range(4):
                r0 = tok0 + q0 + tt * 128
                dma("sp", lambda e: e.dma_start(out=xr[:], in_=x_d[r0:r0 + 128, :]), writes=[b_xr])
                for cg in range(2):
                    for i in range(16):
                        op("pe", lambda e: e.matmul(PB[4 + cg][:], lhsT=mixT[:, i, tt * 128:(tt + 1) * 128],
                                                    rhs=wo[:, i, cg * 512:(cg + 1) * 512], start=(i == 0), stop=(i == 15)),
                           reads=[b_mix, b_wo], writes=[b_pb[4 + cg]])
                for cg in range(2):
                    op("act", lambda e: e.activation(out=ty[:, cg * 512:(cg + 1) * 512], in_=PB[4 + cg][:], func=AF.Copy),
                       reads=[b_pb[4 + cg]], writes=[b_ty])
                r = rstd_col("act", ty[:], ssB, D, EPS, [b_ty], b_ssB, junkB, b_junkB)
                op("dve", lambda e: e.scalar_tensor_tensor(out=ty[:], in0=ty[:], scalar=r, in1=GM[:, b, :],
                                                           op0=ALU.mult, op1=ALU.mult), reads=[b_ty, b_ssB, b_mod], writes=[b_ty])
                op("pool", lambda e: e.tensor_tensor(out=xm[:], in0=ty[:], in1=xr[:], op=ALU.add),
                   reads=[b_ty, b_xr], writes=[b_xm])
                if stage == "xm":
                    dma("sp", lambda e: e.dma_start(out=out_d[r0:r0 + 128, :], in_=xm[:]), reads=[b_xm], writes=[b_xmid])
                else:
                    dma("sp", lambda e: e.dma_start(out=xmid_d[r0:r0 + 128, :], in_=xm[:]), reads=[b_xm], writes=[b_xmid])
                r2 = rstd_col("act", xm[:], ssB, D, EPS, [b_xm], b_ssB, junkB, b_junkB)
                op("act", lambda e: e.activation(out=ty[:], in_=xm[:], func=AF.Copy, scale=r2), reads=[b_xm, b_ssB], writes=[b_ty])
                for kc in range(8):
                    pbk = PB[6 + kc // 4]; bpbk = b_pb[6 + kc // 4]
                    op("pe", lambda e: e.transpose(out=pbk[:, (kc % 4) * 128:(kc % 4 + 1) * 128], in_=ty[:, kc * 128:(kc + 1) * 128],
                                                   identity=ident_f[:]), reads=[b_ty, b_id], writes=[bpbk])
                for kc in range(8):
                    pbk = PB[6 + kc // 4]; bpbk = b_pb[6 + kc // 4]
                    src = pbk[:, (kc % 4) * 128:(kc % 4 + 1) * 128]
                    if kc % 2:
                        op("dve", lambda e: e.tensor_scalar(out=h2f[:, kc, :], in0=src, scalar1=FS[:, b, kc:kc + 1],
                                                            scalar2=FH[:, b, kc:kc + 1], op0=ALU.mult, op1=ALU.add),
                           reads=[bpbk, b_mod], writes=[b_h2f])
                    else:
                        op("act", lambda e: e.activation(out=h2f[:, kc, :], in_=src, func=AF.Identity,
                                                         bias=FH[:, b, kc:kc + 1], scale=FS[:, b, kc:kc + 1]),
                           reads=[bpbk, b_mod], writes=[b_h2f])
                op("pool", lambda e: e.tensor_copy(out=h2b[:], in_=h2f[:]), reads=[b_h2f], writes=[b_h2b])
                dma("sp", lambda e: e.dma_start(out=h2T_d[:, :, r0:r0 + 128].rearrange("k p t -> p k t"), in_=h2b[:]),
                    reads=[b_h2b], writes=[b_h2d])
                for kc in range(8):
                    op("pe", lambda e: e.matmul(PB[4][:, 0:NE], lhsT=h2f[:, kc, :], rhs=wr[:, kc, :], start=(kc == 0), stop=(kc == 7)),
                       reads=[b_h2f, b_w], writes=[b_pb[4]])
                lg = rt[:, 0, :]; ex = rt[:, 1, :]; mk = rt[:, 2, :]; em = rt[:, 3, :]; gg = rt[:, 4, :]; m8 = rt[:, 5, 0:8]
                op("dve", lambda e: e.tensor_tensor(out=lg, in0=PB[4][:, 0:NE], in1=brr[:], op=ALU.add),
                   reads=[b_pb[4], b_w], writes=[b_rt])
                op("dve", lambda e: e.max(out=m8, in_=lg), reads=[b_rt], writes=[b_rt])
                op("dve", lambda e: e.tensor_scalar(out=mk, in0=lg, scalar1=rt[:, 5, 3:4], scalar2=1.0, op0=ALU.is_ge, op1=ALU.mult),
                   reads=[b_rt], writes=[b_rt])
                op("dve", lambda e: e.tensor_scalar(out=rt[:, 5, 8:9], in0=rt[:, 5, 0:1], scalar1=-1.0, scalar2=0.0, op0=ALU.mult, op1=ALU.add),
                   reads=[b_rt], writes=[b_rt])
                op("act", lambda e: e.activation(out=ex, in_=lg, func=AF.Exp, bias=rt[:, 5, 8:9], scale=1.0),
                   reads=[b_rt], writes=[b_rt])
                op("dve", lambda e: e.memset(rt[:, 5, 9:10], 0.0), writes=[b_rt])
                op("dve", lambda e: e.scalar_tensor_tensor(out=em, in0=ex, scalar=1.0, in1=mk, op0=ALU.mult, op1=ALU.mult,
                                                           accum_out=rt[:, 5, 9:10]), reads=[b_rt], writes=[b_rt])
                op("dve", lambda e: e.reciprocal(out=rt[:, 5, 10:11], in_=rt[:, 5, 9:10]), reads=[b_rt], writes=[b_rt])
                op("dve", lambda e: e.tensor_scalar(out=gg, in0=em, scalar1=rt[:, 5, 10:11], scalar2=1.0, op0=ALU.mult, op1=ALU.mult),
                   reads=[b_rt], writes=[b_rt])
                op("pe", lambda e: e.transpose(out=PB[5][0:32, 0:128], in_=gg, identity=ident_f[:]),
                   reads=[b_rt, b_id], writes=[b_pb[5]])
                op("dve", lambda e: e.tensor_copy(out=gts[:], in_=PB[5][0:32, 0:128]), reads=[b_pb[5]], writes=[b_gts])
                dma("sp", lambda e: e.dma_start(out=GT_d[:, r0:r0 + 128], in_=gts[:]), reads=[b_gts], writes=[b_gtd])
        Bs.close()
        AB.close()
    P0.close()

    if stage == "xm":
        fence(c, [b_xmid])
        c.wait_all("sp", [b_xmid])
        print("instructions", c.n_ins, "waits", c.n_wait, "dsems", c.n_dsem)
        return nc

    TG = 1024
    M = Scope()
    yacc = M.T("yacc", [128, 8, TG], F32); b_yacc = M.B("yacc")
    h2g = M.T("h2g", [128, 8, TG], BF16); b_h2g = M.B("h2g")
    wgu = [M.T("wgu%d" % i, [128, 8, 2 * D], BF16) for i in range(2)]; b_wgu = [M.B("wgu0"), M.B("wgu1")]
    wdn = M.T("wdn", [128, 8, D], BF16); b_wdn = M.B("wdn")
    gtg = M.T("gtg", [32, TG], F32); b_gtg = M.B("gtg")
    bdn = M.T("bdn", [32, D], F32); bguc = M.T("bguc", [128, NE, 16], F32); b_mw = M.B("moew")
    gbc = [M.T("gbc%d" % i, [128, TG], F32) for i in range(2)]; b_gbc = [M.B("gbc0"), M.B("gbc1")]
    tA = [M.T("tA%d" % i, [128, 512], F32) for i in range(2)]; b_tA = [M.B("tA0"), M.B("tA1")]
    tS = [M.T("tS%d" % i, [128, 512], F32) for i in range(2)]; b_tS = [M.B("tS0"), M.B("tS1")]
    tU = [M.T("tU%d" % i, [128, 512], F32) for i in range(2)]; b_tU = [M.B("tU0"), M.B("tU1")]
    actT = [M.T("actT%d" % i, [128, 8, 512], BF16) for i in range(2)]; b_act = [M.B("act0"), M.B("act1")]
    GFt = M.T("GFt", [128, NB, D], F32); b_gft2 = M.B("GFt")
    tyF = M.T("tyF", [128, D], F32); b_tyF = M.B("tyF")
    xmt = M.T("xmt", [128, D], F32); b_xmt = M.B("xmt")
    ot = M.T("ot", [128, D], F32); b_ot = M.B("ot")
    ssF = M.T("ssF", [128, 8], F32); b_ssF = M.B("ssF")
    b_out = Buf("out")
    dma("sp", lambda e: [e.dma_start(out=bdn[:], in_=bdn_d), e.dma_start(out=bguc[:], in_=bguc_d)], writes=[b_mw])
    dma("sp", lambda e: e.dma_start(out=GFt[:], in_=gf_d), writes=[b_gft2])
    units = [(g_, e_) for g_ in range(NT // TG) for e_ in range(NE)]

    def load_wgu(u):
        ex_ = units[u][1]
        dma("pool", lambda e: e.dma_start(out=wgu[u % 2][:], in_=wgu_d[ex_].rearrange("(k p) n -> p k n", p=128)),
            writes=[b_wgu[u % 2]])

    def load_wdn(u):
        ex_ = units[u][1]
        dma("pool", lambda e: e.dma_start(out=wdn[:], in_=wdn_d[ex_].rearrange("(k p) n -> p k n", p=128)), writes=[b_wdn])

    load_wgu(0); load_wdn(0)
    for g in range(NT // TG):
        ts = slice(g * TG, (g + 1) * TG)
        dma("sp", lambda e: e.dma_start(out=h2g[:], in_=h2T_d[:, :, ts].rearrange("k p t -> p k t")), writes=[b_h2g])
        dma("sp", lambda e: e.dma_start(out=gtg[:], in_=GT_d[:, ts]), writes=[b_gtg])
        for dc in range(8):
            for tc in range(2):
                pbk = PB[4 + (2 * dc + tc) % 4]; bpbk = b_pb[4 + (2 * dc + tc) % 4]
                op("pe", lambda e: e.matmul(pbk[:], lhsT=bdn[:, dc * 128:(dc + 1) * 128], rhs=gtg[:, tc * 512:(tc + 1) * 512],
                                            start=True, stop=True), reads=[b_mw, b_gtg], writes=[bpbk])
                op("act", lambda e: e.activation(out=yacc[:, dc, tc * 512:(tc + 1) * 512], in_=pbk[:], func=AF.Copy),
                   reads=[bpbk], writes=[b_yacc])
        for ex in range(NE):
            u = g * NE + ex
            wi = u % 2
            w_ = wgu[wi]; bw_ = b_wgu[wi]
            if u + 1 < len(units):
                load_wgu(u + 1)
            gb_ = gbc[u % 2]; bgb_ = b_gbc[u % 2]
            dma("sp", lambda e: e.dma_start(out=gb_[:], in_=GT_d[ex:ex + 1, ts].partition_broadcast(128)), writes=[bgb_])
            for tc in range(2):
                tcs = slice(tc * 512, (tc + 1) * 512)
                at = actT[tc]; bat = b_act[tc]
                for fc in range(8):
                    pa = PB[2 * (fc % 2)]; bpa = b_pb[2 * (fc % 2)]; pu = PB[2 * (fc % 2) + 1]; bpu = b_pb[2 * (fc % 2) + 1]
                    for kc in range(8):
                        op("pe", lambda e: e.matmul(pa[:], lhsT=w_[:, kc, fc * 128:(fc + 1) * 128], rhs=h2g[:, kc, tcs],
                                                    start=(kc == 0), stop=(kc == 7)), reads=[bw_, b_h2g], writes=[bpa])
                    for kc in range(8):
                        op("pe", lambda e: e.matmul(pu[:], lhsT=w_[:, kc, D + fc * 128:D + (fc + 1) * 128], rhs=h2g[:, kc, tcs],
                                                    start=(kc == 0), stop=(kc == 7)), reads=[bw_, b_h2g], writes=[bpu])
                    a_ = tA[fc % 2]; ba_ = b_tA[fc % 2]; s_ = tS[fc % 2]; bs_ = b_tS[fc % 2]; u_ = tU[fc % 2]; bu_ = b_tU[fc % 2]
                    op("dve", lambda e: e.tensor_scalar(out=a_[:], in0=pa[:], scalar1=bguc[:, ex, fc:fc + 1], scalar2=7.0,
                                                        op0=ALU.add, op1=ALU.min), reads=[bpa, b_mw], writes=[ba_])
                    op("act", lambda e: e.activation(out=s_[:], in_=a_[:], func=AF.Sigmoid, scale=1.702), reads=[ba_], writes=[bs_])
                    op("dve", lambda e: e.tensor_scalar(out=u_[:], in0=pu[:], scalar1=bguc[:, ex, 8 + fc:9 + fc], scalar2=-7.0,
                                                        op0=ALU.add, op1=ALU.max), reads=[bpu, b_mw], writes=[bu_])
                    op("pool", lambda e: e.tensor_scalar(out=u_[:], in0=u_[:], scalar1=7.0, scalar2=1.0,
                                                         op0=ALU.min, op1=ALU.add), reads=[bu_], writes=[bu_])
                    op("pool", lambda e: e.tensor_tensor(out=a_[:], in0=a_[:], in1=s_[:], op=ALU.mult), reads=[ba_, bs_], writes=[ba_])
                    op("dve", lambda e: e.tensor_tensor(out=a_[:], in0=a_[:], in1=u_[:], op=ALU.mult), reads=[ba_, bu_], writes=[ba_])
                    op("dve", lambda e: e.tensor_tensor(out=at[:, fc, :], in0=a_[:], in1=gb_[:, tcs], op=ALU.mult),
                       reads=[ba_, bgb_], writes=[bat])
                for dc in range(8):
                    py = PB[4 + dc % 4]; bpy = b_pb[4 + dc % 4]
                    for fc in range(8):
                        op("pe", lambda e: e.matmul(py[:], lhsT=wdn[:, fc, dc * 128:(dc + 1) * 128], rhs=at[:, fc, :],
                                                    start=(fc == 0), stop=(fc == 7)), reads=[b_wdn, bat], writes=[bpy])
                    op("dve", lambda e: e.tensor_tensor(out=yacc[:, dc, tcs], in0=py[:], in1=yacc[:, dc, tcs], op=ALU.add),
                       reads=[bpy, b_yacc], writes=[b_yacc])
            if u + 1 < len(units):
                load_wdn(u + 1)
        for tt in range(TG // 128):
            r0 = g * TG + tt * 128
            bb = r0 // S
            dma("sp", lambda e: e.dma_start(out=xmt[:], in_=xmid_d[r0:r0 + 128, :]), writes=[b_xmt])
            for dc in range(8):
                pbk = PB[dc // 4]; bpbk = b_pb[dc // 4]
                op("pe", lambda e: e.transpose(out=pbk[:, (dc % 4) * 128:(dc % 4 + 1) * 128], in_=yacc[:, dc, tt * 128:(tt + 1) * 128],
                                               identity=ident_f[:]), reads=[b_yacc, b_id], writes=[bpbk])
            for cg in range(2):
                op("act", lambda e: e.activation(out=tyF[:, cg * 512:(cg + 1) * 512], in_=PB[cg][:], func=AF.Copy),
                   reads=[b_pb[cg]], writes=[b_tyF])
            r = rstd_col("act", tyF[:], ssF, D, EPS, [b_tyF], b_ssF, ot[:], b_ot)
            op("dve", lambda e: e.scalar_tensor_tensor(out=tyF[:], in0=tyF[:], scalar=r, in1=GFt[:, bb, :],
                                                       op0=ALU.mult, op1=ALU.mult), reads=[b_tyF, b_ssF, b_gft2], writes=[b_tyF])
            op("pool", lambda e: e.tensor_tensor(out=ot[:], in0=tyF[:], in1=xmt[:], op=ALU.add),
               reads=[b_tyF, b_xmt], writes=[b_ot])
            dma("sp", lambda e: e.dma_start(out=out_d[r0:r0 + 128, :], in_=ot[:]), reads=[b_ot], writes=[b_out])
    fence(c, [b_out])
    M.close()
    c.wait_all("sp", [b_out])
    print("instructions", c.n_ins, "waits", c.n_wait, "dsems", c.n_dsem)
    return nc


def fence(c, bufs):
    deps = []
    for b in bufs:
        deps.append(b.last_w)
        deps.extend(b.readers)
    for e in c.ENGS:
        c._need(e, deps)


_NC_CACHE = {}


def kernel(**inputs):
    stage = inputs.pop("_stage", "full")
    inp = {k: np.asarray(v) for k, v in inputs.items()}
    maps = host_prep(inp)
    if stage not in _NC_CACHE:
        _NC_CACHE[stage] = build(stage)
    nc = _NC_CACHE[stage]
    ncr = NCORES if stage == "full" else 1
    maps = [{k: m[k] for k in nc._in_names} for m in maps[:ncr]]
    import os
    if os.environ.get("KTRACE"):
        res = run_bass_kernel_spmd(nc, maps, core_ids=list(range(ncr)), trace=True)
        print("KTRACE exec_time_ns", res.exec_time_ns)
    else:
        res = run_bass_kernel_spmd(nc, maps, core_ids=list(range(ncr)))
    out = np.concatenate([r["out"] for r in res.results], axis=0)
    if ncr < NCORES:
        out = np.concatenate([out, np.zeros(((NCORES - ncr) * NT, D), np.float32)], 0)
    return out.reshape(16, S, D).astype(np.float32)
```

```python
import math
import numpy as np
import concourse.bass as bass
import concourse.mybir as mybir
from concourse.bass_utils import run_bass_kernel_spmd

F32 = mybir.dt.float32
BF16 = mybir.dt.bfloat16
I32 = mybir.dt.int32
ALU = mybir.AluOpType
AF = mybir.ActivationFunctionType
AX = mybir.AxisListType

NCORES = 8
D = 1024
S = 2048
NB = 2
NT = NB * S
NE = 32
PI = math.pi
CW1 = 6.28125
CW2 = 2 * math.pi - 6.28125
PI_LO = 3.1415925


class Buf:
    __slots__ = ("name", "last_w", "readers", "dsem", "dcnt")

    def __init__(self, name):
        self.name = name
        self.last_w = None
        self.readers = []
        self.dsem = None
        self.dcnt = 0


class Ctx:
    ENGS = ("pe", "act", "dve", "pool", "sp")

    def __init__(self, nc):
        self.nc = nc
        self.eng = {"pe": nc.tensor, "act": nc.scalar, "dve": nc.vector,
                    "pool": nc.gpsimd, "sp": nc.sync}
        self.sems = {}
        self.cnt = {}
        for e in self.ENGS:
            self.sems[e] = nc.alloc_semaphore("s_" + e)
            self.cnt[e] = 0
        self.waited = {}
        self.n_dsem = 0
        self.n_wait = 0
        self.n_ins = 0

    def sb(self, name, shape, dt):
        return self.nc.alloc_sbuf_tensor("sb_" + name, list(shape), dt)

    def ps(self, name, shape, dt=F32):
        return self.nc.alloc_psum_tensor("ps_" + name, list(shape), dt)

    def _need(self, eng, deps):
        best = {}
        for d in deps:
            if d is None:
                continue
            k, v = d
            if k == "pe" and eng == "pe":
                continue
            if best.get(k, 0) < v:
                best[k] = v
        for k, v in best.items():
            if self.waited.get((eng, k), 0) >= v:
                continue
            self.eng[eng].wait_ge(self.sems[k], v)
            self.waited[(eng, k)] = v
            self.n_wait += 1

    @staticmethod
    def _deps(reads, writes):
        deps = []
        for b in reads:
            deps.append(b.last_w)
        for b in writes:
            deps.append(b.last_w)
            deps.extend(b.readers)
        return deps

    def op(self, eng, fn, reads=(), writes=()):
        self._need(eng, self._deps(reads, writes))
        ins = fn(self.eng[eng])
        self.cnt[eng] += 1
        ins.then_inc(self.sems[eng], 1)
        self.n_ins += 1
        tok = (eng, self.cnt[eng])
        for b in reads:
            b.readers.append(tok)
            if len(b.readers) > 10:
                best = {}
                for k, v in b.readers:
                    if best.get(k, 0) < v:
                        best[k] = v
                b.readers = list(best.items())
        for b in writes:
            b.last_w = tok
            b.readers = []
        return ins

    def _dsem(self, b):
        if b.dsem is None:
            b.dsem = "d%d_%s" % (self.n_dsem, b.name)
            self.n_dsem += 1
            self.sems[b.dsem] = self.nc.alloc_semaphore(b.dsem)
        return b.dsem

    def dma(self, eng, fn, reads=(), writes=(), owner=None):
        self._need(eng, self._deps(reads, writes))
        own = owner if owner is not None else writes[0]
        k = self._dsem(own)
        res = fn(self.eng[eng])
        if not isinstance(res, (list, tuple)):
            res = [res]
        for ins in res:
            ins.then_inc(self.sems[k], 16)
            own.dcnt += 16
            self.n_ins += 1
        tok = (k, own.dcnt)
        for b in reads:
            b.readers.append(tok)
        for b in writes:
            b.last_w = tok
            b.readers = []
        return tok

    def wait_all(self, eng, bufs):
        self._need(eng, [b.last_w for b in bufs])


def _col(v, p=128):
    return np.ascontiguousarray(v.reshape(-1, p).T)


def _rope_perm(n_half):
    i = np.arange(2 * n_half)
    return (i + n_half) % (2 * n_half)


def host_prep(inp):
    f = np.float32
    x = inp["x"]; c = inp["c"]; pos = inp["positions"]
    w_ada = np.ascontiguousarray(inp["w_ada"][0]); b_ada = inp["b_ada"][0]
    w_in = inp["w_in"][0]
    cq = w_in[:, 0:256]; ckv = w_in[:, 256:384]; kr = w_in[:, 384:416]
    qd = w_in[:, 416:928]; kd = w_in[:, 928:1440]; vd = w_in[:, 1440:1952]
    pm = _rope_perm(16)
    z64 = np.zeros((D, 64), f); z32 = np.zeros((D, 32), f)
    groups = [cq[:, 0:128], cq[:, 128:256], ckv,
              np.concatenate([z64, kr, z32], 1), np.concatenate([z64, kr[:, pm], z32], 1)]
    pd = _rope_perm(32)
    def permd(w):
        w4 = w.reshape(D, 4, 2, 64)
        return np.ascontiguousarray(w4[:, :, :, pd]).reshape(D, 512)
    qdp = permd(qd); kdp = permd(kd)
    for h in range(4):
        groups.append(qd[:, h * 128:(h + 1) * 128])
    for h in range(4):
        groups.append(qdp[:, h * 128:(h + 1) * 128])
    for h in range(4):
        groups.append(kd[:, h * 128:(h + 1) * 128])
    for h in range(4):
        groups.append(kdp[:, h * 128:(h + 1) * 128])
    w_in_g = np.ascontiguousarray(np.stack(groups, 0))
    wq = inp["w_q_b"][0].reshape(256, 8, 96)
    wqA = np.ascontiguousarray(wq)
    wqB = np.zeros_like(wq)
    wqB[:, :, 64:96] = wq[:, :, 64:96][:, :, pm]
    wkv = inp["w_kv_b"][0].reshape(128, 8, 128)
    wkvk = np.ascontiguousarray(wkv[:, :, 0:64])
    wkvv = np.ascontiguousarray(wkv[:, :, 64:128]).reshape(128, 512)
    invf_d = (10000.0 ** (-np.arange(0, 64, 2, dtype=f) / f(64))).astype(f)
    invf_m = (10000.0 ** (-np.arange(0, 32, 2, dtype=f) / f(32))).astype(f)
    cst = np.zeros((128, 8), f)
    p = np.arange(128)
    cst[:, 0] = invf_d[p % 32]
    cst[:, 1] = np.where((p % 64) < 32, -1.0, 1.0)
    cst[64:96, 2] = invf_m[(p[64:96] - 64) % 16]
    cst[:, 3] = 1.0
    cst[64:80, 3] = -1.0
    cst[:, 4] = -PI * cst[:, 1]
    cst[:, 5] = -PI * cst[:, 3]
    cst[:, 6] = -PI
    ident = np.eye(128, dtype=f)
    maskT = (p[None, :] >= p[:, None]).astype(f)
    lam = np.stack([inp["lambda_q1"][0], inp["lambda_k1"][0], inp["lambda_q2"][0], inp["lambda_k2"][0]], 0)
    lam_rep = np.ascontiguousarray(np.broadcast_to(lam[None], (128, 4, 64))).astype(f)
    shared = dict(
        w_ada=w_ada, bada_col=_col(b_ada),
        bada_rep=np.ascontiguousarray(np.broadcast_to(
            np.concatenate([b_ada[2048:3072], b_ada[5120:6144]])[None], (128, 2048))).astype(f),
        gpre_col=_col(inp["g_mix_pre"][0]), gfpre_col=_col(inp["g_ffn_pre"][0]),
        gpost_rep=np.ascontiguousarray(np.broadcast_to(np.concatenate(
            [inp["g_mix_post"][0], inp["g_ffn_post"][0]])[None], (128, 2048))).astype(f),
        w_in_g=w_in_g, w_vd=np.ascontiguousarray(vd),
        wqA=wqA, wqB=wqB, gqa_col=_col(inp["g_q_a"][0]),
        wkvk=wkvk, wkvv=wkvv, gkva_col=_col(inp["g_kv_a"][0]),
        lam_rep=lam_rep, gsub_col=_col(inp["g_subln"][0], 64),
        w_o=np.ascontiguousarray(inp["w_o"][0]),
        w_r=np.ascontiguousarray(inp["w_router"][0]),
        br_rep=np.ascontiguousarray(np.broadcast_to(inp["b_router"][0][None], (128, NE))).astype(f),
        w_gu=np.ascontiguousarray(inp["w_gate_up"][0]),
        bgu_col=np.ascontiguousarray(inp["b_gate_up"][0].reshape(NE, 16, 128).transpose(2, 0, 1)),
        w_dn=np.ascontiguousarray(inp["w_down"][0]),
        b_dn=np.ascontiguousarray(inp["b_down"][0]),
        cst=cst, ident=ident, maskT=maskT,
    )
    maps = []
    for i in range(NCORES):
        bs = slice(i * NB, (i + 1) * NB)
        m = dict(shared)
        m["x"] = np.ascontiguousarray(x[bs].reshape(NT, D))
        cc = c[bs]
        m["ccol"] = np.ascontiguousarray(cc.reshape(NB, 8, 128).transpose(2, 1, 0))
        m["crep"] = np.ascontiguousarray(np.broadcast_to(
            m["ccol"][:, :, :, None], (128, 8, NB, 128))).astype(f)
        m["posr"] = np.ascontiguousarray(np.broadcast_to(pos[bs][:, None, :], (NB, 128, S))).astype(np.int32)
        maps.append(m)
    return maps


def build(stage="full"):
    nc = bass.Bass("TRN2", target_bir_lowering=False)
    c = Ctx(nc)

    names = []

    def din(name, shape, dt=F32):
        names.append(name)
        return nc.dram_tensor(name, list(shape), dt, kind="ExternalInput").ap()

    x_d = din("x", [NT, D]); ccol_d = din("ccol", [128, 8, NB]); crep_d = din("crep", [128, 8, NB, 128])
    posr_d = din("posr", [NB, 128, S], I32)
    wada_d = din("w_ada", [D, 6 * D]); badac_d = din("bada_col", [128, 48]); badar_d = din("bada_rep", [128, 2048])
    gpre_d = din("gpre_col", [128, 8]); gfpre_d = din("gfpre_col", [128, 8]); gpost_d = din("gpost_rep", [128, 2048])
    wing_d = din("w_in_g", [21, D, 128]); wvd_d = din("w_vd", [D, 512])
    wqA_d = din("wqA", [256, 8, 96]); wqB_d = din("wqB", [256, 8, 96]); gqa_d = din("gqa_col", [128, 2])
    wkvk_d = din("wkvk", [128, 8, 64]); wkvv_d = din("wkvv", [128, 512]); gkva_d = din("gkva_col", [128, 1])
    lam_d = din("lam_rep", [128, 4, 64]); gsub_d = din("gsub_col", [64, 2])
    wo_d = din("w_o", [D, D]); wr_d = din("w_r", [D, NE]); brr_d = din("br_rep", [128, NE])
    if stage == "full":
        wgu_d = din("w_gu", [NE, D, 2 * D]); bguc_d = din("bgu_col", [128, NE, 16])
        wdn_d = din("w_dn", [NE, D, D]); bdn_d = din("b_dn", [NE, D])
    nc._in_names = names
    cst_d = din("cst", [128, 8]); ident_d = din("ident", [128, 128]); maskT_d = din("maskT", [128, 128])
    out_d = nc.dram_tensor("out", [NT, D], F32, kind="ExternalOutput").ap()
    xmid_d = nc.dram_tensor("xmid", [NT, D], F32, kind="Internal").ap()
    h2T_d = nc.dram_tensor("h2T", [8, 128, NT], BF16, kind="Internal").ap()
    GT_d = nc.dram_tensor("GTd", [NE, NT], F32, kind="Internal").ap()
    gf_d = nc.dram_tensor("gfd", [128, NB, D], F32, kind="Internal").ap()
    b_gfd = Buf("gfd")

    op = c.op; dma = c.dma

    cst = c.sb("cst", [128, 8], F32); b_cst = Buf("cst")
    ident_f = c.sb("ident_f", [128, 128], F32); ident_b = c.sb("ident_b", [128, 128], BF16); b_id = Buf("ident")
    maskT = c.sb("maskT", [128, 128], BF16); b_mask = Buf("maskT")
    ones_b = c.sb("ones_b", [128, 128], BF16); ones_f = c.sb("ones_f", [128, 128], F32); b_ones = Buf("ones")
    dma("sp", lambda e: e.dma_start(out=cst[:], in_=cst_d), writes=[b_cst])
    dma("sp", lambda e: e.dma_start(out=ident_f[:], in_=ident_d), writes=[b_id])
    b_idb = Buf("identb")
    dma("pool", lambda e: [e.dma_start(out=ident_b[:], in_=ident_d)], writes=[b_idb])
    dma("pool", lambda e: e.dma_start(out=maskT[:], in_=maskT_d), writes=[b_mask])
    op("dve", lambda e: e.memset(ones_b[:], 1.0), writes=[b_ones])
    op("dve", lambda e: e.memset(ones_f[:], 1.0), writes=[b_ones])

    from contextlib import ExitStack
    uid = [0]

    class Scope:
        def __init__(self):
            self.es = ExitStack(); self.bufs = []
        def T(self, name, shape, dt):
            uid[0] += 1
            return self.es.enter_context(nc.sbuf_tensor("t_%s_%d" % (name, uid[0]), list(shape), dt))
        def B(self, name):
            b = Buf(name); self.bufs.append(b); return b
        def close(self):
            fence(c, self.bufs)
            self.es.close()

    def bf(ap):
        return ap.bitcast(BF16)

    MS = c.sb("MS", [128, NB, 8], F32); MH = c.sb("MH", [128, NB, 8], F32)
    FS = c.sb("FS", [128, NB, 8], F32); FH = c.sb("FH", [128, NB, 8], F32)
    b_mod = Buf("mod")
    lamc = c.sb("lamc", [128, 4], F32); b_lam = Buf("lam")
    gqa = c.sb("gqa", [128, 2], F32); gkva = c.sb("gkva", [128, 1], F32); gsub = c.sb("gsub", [64, 2], F32)
    b_g = Buf("gains")
    dma("sp", lambda e: [e.dma_start(out=gqa[:], in_=gqa_d), e.dma_start(out=gkva[:], in_=gkva_d),
                         e.dma_start(out=gsub[:], in_=gsub_d)], writes=[b_g])

    P0 = Scope()
    GM = P0.T("GM", [128, NB, D], F32)
    PB = [c.ps("pb%d" % i, [128, 512], F32) for i in range(8)]
    b_pb = [Buf("pb%d" % i) for i in range(8)]

    with nc.sbuf_tensor("t_ccol", [128, 8, NB], F32) as ccol, \
         nc.sbuf_tensor("t_crep", [128, 8, NB, 128], F32) as crep, \
         nc.sbuf_tensor("t_wa0", [128, 8, 512], F32) as wa0, \
         nc.sbuf_tensor("t_wa1", [128, 8, 512], F32) as wa1, \
         nc.sbuf_tensor("t_badac", [128, 48], F32) as badac, \
         nc.sbuf_tensor("t_badar", [128, 2048], F32) as badar, \
         nc.sbuf_tensor("t_gpre", [128, 8], F32) as gpre, \
         nc.sbuf_tensor("t_gfpre", [128, 8], F32) as gfpre, \
         nc.sbuf_tensor("t_gpost", [128, 2048], F32) as gpost, \
         nc.sbuf_tensor("t_modc", [128, 48, NB], F32) as modc, \
         nc.sbuf_tensor("t_lamt", [128, 4, 64], F32) as lamt, \
         nc.sbuf_tensor("t_lamj", [128, 64], F32) as lamj, \
         nc.sbuf_tensor("t_GF", [128, NB, D], F32) as GF:
        b_cc = Buf("cc"); b_wa = [Buf("wa0"), Buf("wa1")]; b_misc = Buf("misc0"); b_modc = Buf("modc")
        wa = [wa0, wa1]
        dma("sp", lambda e: [e.dma_start(out=ccol[:], in_=ccol_d), e.dma_start(out=crep[:], in_=crep_d)],
            writes=[b_cc])
        dma("sp", lambda e: [e.dma_start(out=badac[:], in_=badac_d), e.dma_start(out=badar[:], in_=badar_d),
                             e.dma_start(out=gpre[:], in_=gpre_d), e.dma_start(out=gfpre[:], in_=gfpre_d),
                             e.dma_start(out=gpost[:], in_=gpost_d), e.dma_start(out=lamt[:], in_=lam_d)],
            writes=[b_misc])
        op("act", lambda e: e.activation(out=ccol[:], in_=ccol[:], func=AF.Silu), reads=[b_cc], writes=[b_cc])
        op("act", lambda e: e.activation(out=crep[:], in_=crep[:], func=AF.Silu), reads=[b_cc], writes=[b_cc])
        op("dve", lambda e: e.memset(lamc[:], 0.0), writes=[b_lam])
        for j in range(2):
            op("dve", lambda e: e.scalar_tensor_tensor(out=lamj[:], in0=lamt[:, 2 * j, :], scalar=1.0,
                                                       in1=lamt[:, 2 * j + 1, :], op0=ALU.mult, op1=ALU.mult,
                                                       accum_out=lamc[:, j:j + 1]),
               reads=[b_misc], writes=[b_lam, b_misc])
        op("act", lambda e: e.activation(out=lamc[:, 0:2], in_=lamc[:, 0:2], func=AF.Exp), reads=[b_lam], writes=[b_lam])
        op("dve", lambda e: e.tensor_tensor(out=lamc[:, 2:3], in0=lamc[:, 0:1], in1=lamc[:, 1:2], op=ALU.subtract),
           reads=[b_lam], writes=[b_lam])
        op("dve", lambda e: e.tensor_scalar(out=lamc[:, 3:4], in0=lamc[:, 2:3], scalar1=0.2, scalar2=-1.0,
                                            op0=ALU.add, op1=ALU.mult), reads=[b_lam], writes=[b_lam])
        for sl in range(12):
            w = wa[sl % 2]; bw = b_wa[sl % 2]
            dma("sp", lambda e: e.dma_start(out=w[:], in_=wada_d[:, sl * 512:(sl + 1) * 512]
                                            .rearrange("(k p) n -> p k n", p=128)), writes=[bw])
            if sl in (4, 5, 10, 11):
                gi = 0 if sl < 6 else 1
                half = sl % 2 if sl < 6 else (sl - 10)
                dst = GM if gi == 0 else GF
                for b in range(NB):
                    pbk = PB[b]; bpb = b_pb[b]
                    for kc in range(8):
                        op("pe", lambda e: e.matmul(pbk[:], lhsT=crep[:, kc, b, :], rhs=w[:, kc, :],
                                                    start=(kc == 0), stop=(kc == 7)),
                           reads=[b_cc, bw], writes=[bpb])
                    cs = slice(half * 512, (half + 1) * 512)
                    rs = slice(gi * 1024 + half * 512, gi * 1024 + (half + 1) * 512)
                    op("dve", lambda e: e.tensor_tensor(out=dst[:, b, cs], in0=pbk[:], in1=badar[:, rs], op=ALU.add),
                       reads=[bpb, b_misc], writes=[b_mod])
                    op("dve", lambda e: e.tensor_tensor(out=dst[:, b, cs], in0=dst[:, b, cs], in1=gpost[:, rs],
                                                        op=ALU.mult), reads=[b_mod, b_misc], writes=[b_mod])
            else:
                pbk = PB[2 + sl % 2]; bpb = b_pb[2 + sl % 2]
                for jj in range(4):
                    for kc in range(8):
                        op("pe", lambda e: e.matmul(pbk[:, jj * NB:(jj + 1) * NB], lhsT=w[:, kc, jj * 128:(jj + 1) * 128],
                                                    rhs=ccol[:, kc, :], start=(kc == 0), stop=(kc == 7)),
                           reads=[b_cc, bw], writes=[bpb])
                op("dve", lambda e: e.tensor_copy(out=modc[:, sl * 4:(sl + 1) * 4, :],
                                                  in_=pbk[:, 0:4 * NB].rearrange("p (j b) -> p j b", b=NB)),
                   reads=[bpb], writes=[b_modc])
        for b in range(NB):
            op("dve", lambda e: e.tensor_tensor(out=MH[:, b, :], in0=modc[:, 0:8, b], in1=badac[:, 0:8], op=ALU.add),
               reads=[b_modc, b_misc], writes=[b_mod])
            op("dve", lambda e: e.tensor_tensor(out=FH[:, b, :], in0=modc[:, 24:32, b], in1=badac[:, 24:32], op=ALU.add),
               reads=[b_modc, b_misc], writes=[b_mod])
            for (dst, j0, gg) in ((MS, 8, gpre), (FS, 32, gfpre)):
                op("dve", lambda e: e.scalar_tensor_tensor(out=dst[:, b, :], in0=modc[:, j0:j0 + 8, b], scalar=1.0,
                                                           in1=badac[:, j0:j0 + 8], op0=ALU.add, op1=ALU.add),
                   reads=[b_modc, b_misc], writes=[b_mod])
                op("dve", lambda e: e.tensor_tensor(out=dst[:, b, :], in0=dst[:, b, :], in1=gg[:], op=ALU.mult),
                   reads=[b_mod, b_misc], writes=[b_mod])
        b_gft = Buf("gft")
        op("dve", lambda e: e.tensor_copy(out=GF[:, :, 0:1], in_=GF[:, :, 0:1]), reads=[b_mod], writes=[b_gft])
        dma("sp", lambda e: e.dma_start(out=gf_d, in_=GF[:]), reads=[b_gft, b_mod], writes=[b_gfd])
        fence(c, [b_cc, b_misc, b_modc, b_gft, b_gfd] + b_wa)


    wqA = P0.T("wqA", [128, 2, 8, 96], BF16); wqB = P0.T("wqB", [128, 2, 8, 96], BF16)
    wkvk = P0.T("wkvk", [128, 8, 64], BF16); wkvv = P0.T("wkvv", [128, 512], BF16)
    wr = P0.T("wr", [128, 8, NE], F32); brr = P0.T("brr", [128, NE], F32)
    b_w = P0.B("attw")
    dma("pool", lambda e: [e.dma_start(out=wqA[:], in_=wqA_d.rearrange("(k p) h n -> p k h n", p=128)),
                           e.dma_start(out=wqB[:], in_=wqB_d.rearrange("(k p) h n -> p k h n", p=128)),
                           e.dma_start(out=wkvk[:], in_=wkvk_d), e.dma_start(out=wkvv[:], in_=wkvv_d)],
        writes=[b_w])
    b_wr = P0.B("wr")
    dma("sp", lambda e: [e.dma_start(out=wr[:], in_=wr_d.rearrange("(k p) n -> p k n", p=128)),
                         e.dma_start(out=brr[:], in_=brr_d)], writes=[b_wr])
    EPS = 1e-6
    SC_M = 96.0 ** -0.5
    SC_D = 64.0 ** -0.5

    def rstd_col(eng_sq, src, ss, n, eps, reads, bss, junk, bjunk):
        op("dve", lambda e: e.memset(ss[:, 0:1], 0.0), writes=[bss])
        op("act", lambda e: e.activation(out=junk, in_=src, func=AF.Square, accum_out=ss[:, 0:1]),
           reads=reads + [bss], writes=[bss, bjunk])
        op("dve", lambda e: e.tensor_scalar(out=ss[:, 1:2], in0=ss[:, 0:1], scalar1=1.0 / n, scalar2=eps,
                                            op0=ALU.mult, op1=ALU.add), reads=[bss], writes=[bss])
        op("dve", lambda e: e.reciprocal(out=ss[:, 2:3], in_=ss[:, 1:2]), reads=[bss], writes=[bss])
        op("act", lambda e: e.activation(out=ss[:, 3:4], in_=ss[:, 2:3], func=AF.Sqrt), reads=[bss], writes=[bss])
        return ss[:, 3:4]

    def rstd_bc(pbank, bpbank, n, eps, dst, bdst, rows=128):
        op("dve", lambda e: e.tensor_scalar(out=dst[0:rows, :], in0=pbank[0:rows, :], scalar1=1.0 / n, scalar2=eps,
                                            op0=ALU.mult, op1=ALU.add), reads=[bpbank], writes=[bdst])
        op("dve", lambda e: e.reciprocal(out=dst[0:rows, :], in_=dst[0:rows, :]), reads=[bdst], writes=[bdst])
        op("act", lambda e: e.activation(out=dst[0:rows, :], in_=dst[0:rows, :], func=AF.Sqrt), reads=[bdst], writes=[bdst])

    for b in range(NB):
        tok0 = b * S
        AB = Scope()
        cqnT = AB.T("cqnT", [128, 2, S], BF16); b_cqn = AB.B("cqn")
        ckvnT = AB.T("ckvnT", [128, S], BF16); b_ckvn = AB.B("ckvn")
        krT = AB.T("krT", [96, S], BF16); b_kr = AB.B("kr")
        vm = AB.T("vm", [128, 16, 8, 65], BF16); b_vm = AB.B("vm")
        qdT = AB.T("qdT", [128, 4, S], BF16); b_qd = AB.B("qd")
        kdT = AB.T("kdT", [128, 4, S], BF16); b_kd = AB.B("kd")
        vd = AB.T("vd", [128, 16, 4, 2, 65], BF16); b_vd = AB.B("vd")
        Cm = AB.T("Cm", [96, S], BF16); Sm = AB.T("Sm", [96, S], BF16); b_tm = AB.B("tabm")
        op("pool", lambda e: e.memset(vm[:], 1.0), writes=[b_vm])
        op("pool", lambda e: e.memset(vd[:], 1.0), writes=[b_vd])
        op("pool", lambda e: e.memset(krT[:], 0.0), writes=[b_kr])

        A = Scope()
        hT = A.T("hT", [128, 8, S], BF16); b_hT = A.B("hT")
        wing_m = A.T("wing_m", [128, 8, 640], BF16); b_wm = A.B("wing_m")
        wing_p = [A.T("wing_p%d" % i, [128, 8, 256], BF16) for i in range(2)]; b_wp = [A.B("wp0"), A.B("wp1")]
        wvd = A.T("wvd", [128, 8, 512], BF16); b_wvd = A.B("wvd")
        Cd = A.T("Cd", [128, S], BF16); Sd = A.T("Sd", [128, S], BF16); b_td = A.B("tabd")
        posi = A.T("posi", [128, 512], I32); posf = A.T("posf", [128, 512], F32); angt = A.T("angt", [128, 512], F32)
        angi = A.T("angi", [128, 512], I32); angk = A.T("angk", [128, 512], F32)
        b_pos = A.B("pos"); b_ang = A.B("ang"); b_angi = A.B("angi"); b_angk = A.B("angk")
        xt = [A.T("xt%d" % i, [128, D], F32) for i in range(2)]; b_xt = [A.B("xt0"), A.B("xt1")]
        xn = [A.T("xn%d" % i, [128, D], BF16) for i in range(2)]; b_xn = [A.B("xn0"), A.B("xn1")]
        ssA = A.T("ssA", [128, 8], F32); b_ssA = A.B("ssA")
        tb = [A.T("tb%d" % i, [128, 512], BF16) for i in range(2)]; b_tb = [A.B("tb0"), A.B("tb1")]
        tf = [A.T("tf%d" % i, [128, 512], F32) for i in range(3)]; b_tf = [A.B("tf%d" % i) for i in range(3)]

        for g in range(5):
            dma("pool", lambda e: e.dma_start(out=wing_m[:, :, g * 128:(g + 1) * 128],
                                              in_=wing_d[g].rearrange("(k p) n -> p k n", p=128)), writes=[b_wm])
        dma("pool", lambda e: e.dma_start(out=wvd[:], in_=wvd_d.rearrange("(k p) n -> p k n", p=128)), writes=[b_wvd])
        for ch in range(4):
            cs = slice(ch * 512, (ch + 1) * 512)
            dma("sp", lambda e: e.dma_start(out=posi[:], in_=posr_d[b][:, cs]), writes=[b_pos])
            op("dve", lambda e: e.tensor_copy(out=posf[:], in_=posi[:]), reads=[b_pos], writes=[b_pos])
            for (Ct, St, rows, ci, si, bt) in ((Cd, Sd, 128, 0, 1, b_td), (Cm, Sm, 96, 2, 3, b_tm)):
                for (dstT, shift, use_sgn) in ((Ct, 0.5 * PI, False), (St, 0.0, True)):
                    R = slice(0, rows)
                    op("dve", lambda e: e.tensor_scalar(out=angt[R, :], in0=posf[R, :], scalar1=cst[R, ci:ci + 1],
                                                        scalar2=shift, op0=ALU.mult, op1=ALU.add),
                       reads=[b_pos, b_cst], writes=[b_ang])
                    op("dve", lambda e: e.tensor_scalar(out=angi[R, :], in0=angt[R, :], scalar1=1.0 / (2 * PI), scalar2=0.0,
                                                        op0=ALU.mult, op1=ALU.add), reads=[b_ang], writes=[b_angi])
                    op("dve", lambda e: e.tensor_copy(out=angk[R, :], in_=angi[R, :]), reads=[b_angi], writes=[b_angk])
                    op("dve", lambda e: e.scalar_tensor_tensor(out=angt[R, :], in0=angk[R, :], scalar=-CW1, in1=angt[R, :],
                                                               op0=ALU.mult, op1=ALU.add), reads=[b_angk, b_ang], writes=[b_ang])
                    op("dve", lambda e: e.scalar_tensor_tensor(out=angt[R, :], in0=angk[R, :], scalar=-CW2, in1=angt[R, :],
                                                               op0=ALU.mult, op1=ALU.add), reads=[b_angk, b_ang], writes=[b_ang])
                    op("dve", lambda e: e.tensor_scalar(out=angk[R, :], in0=angt[R, :], scalar1=PI, scalar2=2 * PI,
                                                        op0=ALU.is_gt, op1=ALU.mult), reads=[b_ang], writes=[b_angk])
                    op("dve", lambda e: e.tensor_tensor(out=angt[R, :], in0=angt[R, :], in1=angk[R, :], op=ALU.subtract),
                       reads=[b_ang, b_angk], writes=[b_ang])
                    op("dve", lambda e: e.tensor_scalar(out=angt[R, :], in0=angt[R, :], scalar1=-PI_LO, scalar2=PI_LO,
                                                        op0=ALU.max, op1=ALU.min), reads=[b_ang], writes=[b_ang])
                    if use_sgn:
                        op("act", lambda e: e.activation(out=dstT[R, cs], in_=angt[R, :], func=AF.Sin, scale=cst[R, si:si + 1]),
                           reads=[b_ang, b_cst], writes=[bt])
                    else:
                        op("act", lambda e: e.activation(out=dstT[R, cs], in_=angt[R, :], func=AF.Sin),
                           reads=[b_ang, b_cst], writes=[bt])
        for t in range(16):
            xi = xt[t % 2]; bxi = b_xt[t % 2]; xni = xn[t % 2]; bxni = b_xn[t % 2]
            dma("sp", lambda e: e.dma_start(out=xi[:], in_=x_d[tok0 + t * 128: tok0 + (t + 1) * 128, :]), writes=[bxi])
            r = rstd_col("act", xi[:], ssA, D, EPS, [bxi], b_ssA, xni[:], bxni)
            op("act", lambda e: e.activation(out=xni[:], in_=xi[:], func=AF.Copy, scale=r), reads=[bxi, b_ssA], writes=[bxni])
            pT = bf(PB[6][:]).rearrange("p (k t) -> p k t", t=128)
            for kc in range(8):
                op("pe", lambda e: e.transpose(out=pT[:, kc, :], in_=xni[:, kc * 128:(kc + 1) * 128], identity=ident_b[:]),
                   reads=[bxni, b_idb], writes=[b_pb[6]])
            for kc in range(8):
                dst = hT[:, kc, t * 128:(t + 1) * 128]
                if kc % 2 == 0:
                    op("dve", lambda e: e.tensor_scalar(out=dst, in0=pT[:, kc, :], scalar1=MS[:, b, kc:kc + 1],
                                                        scalar2=MH[:, b, kc:kc + 1], op0=ALU.mult, op1=ALU.add),
                       reads=[b_pb[6], b_mod], writes=[b_hT])
                else:
                    op("act", lambda e: e.activation(out=dst, in_=pT[:, kc, :], func=AF.Identity,
                                                     bias=MH[:, b, kc:kc + 1], scale=MS[:, b, kc:kc + 1]),
                       reads=[b_pb[6], b_mod], writes=[b_hT])
        for ch in range(4):
            cs = slice(ch * 512, (ch + 1) * 512)
            for g in range(5):
                M = 128 if g < 3 else 96
                for kc in range(8):
                    op("pe", lambda e: e.matmul(PB[g][0:M, :], lhsT=wing_m[:, kc, g * 128:g * 128 + M], rhs=hT[:, kc, cs],
                                                start=(kc == 0), stop=(kc == 7)),
                       reads=[b_wm, b_hT], writes=[b_pb[g]])
            for i in range(2):
                op("act", lambda e: e.activation(out=tb[i][:], in_=PB[i][:], func=AF.Square), reads=[b_pb[i]], writes=[b_tb[i]])
            for i in range(2):
                op("pe", lambda e: e.matmul(PB[5][:], lhsT=ones_b[:], rhs=tb[i][:], start=(i == 0), stop=(i == 1)),
                   reads=[b_ones, b_tb[i]], writes=[b_pb[5]])
            rstd_bc(PB[5], b_pb[5], 256.0, EPS, tf[0], b_tf[0])
            for i in range(2):
                op("dve", lambda e: e.scalar_tensor_tensor(out=cqnT[:, i, cs], in0=PB[i][:], scalar=gqa[:, i:i + 1],
                                                           in1=tf[0][:], op0=ALU.mult, op1=ALU.mult),
                   reads=[b_pb[i], b_g, b_tf[0]], writes=[b_cqn])
            op("act", lambda e: e.activation(out=tb[0][:], in_=PB[2][:], func=AF.Square), reads=[b_pb[2]], writes=[b_tb[0]])
            op("pe", lambda e: e.matmul(PB[5][:], lhsT=ones_b[:], rhs=tb[0][:], start=True, stop=True),
               reads=[b_ones, b_tb[0]], writes=[b_pb[5]])
            rstd_bc(PB[5], b_pb[5], 128.0, EPS, tf[1], b_tf[1])
            op("dve", lambda e: e.scalar_tensor_tensor(out=ckvnT[:, cs], in0=PB[2][:], scalar=gkva[:, 0:1],
                                                       in1=tf[1][:], op0=ALU.mult, op1=ALU.mult),
               reads=[b_pb[2], b_g, b_tf[1]], writes=[b_ckvn])
            op("dve", lambda e: e.tensor_tensor(out=tf[2][64:96, :], in0=PB[3][64:96, :], in1=Cm[64:96, cs], op=ALU.mult),
               reads=[b_pb[3], b_tm], writes=[b_tf[2]])
            op("dve", lambda e: e.tensor_tensor(out=tf[0][64:96, :], in0=PB[4][64:96, :], in1=Sm[64:96, cs], op=ALU.mult),
               reads=[b_pb[4], b_tm], writes=[b_tf[0]])
            op("pool", lambda e: e.tensor_tensor(out=krT[64:96, cs], in0=tf[2][64:96, :], in1=tf[0][64:96, :], op=ALU.add),
               reads=[b_tf[2], b_tf[0]], writes=[b_kr])
        for t in range(16):
            pb = PB[6 + t % 2]; bpb = b_pb[6 + t % 2]
            op("pe", lambda e: e.matmul(pb[:], lhsT=ckvnT[:, t * 128:(t + 1) * 128], rhs=wkvv[:], start=True, stop=True),
               reads=[b_ckvn, b_w], writes=[bpb])
            op("act" if t % 2 else "dve",
               (lambda e: e.activation(out=vm[:, t, :, 0:64], in_=pb[:].rearrange("p (h d) -> p h d", d=64), func=AF.Copy))
               if t % 2 else
               (lambda e: e.tensor_copy(out=vm[:, t, :, 0:64], in_=pb[:].rearrange("p (h d) -> p h d", d=64))),
               reads=[bpb], writes=[b_vm])
        pi = 0
        for kind in range(2):
            dstT = qdT if kind == 0 else kdT; bdst = b_qd if kind == 0 else b_kd
            for h in range(4):
                wp = wing_p[pi % 2]; bwp = b_wp[pi % 2]; pi += 1
                gA = 5 + 8 * kind + h; gB = gA + 4
                dma("pool", lambda e: [e.dma_start(out=wp[:, :, 0:128], in_=wing_d[gA].rearrange("(k p) n -> p k n", p=128)),
                                       e.dma_start(out=wp[:, :, 128:256], in_=wing_d[gB].rearrange("(k p) n -> p k n", p=128))],
                    writes=[bwp])
                for ch in range(4):
                    cs = slice(ch * 512, (ch + 1) * 512)
                    pa = PB[2 * (ch % 2)]; bpa = b_pb[2 * (ch % 2)]; pbb = PB[2 * (ch % 2) + 1]; bpbb = b_pb[2 * (ch % 2) + 1]
                    for kc in range(8):
                        op("pe", lambda e: e.matmul(pa[:], lhsT=wp[:, kc, 0:128], rhs=hT[:, kc, cs], start=(kc == 0), stop=(kc == 7)),
                           reads=[bwp, b_hT], writes=[bpa])
                    for kc in range(8):
                        op("pe", lambda e: e.matmul(pbb[:], lhsT=wp[:, kc, 128:256], rhs=hT[:, kc, cs], start=(kc == 0), stop=(kc == 7)),
                           reads=[bwp, b_hT], writes=[bpbb])
                    op("dve", lambda e: e.tensor_tensor(out=tf[0][:], in0=pa[:], in1=Cd[:, cs], op=ALU.mult),
                       reads=[bpa, b_td], writes=[b_tf[0]])
                    op("dve", lambda e: e.tensor_tensor(out=tf[1][:], in0=pbb[:], in1=Sd[:, cs], op=ALU.mult),
                       reads=[bpbb, b_td], writes=[b_tf[1]])
                    op("pool", lambda e: e.tensor_tensor(out=dstT[:, h, cs], in0=tf[0][:], in1=tf[1][:], op=ALU.add),
                       reads=[b_tf[0], b_tf[1]], writes=[bdst])
        for t in range(16):
            pb = PB[6 + t % 2]; bpb = b_pb[6 + t % 2]
            for kc in range(8):
                op("pe", lambda e: e.matmul(pb[:], lhsT=hT[:, kc, t * 128:(t + 1) * 128], rhs=wvd[:, kc, :],
                                            start=(kc == 0), stop=(kc == 7)), reads=[b_hT, b_wvd], writes=[bpb])
            src = pb[:].rearrange("p (h j d) -> p h j d", h=4, j=2)
            if t % 2:
                op("act", lambda e: e.activation(out=vd[:, t, :, :, 0:64], in_=src, func=AF.Copy), reads=[bpb], writes=[b_vd])
            else:
                op("dve", lambda e: e.tensor_copy(out=vd[:, t, :, :, 0:64], in_=src), reads=[bpb], writes=[b_vd])
        A.close()

        Bs = Scope()
        wo = Bs.T("wo", [64, 16, D], BF16); b_wo = Bs.B("wo")
        dma("pool", lambda e: e.dma_start(out=wo[:], in_=wo_d.rearrange("(i p) n -> p i n", p=64)), writes=[b_wo])
        mixT = Bs.T("mixT", [64, 16, 512], BF16); b_mix = Bs.B("mix")
        kTh = [Bs.T("kTh%d" % i, [96, S], BF16) for i in range(2)]; b_kTh = [Bs.B("kTh0"), Bs.B("kTh1")]
        pt = [Bs.T("pt%d" % i, [128, 512], BF16) for i in range(3)]; b_pt = [Bs.B("pt%d" % i) for i in range(3)]
        qh = [Bs.T("qh%d" % i, [96, 512], BF16) for i in range(2)]; b_qh = [Bs.B("qh0"), Bs.B("qh1")]
        tg = [Bs.T("tg%d" % i, [128, 512], F32) for i in range(5)]; b_tg = [Bs.B("tg%d" % i) for i in range(5)]
        rd = Bs.T("rd", [128, 512], F32); b_rd = Bs.B("rd")
        sqb = [Bs.T("sqb%d" % i, [64, 512], BF16) for i in range(2)]; b_sqb = [Bs.B("sqb0"), Bs.B("sqb1")]
        xr = Bs.T("xr", [128, D], F32); b_xr = Bs.B("xr")
        ty = Bs.T("ty", [128, D], F32); b_ty = Bs.B("ty")
        xm = Bs.T("xm", [128, D], F32); b_xm = Bs.B("xm")
        ssB = Bs.T("ssB", [128, 8], F32); b_ssB = Bs.B("ssB")
        h2f = Bs.T("h2f", [128, 8, 128], F32); b_h2f = Bs.B("h2f")
        junkB = h2f[:].rearrange("p k t -> p (k t)"); b_junkB = b_h2f
        h2b = Bs.T("h2b", [128, 8, 128], BF16); b_h2b = Bs.B("h2b")
        rt = Bs.T("rt", [128, 6, NE], F32); b_rt = Bs.B("rt")
        gts = Bs.T("gts", [32, 128], F32); b_gts = Bs.B("gts")
        b_xmid = Bs.B("xmid_d"); b_h2d = Bs.B("h2T_d"); b_gtd = Bs.B("GT_d")
        pti = [0]; sbi = [0]

        def attn_map(kT_ap_fn, q_ap_fn, scale, pv_list, c_, kreads, qreads, vreads):
            nkt = 4 * c_ + 4

            def issue_S(kt):
                lo = max(0, kt * 128 - c_ * 512); n = 512 - lo
                sb_ = PB[sbi[0] % 2]; bsb = b_pb[sbi[0] % 2]; sbi[0] += 1
                op("pe", lambda e: e.matmul(sb_[:, 0:n], lhsT=kT_ap_fn(kt), rhs=q_ap_fn(lo, 512), start=True, stop=True),
                   reads=kreads + qreads, writes=[bsb])
                p_ = pt[pti[0] % 3]; bp_ = b_pt[pti[0] % 3]; pti[0] += 1
                op("act", lambda e: e.activation(out=p_[:, 0:n], in_=sb_[:, 0:n], func=AF.Exp, scale=scale),
                   reads=[bsb], writes=[bp_])
                if kt >= 4 * c_:
                    op("pool", lambda e: e.tensor_tensor(out=p_[:, 0:128], in0=p_[:, 0:128], in1=maskT[:], op=ALU.mult),
                       reads=[bp_, b_mask], writes=[bp_])
                return (p_, bp_, lo, n)

            pend = issue_S(0)
            for kt in range(nkt):
                nxt = issue_S(kt + 1) if kt + 1 < nkt else None
                p_, bp_, lo, n = pend
                for (acc, bacc, v_fn) in pv_list:
                    op("pe", lambda e: e.matmul(acc[:, lo:512], lhsT=v_fn(kt), rhs=p_[:, 0:n],
                                                start=(kt == 0), stop=(kt == nkt - 1)),
                       reads=[bp_] + vreads, writes=[bacc])
                pend = nxt

        def normalize(acc, bacc, dst, bdst, tmp, btmp):
            op("dve", lambda e: e.reciprocal(out=rd[64:65, :], in_=acc[64:65, :]), reads=[bacc], writes=[b_rd])
            op("pe", lambda e: e.matmul(PB[7][0:64, :], lhsT=ones_f[64:65, 0:64], rhs=rd[64:65, :], start=True, stop=True),
               reads=[b_rd, b_ones], writes=[b_pb[7]])
            op("act", lambda e: e.activation(out=tmp[0:64, :], in_=PB[7][0:64, :], func=AF.Copy), reads=[b_pb[7]], writes=[btmp])
            op("dve", lambda e: e.tensor_tensor(out=dst, in0=acc[0:64, :], in1=tmp[0:64, :], op=ALU.mult),
               reads=[bacc, btmp], writes=[bdst])

        for c_ in range(4):
            q0 = c_ * 512
            qs = slice(q0, q0 + 512)
            nk = (c_ + 1) * 512
            for h in range(8):
                kt_ = kTh[h % 2]; bkt = b_kTh[h % 2]
                for k2 in range(c_ + 1):
                    op("pe", lambda e: e.matmul(PB[6][0:64, :], lhsT=wkvk[:, h, :], rhs=ckvnT[:, k2 * 512:(k2 + 1) * 512],
                                                start=True, stop=True), reads=[b_w, b_ckvn], writes=[b_pb[6]])
                    op("dve", lambda e: e.tensor_copy(out=kt_[0:64, k2 * 512:(k2 + 1) * 512], in_=PB[6][0:64, :]),
                       reads=[b_pb[6]], writes=[bkt])
                op("pool", lambda e: e.tensor_copy(out=kt_[64:96, 0:nk], in_=krT[64:96, 0:nk]), reads=[b_kr], writes=[bkt])
                for (wq_, pbi) in ((wqA, 6), (wqB, 7)):
                    for kc in range(2):
                        op("pe", lambda e: e.matmul(PB[pbi][0:96, :], lhsT=wq_[:, kc, h, :], rhs=cqnT[:, kc, qs],
                                                    start=(kc == 0), stop=(kc == 1)), reads=[b_w, b_cqn], writes=[b_pb[pbi]])
                q_ = qh[h % 2]; bq_ = b_qh[h % 2]
                op("dve", lambda e: e.tensor_tensor(out=tg[0][0:96, :], in0=PB[6][0:96, :], in1=Cm[:, qs], op=ALU.mult),
                   reads=[b_pb[6], b_tm], writes=[b_tg[0]])
                op("dve", lambda e: e.tensor_tensor(out=tg[1][0:96, :], in0=PB[7][0:96, :], in1=Sm[:, qs], op=ALU.mult),
                   reads=[b_pb[7], b_tm], writes=[b_tg[1]])
                op("pool", lambda e: e.tensor_tensor(out=q_[:], in0=tg[0][0:96, :], in1=tg[1][0:96, :], op=ALU.add),
                   reads=[b_tg[0], b_tg[1]], writes=[bq_])
                acc = PB[2 + h % 2]; bacc = b_pb[2 + h % 2]
                attn_map(lambda kt: kt_[:, kt * 128:(kt + 1) * 128], lambda lo, hi: q_[:, lo:hi], SC_M,
                         [(acc[0:65, :], bacc, lambda kt: vm[:, kt, h, :])], c_, [bkt], [bq_], [b_vm])
                normalize(acc, bacc, mixT[:, h, :], b_mix, tg[2], b_tg[2])
            for h in range(4):
                for j in range(2):
                    js = slice(j * 64, (j + 1) * 64)
                    accs = [(PB[2 + 2 * j + hf][0:65, :], b_pb[2 + 2 * j + hf], (lambda kt, hf=hf: vd[:, kt, h, hf, :]))
                            for hf in range(2)]
                    attn_map(lambda kt: kdT[js, h, kt * 128:(kt + 1) * 128], lambda lo, hi: qdT[js, h, q0 + lo:q0 + hi], SC_D,
                             accs, c_, [b_kd], [b_qd], [b_vd])
                for j in range(2):
                    for hf in range(2):
                        normalize(PB[2 + 2 * j + hf], b_pb[2 + 2 * j + hf], tg[2 * j + hf][0:64, :], b_tg[2 * j + hf],
                                  tg[4], b_tg[4])
                for hf in range(2):
                    op("dve", lambda e: e.scalar_tensor_tensor(out=tg[hf][0:64, :], in0=tg[2 + hf][0:64, :], scalar=lamc[0:64, 3:4],
                                                               in1=tg[hf][0:64, :], op0=ALU.mult, op1=ALU.add),
                       reads=[b_tg[2 + hf], b_tg[hf], b_lam], writes=[b_tg[hf]])
                    op("act", lambda e: e.activation(out=sqb[hf][:], in_=tg[hf][0:64, :], func=AF.Square),
                       reads=[b_tg[hf]], writes=[b_sqb[hf]])
                for hf in range(2):
                    op("pe", lambda e: e.matmul(PB[7][0:64, :], lhsT=ones_b[0:64, 0:64], rhs=sqb[hf][:], start=(hf == 0), stop=(hf == 1)),
                       reads=[b_ones, b_sqb[hf]], writes=[b_pb[7]])
                rstd_bc(PB[7], b_pb[7], 128.0, 1e-5, tg[4], b_tg[4], rows=64)
                for hf in range(2):
                    op("dve", lambda e: e.tensor_scalar(out=tg[2 + hf][0:64, :], in0=tg[hf][0:64, :], scalar1=gsub[:, hf:hf + 1],
                                                        scalar2=0.8, op0=ALU.mult, op1=ALU.mult),
                       reads=[b_tg[hf], b_g], writes=[b_tg[2 + hf]])
                    op("dve", lambda e: e.tensor_tensor(out=mixT[:, 8 + 2 * h + hf, :], in0=tg[2 + hf][0:64, :], in1=tg[4][0:64, :],
                                                        op=ALU.mult), reads=[b_tg[2 + hf], b_tg[4]], writes=[b_mix])
            for tt in range(4):
                r0 = tok0 + q0 + tt * 128
                dma("sp", lambda e: e.dma_start(out=xr[:], in_=x_d[r0:r0 + 128, :]), writes=[b_xr])
                for cg in range(2):
                    for i in range(16):
                        op("pe", lambda e: e.matmul(PB[4 + cg][:], lhsT=mixT[:, i, tt * 128:(tt + 1) * 128],
                                                    rhs=wo[:, i, cg * 512:(cg + 1) * 512], start=(i == 0), stop=(i == 15)),
                           reads=[b_mix, b_wo], writes=[b_pb[4 + cg]])
                for cg in range(2):
                    op("act", lambda e: e.activation(out=ty[:, cg * 512:(cg + 1) * 512], in_=PB[4 + cg][:], func=AF.Copy),
                       reads=[b_pb[4 + cg]], writes=[b_ty])
                r = rstd_col("act", ty[:], ssB, D, EPS, [b_ty], b_ssB, junkB, b_junkB)
                op("dve", lambda e: e.scalar_tensor_tensor(out=ty[:], in0=ty[:], scalar=r, in1=GM[:, b, :],
                                                           op0=ALU.mult, op1=ALU.mult), reads=[b_ty, b_ssB, b_mod], writes=[b_ty])
                op("pool", lambda e: e.tensor_tensor(out=xm[:], in0=ty[:], in1=xr[:], op=ALU.add),
                   reads=[b_ty, b_xr], writes=[b_xm])
                if stage == "xm":
                    dma("sp", lambda e: e.dma_start(out=out_d[r0:r0 + 128, :], in_=xm[:]), reads=[b_xm], writes=[b_xmid])
                else:
                    dma("sp", lambda e: e.dma_start(out=xmid_d[r0:r0 + 128, :], in_=xm[:]), reads=[b_xm], writes=[b_xmid])
                r2 = rstd_col("act", xm[:], ssB, D, EPS, [b_xm], b_ssB, junkB, b_junkB)
                op("act", lambda e: e.activation(out=ty[:], in_=xm[:], func=AF.Copy, scale=r2), reads=[b_xm, b_ssB], writes=[b_ty])
                for kc in range(8):
                    pbk = PB[6 + kc // 4]; bpbk = b_pb[6 + kc // 4]
                    op("pe", lambda e: e.transpose(out=pbk[:, (kc % 4) * 128:(kc % 4 + 1) * 128], in_=ty[:, kc * 128:(kc + 1) * 128],
                                                   identity=ident_f[:]), reads=[b_ty, b_id], writes=[bpbk])
                for kc in range(8):
                    pbk = PB[6 + kc // 4]; bpbk = b_pb[6 + kc // 4]
                    src = pbk[:, (kc % 4) * 128:(kc % 4 + 1) * 128]
                    if kc % 2:
                        op("dve", lambda e: e.tensor_scalar(out=h2f[:, kc, :], in0=src, scalar1=FS[:, b, kc:kc + 1],
                                                            scalar2=FH[:, b, kc:kc + 1], op0=ALU.mult, op1=ALU.add),
                           reads=[bpbk, b_mod], writes=[b_h2f])
                    else:
                        op("act", lambda e: e.activation(out=h2f[:, kc, :], in_=src, func=AF.Identity,
                                                         bias=FH[:, b, kc:kc + 1], scale=FS[:, b, kc:kc + 1]),
                           reads=[bpbk, b_mod], writes=[b_h2f])
                op("pool", lambda e: e.tensor_copy(out=h2b[:], in_=h2f[:]), reads=[b_h2f], writes=[b_h2b])
                dma("sp", lambda e: e.dma_start(out=h2T_d[:, :, r0:r0 + 128].rearrange("k p t -> p k t"), in_=h2b[:]),
                    reads=[b_h2b], writes=[b_h2d])
                for kc in range(8):
                    op("pe", lambda e: e.matmul(PB[4][:, 0:NE], lhsT=h2f[:, kc, :], rhs=wr[:, kc, :], start=(kc == 0), stop=(kc == 7)),
                       reads=[b_h2f, b_wr], writes=[b_pb[4]])
                lg = rt[:, 0, :]; ex = rt[:, 1, :]; mk = rt[:, 2, :]; em = rt[:, 3, :]; gg = rt[:, 4, :]; m8 = rt[:, 5, 0:8]
                op("dve", lambda e: e.tensor_tensor(out=lg, in0=PB[4][:, 0:NE], in1=brr[:], op=ALU.add),
                   reads=[b_pb[4], b_wr], writes=[b_rt])
                op("dve", lambda e: e.max(out=m8, in_=lg), reads=[b_rt], writes=[b_rt])
                op("dve", lambda e: e.tensor_scalar(out=mk, in0=lg, scalar1=rt[:, 5, 3:4], scalar2=1.0, op0=ALU.is_ge, op1=ALU.mult),
                   reads=[b_rt], writes=[b_rt])
                op("dve", lambda e: e.tensor_scalar(out=rt[:, 5, 8:9], in0=rt[:, 5, 0:1], scalar1=-1.0, scalar2=0.0, op0=ALU.mult, op1=ALU.add),
                   reads=[b_rt], writes=[b_rt])
                op("act", lambda e: e.activation(out=ex, in_=lg, func=AF.Exp, bias=rt[:, 5, 8:9], scale=1.0),
                   reads=[b_rt], writes=[b_rt])
                op("dve", lambda e: e.memset(rt[:, 5, 9:10], 0.0), writes=[b_rt])
                op("dve", lambda e: e.scalar_tensor_tensor(out=em, in0=ex, scalar=1.0, in1=mk, op0=ALU.mult, op1=ALU.mult,
                                                           accum_out=rt[:, 5, 9:10]), reads=[b_rt], writes=[b_rt])
                op("dve", lambda e: e.reciprocal(out=rt[:, 5, 10:11], in_=rt[:, 5, 9:10]), reads=[b_rt], writes=[b_rt])
                op("dve", lambda e: e.tensor_scalar(out=gg, in0=em, scalar1=rt[:, 5, 10:11], scalar2=1.0, op0=ALU.mult, op1=ALU.mult),
                   reads=[b_rt], writes=[b_rt])
                op("pe", lambda e: e.transpose(out=PB[5][0:32, 0:128], in_=gg, identity=ident_f[:]),
                   reads=[b_rt, b_id], writes=[b_pb[5]])
                op("dve", lambda e: e.tensor_copy(out=gts[:], in_=PB[5][0:32, 0:128]), reads=[b_pb[5]], writes=[b_gts])
                dma("sp", lambda e: e.dma_start(out=GT_d[:, r0:r0 + 128], in_=gts[:]), reads=[b_gts], writes=[b_gtd])
        Bs.close()
        AB.close()
    P0.close()

    if stage == "xm":
        fence(c, [b_xmid])
        c.wait_all("sp", [b_xmid])
        print("instructions", c.n_ins, "waits", c.n_wait, "dsems", c.n_dsem)
        return nc

    TG = 1024
    M = Scope()
    yacc = M.T("yacc", [128, 8, TG], F32); b_yacc = M.B("yacc")
    h2g = M.T("h2g", [128, 8, TG], BF16); b_h2g = M.B("h2g")
    wgu = [M.T("wgu%d" % i, [128, 8, 2 * D], BF16) for i in range(2)]; b_wgu = [M.B("wgu0"), M.B("wgu1")]
    wdn = M.T("wdn", [128, 8, D], BF16); b_wdn = M.B("wdn")
    gtg = M.T("gtg", [32, TG], F32); b_gtg = M.B("gtg")
    bdn = M.T("bdn", [32, D], F32); bguc = M.T("bguc", [128, NE, 16], F32); b_mw = M.B("moew")
    gbc = [M.T("gbc%d" % i, [128, TG], F32) for i in range(2)]; b_gbc = [M.B("gbc0"), M.B("gbc1")]
    tA = [M.T("tA%d" % i, [128, 512], F32) for i in range(2)]; b_tA = [M.B("tA0"), M.B("tA1")]
    tS = [M.T("tS%d" % i, [128, 512], F32) for i in range(2)]; b_tS = [M.B("tS0"), M.B("tS1")]
    tU = [M.T("tU%d" % i, [128, 512], F32) for i in range(2)]; b_tU = [M.B("tU0"), M.B("tU1")]
    actT = [M.T("actT%d" % i, [128, 8, 512], BF16) for i in range(2)]; b_act = [M.B("act0"), M.B("act1")]
    GFt = M.T("GFt", [128, NB, D], F32); b_gft2 = M.B("GFt")
    tyF = M.T("tyF", [128, D], F32); b_tyF = M.B("tyF")
    xmt = M.T("xmt", [128, D], F32); b_xmt = M.B("xmt")
    ot = M.T("ot", [128, D], F32); b_ot = M.B("ot")
    ssF = M.T("ssF", [128, 8], F32); b_ssF = M.B("ssF")
    b_out = Buf("out")
    dma("sp", lambda e: [e.dma_start(out=bdn[:], in_=bdn_d), e.dma_start(out=bguc[:], in_=bguc_d)], writes=[b_mw])
    dma("sp", lambda e: e.dma_start(out=GFt[:], in_=gf_d), writes=[b_gft2])
    units = [(g_, e_) for g_ in range(NT // TG) for e_ in range(NE)]

    def load_wgu(u):
        ex_ = units[u][1]
        dma("pool", lambda e: e.dma_start(out=wgu[u % 2][:], in_=wgu_d[ex_].rearrange("(k p) n -> p k n", p=128)),
            writes=[b_wgu[u % 2]])

    def load_wdn(u):
        ex_ = units[u][1]
        dma("pool", lambda e: e.dma_start(out=wdn[:], in_=wdn_d[ex_].rearrange("(k p) n -> p k n", p=128)), writes=[b_wdn])

    load_wgu(0); load_wdn(0)
    for g in range(NT // TG):
        ts = slice(g * TG, (g + 1) * TG)
        dma("sp", lambda e: e.dma_start(out=h2g[:], in_=h2T_d[:, :, ts].rearrange("k p t -> p k t")), writes=[b_h2g])
        dma("sp", lambda e: e.dma_start(out=gtg[:], in_=GT_d[:, ts]), writes=[b_gtg])
        for dc in range(8):
            for tc in range(2):
                pbk = PB[4 + (2 * dc + tc) % 4]; bpbk = b_pb[4 + (2 * dc + tc) % 4]
                op("pe", lambda e: e.matmul(pbk[:], lhsT=bdn[:, dc * 128:(dc + 1) * 128], rhs=gtg[:, tc * 512:(tc + 1) * 512],
                                            start=True, stop=True), reads=[b_mw, b_gtg], writes=[bpbk])
                op("act", lambda e: e.activation(out=yacc[:, dc, tc * 512:(tc + 1) * 512], in_=pbk[:], func=AF.Copy),
                   reads=[bpbk], writes=[b_yacc])
        for ex in range(NE):
            u = g * NE + ex
            wi = u % 2
            w_ = wgu[wi]; bw_ = b_wgu[wi]
            if u + 1 < len(units):
                load_wgu(u + 1)
            gb_ = gbc[u % 2]; bgb_ = b_gbc[u % 2]
            dma("sp", lambda e: e.dma_start(out=gb_[:], in_=GT_d[ex:ex + 1, ts].partition_broadcast(128)), writes=[bgb_])
            for tc in range(2):
                tcs = slice(tc * 512, (tc + 1) * 512)
                at = actT[tc]; bat = b_act[tc]
                for fc in range(8):
                    pa = PB[2 * (fc % 2)]; bpa = b_pb[2 * (fc % 2)]; pu = PB[2 * (fc % 2) + 1]; bpu = b_pb[2 * (fc % 2) + 1]
                    for kc in range(8):
                        op("pe", lambda e: e.matmul(pa[:], lhsT=w_[:, kc, fc * 128:(fc + 1) * 128], rhs=h2g[:, kc, tcs],
                                                    start=(kc == 0), stop=(kc == 7)), reads=[bw_, b_h2g], writes=[bpa])
                    for kc in range(8):
                        op("pe", lambda e: e.matmul(pu[:], lhsT=w_[:, kc, D + fc * 128:D + (fc + 1) * 128], rhs=h2g[:, kc, tcs],
                                                    start=(kc == 0), stop=(kc == 7)), reads=[bw_, b_h2g], writes=[bpu])
                    a_ = tA[fc % 2]; ba_ = b_tA[fc % 2]; s_ = tS[fc % 2]; bs_ = b_tS[fc % 2]; u_ = tU[fc % 2]; bu_ = b_tU[fc % 2]
                    op("dve", lambda e: e.tensor_scalar(out=a_[:], in0=pa[:], scalar1=bguc[:, ex, fc:fc + 1], scalar2=7.0,
                                                        op0=ALU.add, op1=ALU.min), reads=[bpa, b_mw], writes=[ba_])
                    op("act", lambda e: e.activation(out=s_[:], in_=a_[:], func=AF.Sigmoid, scale=1.702), reads=[ba_], writes=[bs_])
                    op("act", lambda e: e.activation(out=u_[:], in_=pu[:], func=AF.Identity, bias=bguc[:, ex, 8 + fc:9 + fc], scale=1.0),
                       reads=[bpu, b_mw], writes=[bu_])
                    op("dve", lambda e: e.tensor_scalar(out=u_[:], in0=u_[:], scalar1=-7.0, scalar2=7.0,
                                                        op0=ALU.max, op1=ALU.min), reads=[bu_], writes=[bu_])
                    op("dve", lambda e: e.tensor_tensor(out=a_[:], in0=a_[:], in1=s_[:], op=ALU.mult), reads=[ba_, bs_], writes=[ba_])
                    op("dve", lambda e: e.scalar_tensor_tensor(out=a_[:], in0=u_[:], scalar=1.0, in1=a_[:], op0=ALU.add, op1=ALU.mult),
                       reads=[ba_, bu_], writes=[ba_])
                    op("dve", lambda e: e.tensor_tensor(out=at[:, fc, :], in0=a_[:], in1=gb_[:, tcs], op=ALU.mult),
                       reads=[ba_, bgb_], writes=[bat])
                for dc in range(8):
                    py = PB[4 + dc % 4]; bpy = b_pb[4 + dc % 4]
                    for fc in range(8):
                        op("pe", lambda e: e.matmul(py[:], lhsT=wdn[:, fc, dc * 128:(dc + 1) * 128], rhs=at[:, fc, :],
                                                    start=(fc == 0), stop=(fc == 7)), reads=[b_wdn, bat], writes=[bpy])
                    op("dve", lambda e: e.tensor_tensor(out=yacc[:, dc, tcs], in0=py[:], in1=yacc[:, dc, tcs], op=ALU.add),
                       reads=[bpy, b_yacc], writes=[b_yacc])
            if u + 1 < len(units):
                load_wdn(u + 1)
        for tt in range(TG // 128):
            r0 = g * TG + tt * 128
            bb = r0 // S
            dma("sp", lambda e: e.dma_start(out=xmt[:], in_=xmid_d[r0:r0 + 128, :]), writes=[b_xmt])
            for dc in range(8):
                pbk = PB[dc // 4]; bpbk = b_pb[dc // 4]
                op("pe", lambda e: e.transpose(out=pbk[:, (dc % 4) * 128:(dc % 4 + 1) * 128], in_=yacc[:, dc, tt * 128:(tt + 1) * 128],
                                               identity=ident_f[:]), reads=[b_yacc, b_id], writes=[bpbk])
            for cg in range(2):
                op("act", lambda e: e.activation(out=tyF[:, cg * 512:(cg + 1) * 512], in_=PB[cg][:], func=AF.Copy),
                   reads=[b_pb[cg]], writes=[b_tyF])
            r = rstd_col("act", tyF[:], ssF, D, EPS, [b_tyF], b_ssF, ot[:], b_ot)
            op("dve", lambda e: e.scalar_tensor_tensor(out=tyF[:], in0=tyF[:], scalar=r, in1=GFt[:, bb, :],
                                                       op0=ALU.mult, op1=ALU.mult), reads=[b_tyF, b_ssF, b_gft2], writes=[b_tyF])
            op("pool", lambda e: e.tensor_tensor(out=ot[:], in0=tyF[:], in1=xmt[:], op=ALU.add),
               reads=[b_tyF, b_xmt], writes=[b_ot])
            dma("sp", lambda e: e.dma_start(out=out_d[r0:r0 + 128, :], in_=ot[:]), reads=[b_ot], writes=[b_out])
    fence(c, [b_out])
    M.close()
    c.wait_all("sp", [b_out])
    print("instructions", c.n_ins, "waits", c.n_wait, "dsems", c.n_dsem)
    return nc


def fence(c, bufs):
    deps = []
    for b in bufs:
        deps.append(b.last_w)
        deps.extend(b.readers)
    for e in c.ENGS:
        c._need(e, deps)


_NC_CACHE = {}


def kernel(**inputs):
    stage = inputs.pop("_stage", "full")
    inp = {k: np.asarray(v) for k, v in inputs.items()}
    maps = host_prep(inp)
    if stage not in _NC_CACHE:
        _NC_CACHE[stage] = build(stage)
    nc = _NC_CACHE[stage]
    ncr = NCORES if stage == "full" else 1
    maps = [{k: m[k] for k in nc._in_names} for m in maps[:ncr]]
    import os
    if os.environ.get("KTRACE"):
        res = run_bass_kernel_spmd(nc, maps, core_ids=list(range(ncr)), trace=True)
        print("KTRACE exec_time_ns", res.exec_time_ns)
    else:
        res = run_bass_kernel_spmd(nc, maps, core_ids=list(range(ncr)))
    out = np.concatenate([r["out"] for r in res.results], axis=0)
    if ncr < NCORES:
        out = np.concatenate([out, np.zeros(((NCORES - ncr) * NT, D), np.float32)], 0)
    return out.reshape(16, S, D).astype(np.float32)
```

```python
import math
import numpy as np
import concourse.bass as bass
import concourse.mybir as mybir
from concourse.bass_utils import run_bass_kernel_spmd

F32 = mybir.dt.float32
BF16 = mybir.dt.bfloat16
I32 = mybir.dt.int32
ALU = mybir.AluOpType
AF = mybir.ActivationFunctionType
AX = mybir.AxisListType

NCORES = 8
D = 1024
S = 2048
NB = 2
NT = NB * S
NE = 32
PI = math.pi
CW1 = 6.28125
CW2 = 2 * math.pi - 6.28125
PI_LO = 3.1415925


class Buf:
    __slots__ = ("name", "last_w", "readers", "dsem", "dcnt")

    def __init__(self, name):
        self.name = name
        self.last_w = None
        self.readers = []
        self.dsem = None
        self.dcnt = 0


class Ctx:
    ENGS = ("pe", "act", "dve", "pool", "sp")

    def __init__(self, nc):
        self.nc = nc
        self.eng = {"pe": nc.tensor, "act": nc.scalar, "dve": nc.vector,
                    "pool": nc.gpsimd, "sp": nc.sync}
        self.sems = {}
        self.cnt = {}
        for e in self.ENGS:
            self.sems[e] = nc.alloc_semaphore("s_" + e)
            self.cnt[e] = 0
        self.waited = {}
        self.n_dsem = 0
        self.n_wait = 0
        self.n_ins = 0

    def sb(self, name, shape, dt):
        return self.nc.alloc_sbuf_tensor("sb_" + name, list(shape), dt)

    def ps(self, name, shape, dt=F32):
        return self.nc.alloc_psum_tensor("ps_" + name, list(shape), dt)

    def _need(self, eng, deps):
        best = {}
        for d in deps:
            if d is None:
                continue
            k, v = d
            if k == "pe" and eng == "pe":
                continue
            if best.get(k, 0) < v:
                best[k] = v
        for k, v in best.items():
            if self.waited.get((eng, k), 0) >= v:
                continue
            self.eng[eng].wait_ge(self.sems[k], v)
            self.waited[(eng, k)] = v
            self.n_wait += 1

    @staticmethod
    def _deps(reads, writes):
        deps = []
        for b in reads:
            deps.append(b.last_w)
        for b in writes:
            deps.append(b.last_w)
            deps.extend(b.readers)
        return deps

    def op(self, eng, fn, reads=(), writes=()):
        self._need(eng, self._deps(reads, writes))
        ins = fn(self.eng[eng])
        self.cnt[eng] += 1
        ins.then_inc(self.sems[eng], 1)
        self.n_ins += 1
        tok = (eng, self.cnt[eng])
        for b in reads:
            b.readers.append(tok)
            if len(b.readers) > 10:
                best = {}
                for k, v in b.readers:
                    if best.get(k, 0) < v:
                        best[k] = v
                b.readers = list(best.items())
        for b in writes:
            b.last_w = tok
            b.readers = []
        return ins

    def _dsem(self, b):
        if b.dsem is None:
            b.dsem = "d%d_%s" % (self.n_dsem, b.name)
            self.n_dsem += 1
            self.sems[b.dsem] = self.nc.alloc_semaphore(b.dsem)
        return b.dsem

    def dma(self, eng, fn, reads=(), writes=(), owner=None):
        self._need(eng, self._deps(reads, writes))
        own = owner if owner is not None else writes[0]
        k = self._dsem(own)
        res = fn(self.eng[eng])
        if not isinstance(res, (list, tuple)):
            res = [res]
        for ins in res:
            ins.then_inc(self.sems[k], 16)
            own.dcnt += 16
            self.n_ins += 1
        tok = (k, own.dcnt)
        for b in reads:
            b.readers.append(tok)
        for b in writes:
            b.last_w = tok
            b.readers = []
        return tok

    def wait_all(self, eng, bufs):
        self._need(eng, [b.last_w for b in bufs])


def _col(v, p=128):
    return np.ascontiguousarray(v.reshape(-1, p).T)


def _rope_perm(n_half):
    i = np.arange(2 * n_half)
    return (i + n_half) % (2 * n_half)


def host_prep(inp):
    f = np.float32
    x = inp["x"]; c = inp["c"]; pos = inp["positions"]
    w_ada = np.ascontiguousarray(inp["w_ada"][0]); b_ada = inp["b_ada"][0]
    w_in = inp["w_in"][0]
    cq = w_in[:, 0:256]; ckv = w_in[:, 256:384]; kr = w_in[:, 384:416]
    qd = w_in[:, 416:928]; kd = w_in[:, 928:1440]; vd = w_in[:, 1440:1952]
    pm = _rope_perm(16)
    z64 = np.zeros((D, 64), f); z32 = np.zeros((D, 32), f)
    groups = [cq[:, 0:128], cq[:, 128:256], ckv,
              np.concatenate([z64, kr, z32], 1), np.concatenate([z64, kr[:, pm], z32], 1)]
    pd = _rope_perm(32)
    def permd(w):
        w4 = w.reshape(D, 4, 2, 64)
        return np.ascontiguousarray(w4[:, :, :, pd]).reshape(D, 512)
    qdp = permd(qd); kdp = permd(kd)
    for h in range(4):
        groups.append(qd[:, h * 128:(h + 1) * 128])
    for h in range(4):
        groups.append(qdp[:, h * 128:(h + 1) * 128])
    for h in range(4):
        groups.append(kd[:, h * 128:(h + 1) * 128])
    for h in range(4):
        groups.append(kdp[:, h * 128:(h + 1) * 128])
    w_in_g = np.ascontiguousarray(np.stack(groups, 0))
    wq = inp["w_q_b"][0].reshape(256, 8, 96)
    wqA = np.ascontiguousarray(wq)
    wqB = np.zeros_like(wq)
    wqB[:, :, 64:96] = wq[:, :, 64:96][:, :, pm]
    wkv = inp["w_kv_b"][0].reshape(128, 8, 128)
    wkvk = np.ascontiguousarray(wkv[:, :, 0:64])
    wkvv = np.ascontiguousarray(wkv[:, :, 64:128]).reshape(128, 512)
    invf_d = (10000.0 ** (-np.arange(0, 64, 2, dtype=f) / f(64))).astype(f)
    invf_m = (10000.0 ** (-np.arange(0, 32, 2, dtype=f) / f(32))).astype(f)
    cst = np.zeros((128, 8), f)
    p = np.arange(128)
    cst[:, 0] = invf_d[p % 32]
    cst[:, 1] = np.where((p % 64) < 32, -1.0, 1.0)
    cst[64:96, 2] = invf_m[(p[64:96] - 64) % 16]
    cst[:, 3] = 1.0
    cst[64:80, 3] = -1.0
    cst[:, 4] = -PI * cst[:, 1]
    cst[:, 5] = -PI * cst[:, 3]
    cst[:, 6] = -PI
    ident = np.eye(128, dtype=f)
    maskT = (p[None, :] >= p[:, None]).astype(f)
    lam = np.stack([inp["lambda_q1"][0], inp["lambda_k1"][0], inp["lambda_q2"][0], inp["lambda_k2"][0]], 0)
    lam_rep = np.ascontiguousarray(np.broadcast_to(lam[None], (128, 4, 64))).astype(f)
    shared = dict(
        w_ada=w_ada, bada_col=_col(b_ada),
        bada_rep=np.ascontiguousarray(np.broadcast_to(
            np.concatenate([b_ada[2048:3072], b_ada[5120:6144]])[None], (128, 2048))).astype(f),
        gpre_col=_col(inp["g_mix_pre"][0]), gfpre_col=_col(inp["g_ffn_pre"][0]),
        gpost_rep=np.ascontiguousarray(np.broadcast_to(np.concatenate(
            [inp["g_mix_post"][0], inp["g_ffn_post"][0]])[None], (128, 2048))).astype(f),
        w_in_g=w_in_g, w_vd=np.ascontiguousarray(vd),
        wqA=wqA, wqB=wqB, gqa_col=_col(inp["g_q_a"][0]),
        wkvk=wkvk, wkvv=wkvv, gkva_col=_col(inp["g_kv_a"][0]),
        lam_rep=lam_rep, gsub_col=_col(inp["g_subln"][0], 64),
        w_o=np.ascontiguousarray(inp["w_o"][0]),
        w_r=np.ascontiguousarray(inp["w_router"][0]),
        br_rep=np.ascontiguousarray(np.broadcast_to(inp["b_router"][0][None], (128, NE))).astype(f),
        w_gu=np.ascontiguousarray(inp["w_gate_up"][0]),
        bgu_col=np.ascontiguousarray(inp["b_gate_up"][0].reshape(NE, 16, 128).transpose(2, 0, 1)),
        w_dn=np.ascontiguousarray(inp["w_down"][0]),
        b_dn=np.ascontiguousarray(inp["b_down"][0]),
        cst=cst, ident=ident, maskT=maskT,
    )
    maps = []
    for i in range(NCORES):
        bs = slice(i * NB, (i + 1) * NB)
        m = dict(shared)
        m["x"] = np.ascontiguousarray(x[bs].reshape(NT, D))
        cc = c[bs]
        m["ccol"] = np.ascontiguousarray(cc.reshape(NB, 8, 128).transpose(2, 1, 0))
        m["crep"] = np.ascontiguousarray(np.broadcast_to(
            m["ccol"][:, :, :, None], (128, 8, NB, 128))).astype(f)
        m["posr"] = np.ascontiguousarray(np.broadcast_to(pos[bs][:, None, :], (NB, 128, S))).astype(np.int32)
        maps.append(m)
    return maps


def build(stage="full"):
    nc = bass.Bass("TRN2", target_bir_lowering=False)
    c = Ctx(nc)

    names = []

    def din(name, shape, dt=F32):
        names.append(name)
        return nc.dram_tensor(name, list(shape), dt, kind="ExternalInput").ap()

    x_d = din("x", [NT, D]); ccol_d = din("ccol", [128, 8, NB]); crep_d = din("crep", [128, 8, NB, 128])
    posr_d = din("posr", [NB, 128, S], I32)
    wada_d = din("w_ada", [D, 6 * D]); badac_d = din("bada_col", [128, 48]); badar_d = din("bada_rep", [128, 2048])
    gpre_d = din("gpre_col", [128, 8]); gfpre_d = din("gfpre_col", [128, 8]); gpost_d = din("gpost_rep", [128, 2048])
    wing_d = din("w_in_g", [21, D, 128]); wvd_d = din("w_vd", [D, 512])
    wqA_d = din("wqA", [256, 8, 96]); wqB_d = din("wqB", [256, 8, 96]); gqa_d = din("gqa_col", [128, 2])
    wkvk_d = din("wkvk", [128, 8, 64]); wkvv_d = din("wkvv", [128, 512]); gkva_d = din("gkva_col", [128, 1])
    lam_d = din("lam_rep", [128, 4, 64]); gsub_d = din("gsub_col", [64, 2])
    wo_d = din("w_o", [D, D]); wr_d = din("w_r", [D, NE]); brr_d = din("br_rep", [128, NE])
    if stage == "full":
        wgu_d = din("w_gu", [NE, D, 2 * D]); bguc_d = din("bgu_col", [128, NE, 16])
        wdn_d = din("w_dn", [NE, D, D]); bdn_d = din("b_dn", [NE, D])
    nc._in_names = names
    cst_d = din("cst", [128, 8]); ident_d = din("ident", [128, 128]); maskT_d = din("maskT", [128, 128])
    out_d = nc.dram_tensor("out", [NT, D], F32, kind="ExternalOutput").ap()
    xmid_d = nc.dram_tensor("xmid", [NT, D], F32, kind="Internal").ap()
    h2T_d = nc.dram_tensor("h2T", [8, 128, NT], BF16, kind="Internal").ap()
    GT_d = nc.dram_tensor("GTd", [NE, NT], F32, kind="Internal").ap()
    gf_d = nc.dram_tensor("gfd", [128, NB, D], F32, kind="Internal").ap()
    b_gfd = Buf("gfd")

    op = c.op; dma = c.dma

    cst = c.sb("cst", [128, 8], F32); b_cst = Buf("cst")
    ident_f = c.sb("ident_f", [128, 128], F32); ident_b = c.sb("ident_b", [128, 128], BF16); b_id = Buf("ident")
    maskT = c.sb("maskT", [128, 128], BF16); b_mask = Buf("maskT")
    ones_b = c.sb("ones_b", [128, 128], BF16); ones_f = c.sb("ones_f", [128, 128], F32); b_ones = Buf("ones")
    dma("sp", lambda e: e.dma_start(out=cst[:], in_=cst_d), writes=[b_cst])
    dma("sp", lambda e: e.dma_start(out=ident_f[:], in_=ident_d), writes=[b_id])
    b_idb = Buf("identb")
    dma("pool", lambda e: [e.dma_start(out=ident_b[:], in_=ident_d)], writes=[b_idb])
    dma("pool", lambda e: e.dma_start(out=maskT[:], in_=maskT_d), writes=[b_mask])
    op("dve", lambda e: e.memset(ones_b[:], 1.0), writes=[b_ones])
    op("dve", lambda e: e.memset(ones_f[:], 1.0), writes=[b_ones])

    from contextlib import ExitStack
    uid = [0]

    class Scope:
        def __init__(self):
            self.es = ExitStack(); self.bufs = []
        def T(self, name, shape, dt):
            uid[0] += 1
            return self.es.enter_context(nc.sbuf_tensor("t_%s_%d" % (name, uid[0]), list(shape), dt))
        def B(self, name):
            b = Buf(name); self.bufs.append(b); return b
        def close(self):
            fence(c, self.bufs)
            self.es.close()

    def bf(ap):
        return ap.bitcast(BF16)

    MS = c.sb("MS", [128, NB, 8], F32); MH = c.sb("MH", [128, NB, 8], F32)
    FS = c.sb("FS", [128, NB, 8], F32); FH = c.sb("FH", [128, NB, 8], F32)
    b_mod = Buf("mod")
    lamc = c.sb("lamc", [128, 4], F32); b_lam = Buf("lam")
    gqa = c.sb("gqa", [128, 2], F32); gkva = c.sb("gkva", [128, 1], F32); gsub = c.sb("gsub", [64, 2], F32)
    b_g = Buf("gains")
    dma("sp", lambda e: [e.dma_start(out=gqa[:], in_=gqa_d), e.dma_start(out=gkva[:], in_=gkva_d),
                         e.dma_start(out=gsub[:], in_=gsub_d)], writes=[b_g])

    P0 = Scope()
    GM = P0.T("GM", [128, NB, D], F32)
    PB = [c.ps("pb%d" % i, [128, 512], F32) for i in range(8)]
    b_pb = [Buf("pb%d" % i) for i in range(8)]

    with nc.sbuf_tensor("t_ccol", [128, 8, NB], F32) as ccol, \
         nc.sbuf_tensor("t_crep", [128, 8, NB, 128], F32) as crep, \
         nc.sbuf_tensor("t_wa0", [128, 8, 512], F32) as wa0, \
         nc.sbuf_tensor("t_wa1", [128, 8, 512], F32) as wa1, \
         nc.sbuf_tensor("t_badac", [128, 48], F32) as badac, \
         nc.sbuf_tensor("t_badar", [128, 2048], F32) as badar, \
         nc.sbuf_tensor("t_gpre", [128, 8], F32) as gpre, \
         nc.sbuf_tensor("t_gfpre", [128, 8], F32) as gfpre, \
         nc.sbuf_tensor("t_gpost", [128, 2048], F32) as gpost, \
         nc.sbuf_tensor("t_modc", [128, 48, NB], F32) as modc, \
         nc.sbuf_tensor("t_lamt", [128, 4, 64], F32) as lamt, \
         nc.sbuf_tensor("t_lamj", [128, 64], F32) as lamj, \
         nc.sbuf_tensor("t_GF", [128, NB, D], F32) as GF:
        b_cc = Buf("cc"); b_wa = [Buf("wa0"), Buf("wa1")]; b_misc = Buf("misc0"); b_modc = Buf("modc")
        wa = [wa0, wa1]
        dma("sp", lambda e: [e.dma_start(out=ccol[:], in_=ccol_d), e.dma_start(out=crep[:], in_=crep_d)],
            writes=[b_cc])
        dma("sp", lambda e: [e.dma_start(out=badac[:], in_=badac_d), e.dma_start(out=badar[:], in_=badar_d),
                             e.dma_start(out=gpre[:], in_=gpre_d), e.dma_start(out=gfpre[:], in_=gfpre_d),
                             e.dma_start(out=gpost[:], in_=gpost_d), e.dma_start(out=lamt[:], in_=lam_d)],
            writes=[b_misc])
        op("act", lambda e: e.activation(out=ccol[:], in_=ccol[:], func=AF.Silu), reads=[b_cc], writes=[b_cc])
        op("act", lambda e: e.activation(out=crep[:], in_=crep[:], func=AF.Silu), reads=[b_cc], writes=[b_cc])
        op("dve", lambda e: e.memset(lamc[:], 0.0), writes=[b_lam])
        for j in range(2):
            op("dve", lambda e: e.scalar_tensor_tensor(out=lamj[:], in0=lamt[:, 2 * j, :], scalar=1.0,
                                                       in1=lamt[:, 2 * j + 1, :], op0=ALU.mult, op1=ALU.mult,
                                                       accum_out=lamc[:, j:j + 1]),
               reads=[b_misc], writes=[b_lam, b_misc])
        op("act", lambda e: e.activation(out=lamc[:, 0:2], in_=lamc[:, 0:2], func=AF.Exp), reads=[b_lam], writes=[b_lam])
        op("dve", lambda e: e.tensor_tensor(out=lamc[:, 2:3], in0=lamc[:, 0:1], in1=lamc[:, 1:2], op=ALU.subtract),
           reads=[b_lam], writes=[b_lam])
        op("dve", lambda e: e.tensor_scalar(out=lamc[:, 3:4], in0=lamc[:, 2:3], scalar1=0.2, scalar2=-1.0,
                                            op0=ALU.add, op1=ALU.mult), reads=[b_lam], writes=[b_lam])
        for sl in range(12):
            w = wa[sl % 2]; bw = b_wa[sl % 2]
            dma("sp", lambda e: e.dma_start(out=w[:], in_=wada_d[:, sl * 512:(sl + 1) * 512]
                                            .rearrange("(k p) n -> p k n", p=128)), writes=[bw])
            if sl in (4, 5, 10, 11):
                gi = 0 if sl < 6 else 1
                half = sl % 2 if sl < 6 else (sl - 10)
                dst = GM if gi == 0 else GF
                for b in range(NB):
                    pbk = PB[b]; bpb = b_pb[b]
                    for kc in range(8):
                        op("pe", lambda e: e.matmul(pbk[:], lhsT=crep[:, kc, b, :], rhs=w[:, kc, :],
                                                    start=(kc == 0), stop=(kc == 7)),
                           reads=[b_cc, bw], writes=[bpb])
                    cs = slice(half * 512, (half + 1) * 512)
                    rs = slice(gi * 1024 + half * 512, gi * 1024 + (half + 1) * 512)
                    op("dve", lambda e: e.tensor_tensor(out=dst[:, b, cs], in0=pbk[:], in1=badar[:, rs], op=ALU.add),
                       reads=[bpb, b_misc], writes=[b_mod])
                    op("dve", lambda e: e.tensor_tensor(out=dst[:, b, cs], in0=dst[:, b, cs], in1=gpost[:, rs],
                                                        op=ALU.mult), reads=[b_mod, b_misc], writes=[b_mod])
            else:
                pbk = PB[2 + sl % 2]; bpb = b_pb[2 + sl % 2]
                for jj in range(4):
                    for kc in range(8):
                        op("pe", lambda e: e.matmul(pbk[:, jj * NB:(jj + 1) * NB], lhsT=w[:, kc, jj * 128:(jj + 1) * 128],
                                                    rhs=ccol[:, kc, :], start=(kc == 0), stop=(kc == 7)),
                           reads=[b_cc, bw], writes=[bpb])
                op("dve", lambda e: e.tensor_copy(out=modc[:, sl * 4:(sl + 1) * 4, :],
                                                  in_=pbk[:, 0:4 * NB].rearrange("p (j b) -> p j b", b=NB)),
                   reads=[bpb], writes=[b_modc])
        for b in range(NB):
            op("dve", lambda e: e.tensor_tensor(out=MH[:, b, :], in0=modc[:, 0:8, b], in1=badac[:, 0:8], op=ALU.add),
               reads=[b_modc, b_misc], writes=[b_mod])
            op("dve", lambda e: e.tensor_tensor(out=FH[:, b, :], in0=modc[:, 24:32, b], in1=badac[:, 24:32], op=ALU.add),
               reads=[b_modc, b_misc], writes=[b_mod])
            for (dst, j0, gg) in ((MS, 8, gpre), (FS, 32, gfpre)):
                op("dve", lambda e: e.scalar_tensor_tensor(out=dst[:, b, :], in0=modc[:, j0:j0 + 8, b], scalar=1.0,
                                                           in1=badac[:, j0:j0 + 8], op0=ALU.add, op1=ALU.add),
                   reads=[b_modc, b_misc], writes=[b_mod])
                op("dve", lambda e: e.tensor_tensor(out=dst[:, b, :], in0=dst[:, b, :], in1=gg[:], op=ALU.mult),
                   reads=[b_mod, b_misc], writes=[b_mod])
        b_gft = Buf("gft")
        op("dve", lambda e: e.tensor_copy(out=GF[:, :, 0:1], in_=GF[:, :, 0:1]), reads=[b_mod], writes=[b_gft])
        dma("sp", lambda e: e.dma_start(out=gf_d, in_=GF[:]), reads=[b_gft, b_mod], writes=[b_gfd])
        fence(c, [b_cc, b_misc, b_modc, b_gft, b_gfd] + b_wa)


    wqA = P0.T("wqA", [128, 2, 8, 96], BF16); wqB = P0.T("wqB", [128, 2, 8, 96], BF16)
    wkvk = P0.T("wkvk", [128, 8, 64], BF16); wkvv = P0.T("wkvv", [128, 512], BF16)
    wr = P0.T("wr", [128, 8, NE], F32); brr = P0.T("brr", [128, NE], F32)
    b_w = P0.B("attw")
    dma("pool", lambda e: [e.dma_start(out=wqA[:], in_=wqA_d.rearrange("(k p) h n -> p k h n", p=128)),
                           e.dma_start(out=wqB[:], in_=wqB_d.rearrange("(k p) h n -> p k h n", p=128)),
                           e.dma_start(out=wkvk[:], in_=wkvk_d), e.dma_start(out=wkvv[:], in_=wkvv_d)],
        writes=[b_w])
    b_wr = P0.B("wr")
    dma("sp", lambda e: [e.dma_start(out=wr[:], in_=wr_d.rearrange("(k p) n -> p k n", p=128)),
                         e.dma_start(out=brr[:], in_=brr_d)], writes=[b_wr])
    EPS = 1e-6
    SC_M = 96.0 ** -0.5
    SC_D = 64.0 ** -0.5

    def rstd_col(eng_sq, src, ss, n, eps, reads, bss, junk, bjunk):
        op("dve", lambda e: e.memset(ss[:, 0:1], 0.0), writes=[bss])
        op("act", lambda e: e.activation(out=junk, in_=src, func=AF.Square, accum_out=ss[:, 0:1]),
           reads=reads + [bss], writes=[bss, bjunk])
        op("dve", lambda e: e.tensor_scalar(out=ss[:, 1:2], in0=ss[:, 0:1], scalar1=1.0 / n, scalar2=eps,
                                            op0=ALU.mult, op1=ALU.add), reads=[bss], writes=[bss])
        op("dve", lambda e: e.reciprocal(out=ss[:, 2:3], in_=ss[:, 1:2]), reads=[bss], writes=[bss])
        op("act", lambda e: e.activation(out=ss[:, 3:4], in_=ss[:, 2:3], func=AF.Sqrt), reads=[bss], writes=[bss])
        return ss[:, 3:4]

    def rstd_bc(pbank, bpbank, n, eps, dst, bdst, rows=128):
        op("dve", lambda e: e.tensor_scalar(out=dst[0:rows, :], in0=pbank[0:rows, :], scalar1=1.0 / n, scalar2=eps,
                                            op0=ALU.mult, op1=ALU.add), reads=[bpbank], writes=[bdst])
        op("dve", lambda e: e.reciprocal(out=dst[0:rows, :], in_=dst[0:rows, :]), reads=[bdst], writes=[bdst])
        op("act", lambda e: e.activation(out=dst[0:rows, :], in_=dst[0:rows, :], func=AF.Sqrt), reads=[bdst], writes=[bdst])

    for b in range(NB):
        tok0 = b * S
        AB = Scope()
        cqnT = AB.T("cqnT", [128, 2, S], BF16); b_cqn = AB.B("cqn")
        ckvnT = AB.T("ckvnT", [128, S], BF16); b_ckvn = AB.B("ckvn")
        krT = AB.T("krT", [96, S], BF16); b_kr = AB.B("kr")
        vm = AB.T("vm", [128, 16, 8, 65], BF16); b_vm = AB.B("vm")
        qdT = AB.T("qdT", [128, 4, S], BF16); b_qd = AB.B("qd")
        kdT = AB.T("kdT", [128, 4, S], BF16); b_kd = AB.B("kd")
        vd = AB.T("vd", [128, 16, 4, 2, 65], BF16); b_vd = AB.B("vd")
        Cm = AB.T("Cm", [96, S], BF16); Sm = AB.T("Sm", [96, S], BF16); b_tm = AB.B("tabm")
        op("pool", lambda e: e.memset(vm[:], 1.0), writes=[b_vm])
        op("pool", lambda e: e.memset(vd[:], 1.0), writes=[b_vd])
        op("pool", lambda e: e.memset(krT[:], 0.0), writes=[b_kr])

        A = Scope()
        hT = A.T("hT", [128, 8, S], BF16); b_hT = A.B("hT")
        wing_m = A.T("wing_m", [128, 8, 640], BF16); b_wm = A.B("wing_m")
        wing_p = [A.T("wing_p%d" % i, [128, 8, 256], BF16) for i in range(2)]; b_wp = [A.B("wp0"), A.B("wp1")]
        wvd = A.T("wvd", [128, 8, 512], BF16); b_wvd = A.B("wvd")
        Cd = A.T("Cd", [128, S], BF16); Sd = A.T("Sd", [128, S], BF16); b_td = A.B("tabd")
        posi = A.T("posi", [128, 512], I32); posf = A.T("posf", [128, 512], F32); angt = A.T("angt", [128, 512], F32)
        angi = A.T("angi", [128, 512], I32); angk = A.T("angk", [128, 512], F32)
        b_pos = A.B("pos"); b_ang = A.B("ang"); b_angi = A.B("angi"); b_angk = A.B("angk")
        xt = [A.T("xt%d" % i, [128, D], F32) for i in range(2)]; b_xt = [A.B("xt0"), A.B("xt1")]
        xn = [A.T("xn%d" % i, [128, D], BF16) for i in range(2)]; b_xn = [A.B("xn0"), A.B("xn1")]
        ssA = A.T("ssA", [128, 8], F32); b_ssA = A.B("ssA")
        tb = [A.T("tb%d" % i, [128, 512], BF16) for i in range(2)]; b_tb = [A.B("tb0"), A.B("tb1")]
        tf = [A.T("tf%d" % i, [128, 512], F32) for i in range(3)]; b_tf = [A.B("tf%d" % i) for i in range(3)]

        for g in range(5):
            dma("pool", lambda e: e.dma_start(out=wing_m[:, :, g * 128:(g + 1) * 128],
                                              in_=wing_d[g].rearrange("(k p) n -> p k n", p=128)), writes=[b_wm])
        dma("pool", lambda e: e.dma_start(out=wvd[:], in_=wvd_d.rearrange("(k p) n -> p k n", p=128)), writes=[b_wvd])
        for ch in range(4):
            cs = slice(ch * 512, (ch + 1) * 512)
            dma("sp", lambda e: e.dma_start(out=posi[:], in_=posr_d[b][:, cs]), writes=[b_pos])
            op("dve", lambda e: e.tensor_copy(out=posf[:], in_=posi[:]), reads=[b_pos], writes=[b_pos])
            for (Ct, St, rows, ci, si, bt) in ((Cd, Sd, 128, 0, 1, b_td), (Cm, Sm, 96, 2, 3, b_tm)):
                for (dstT, shift, use_sgn) in ((Ct, 0.5 * PI, False), (St, 0.0, True)):
                    R = slice(0, rows)
                    op("dve", lambda e: e.tensor_scalar(out=angt[R, :], in0=posf[R, :], scalar1=cst[R, ci:ci + 1],
                                                        scalar2=shift, op0=ALU.mult, op1=ALU.add),
                       reads=[b_pos, b_cst], writes=[b_ang])
                    op("dve", lambda e: e.tensor_scalar(out=angi[R, :], in0=angt[R, :], scalar1=1.0 / (2 * PI), scalar2=0.0,
                                                        op0=ALU.mult, op1=ALU.add), reads=[b_ang], writes=[b_angi])
                    op("dve", lambda e: e.tensor_copy(out=angk[R, :], in_=angi[R, :]), reads=[b_angi], writes=[b_angk])
                    op("dve", lambda e: e.scalar_tensor_tensor(out=angt[R, :], in0=angk[R, :], scalar=-CW1, in1=angt[R, :],
                                                               op0=ALU.mult, op1=ALU.add), reads=[b_angk, b_ang], writes=[b_ang])
                    op("dve", lambda e: e.scalar_tensor_tensor(out=angt[R, :], in0=angk[R, :], scalar=-CW2, in1=angt[R, :],
                                                               op0=ALU.mult, op1=ALU.add), reads=[b_angk, b_ang], writes=[b_ang])
                    op("dve", lambda e: e.tensor_scalar(out=angk[R, :], in0=angt[R, :], scalar1=PI, scalar2=2 * PI,
                                                        op0=ALU.is_gt, op1=ALU.mult), reads=[b_ang], writes=[b_angk])
                    op("dve", lambda e: e.tensor_tensor(out=angt[R, :], in0=angt[R, :], in1=angk[R, :], op=ALU.subtract),
                       reads=[b_ang, b_angk], writes=[b_ang])
                    op("dve", lambda e: e.tensor_scalar(out=angt[R, :], in0=angt[R, :], scalar1=-PI_LO, scalar2=PI_LO,
                                                        op0=ALU.max, op1=ALU.min), reads=[b_ang], writes=[b_ang])
                    if use_sgn:
                        op("act", lambda e: e.activation(out=dstT[R, cs], in_=angt[R, :], func=AF.Sin, scale=cst[R, si:si + 1]),
                           reads=[b_ang, b_cst], writes=[bt])
                    else:
                        op("act", lambda e: e.activation(out=dstT[R, cs], in_=angt[R, :], func=AF.Sin),
                           reads=[b_ang, b_cst], writes=[bt])
        for t in range(16):
            xi = xt[t % 2]; bxi = b_xt[t % 2]; xni = xn[t % 2]; bxni = b_xn[t % 2]
            dma("sp", lambda e: e.dma_start(out=xi[:], in_=x_d[tok0 + t * 128: tok0 + (t + 1) * 128, :]), writes=[bxi])
            r = rstd_col("act", xi[:], ssA, D, EPS, [bxi], b_ssA, xni[:], bxni)
            op("act", lambda e: e.activation(out=xni[:], in_=xi[:], func=AF.Copy, scale=r), reads=[bxi, b_ssA], writes=[bxni])
            pT = bf(PB[6][:]).rearrange("p (k t) -> p k t", t=128)
            for kc in range(8):
                op("pe", lambda e: e.transpose(out=pT[:, kc, :], in_=xni[:, kc * 128:(kc + 1) * 128], identity=ident_b[:]),
                   reads=[bxni, b_idb], writes=[b_pb[6]])
            for kc in range(8):
                dst = hT[:, kc, t * 128:(t + 1) * 128]
                if kc % 2 == 0:
                    op("dve", lambda e: e.tensor_scalar(out=dst, in0=pT[:, kc, :], scalar1=MS[:, b, kc:kc + 1],
                                                        scalar2=MH[:, b, kc:kc + 1], op0=ALU.mult, op1=ALU.add),
                       reads=[b_pb[6], b_mod], writes=[b_hT])
                else:
                    op("act", lambda e: e.activation(out=dst, in_=pT[:, kc, :], func=AF.Identity,
                                                     bias=MH[:, b, kc:kc + 1], scale=MS[:, b, kc:kc + 1]),
                       reads=[b_pb[6], b_mod], writes=[b_hT])
        for ch in range(4):
            cs = slice(ch * 512, (ch + 1) * 512)
            for g in range(5):
                M = 128 if g < 3 else 96
                for kc in range(8):
                    op("pe", lambda e: e.matmul(PB[g][0:M, :], lhsT=wing_m[:, kc, g * 128:g * 128 + M], rhs=hT[:, kc, cs],
                                                start=(kc == 0), stop=(kc == 7)),
                       reads=[b_wm, b_hT], writes=[b_pb[g]])
            for i in range(2):
                op("act", lambda e: e.activation(out=tb[i][:], in_=PB[i][:], func=AF.Square), reads=[b_pb[i]], writes=[b_tb[i]])
            for i in range(2):
                op("pe", lambda e: e.matmul(PB[5][:], lhsT=ones_b[:], rhs=tb[i][:], start=(i == 0), stop=(i == 1)),
                   reads=[b_ones, b_tb[i]], writes=[b_pb[5]])
            rstd_bc(PB[5], b_pb[5], 256.0, EPS, tf[0], b_tf[0])
            for i in range(2):
                op("dve", lambda e: e.scalar_tensor_tensor(out=cqnT[:, i, cs], in0=PB[i][:], scalar=gqa[:, i:i + 1],
                                                           in1=tf[0][:], op0=ALU.mult, op1=ALU.mult),
                   reads=[b_pb[i], b_g, b_tf[0]], writes=[b_cqn])
            op("act", lambda e: e.activation(out=tb[0][:], in_=PB[2][:], func=AF.Square), reads=[b_pb[2]], writes=[b_tb[0]])
            op("pe", lambda e: e.matmul(PB[5][:], lhsT=ones_b[:], rhs=tb[0][:], start=True, stop=True),
               reads=[b_ones, b_tb[0]], writes=[b_pb[5]])
            rstd_bc(PB[5], b_pb[5], 128.0, EPS, tf[1], b_tf[1])
            op("dve", lambda e: e.scalar_tensor_tensor(out=ckvnT[:, cs], in0=PB[2][:], scalar=gkva[:, 0:1],
                                                       in1=tf[1][:], op0=ALU.mult, op1=ALU.mult),
               reads=[b_pb[2], b_g, b_tf[1]], writes=[b_ckvn])
            op("dve", lambda e: e.tensor_tensor(out=tf[2][64:96, :], in0=PB[3][64:96, :], in1=Cm[64:96, cs], op=ALU.mult),
               reads=[b_pb[3], b_tm], writes=[b_tf[2]])
            op("dve", lambda e: e.tensor_tensor(out=tf[0][64:96, :], in0=PB[4][64:96, :], in1=Sm[64:96, cs], op=ALU.mult),
               reads=[b_pb[4], b_tm], writes=[b_tf[0]])
            op("pool", lambda e: e.tensor_tensor(out=krT[64:96, cs], in0=tf[2][64:96, :], in1=tf[0][64:96, :], op=ALU.add),
               reads=[b_tf[2], b_tf[0]], writes=[b_kr])
        for t in range(16):
            pb = PB[6 + t % 2]; bpb = b_pb[6 + t % 2]
            op("pe", lambda e: e.matmul(pb[:], lhsT=ckvnT[:, t * 128:(t + 1) * 128], rhs=wkvv[:], start=True, stop=True),
               reads=[b_ckvn, b_w], writes=[bpb])
            op("act" if t % 2 else "dve",
               (lambda e: e.activation(out=vm[:, t, :, 0:64], in_=pb[:].rearrange("p (h d) -> p h d", d=64), func=AF.Copy))
               if t % 2 else
               (lambda e: e.tensor_copy(out=vm[:, t, :, 0:64], in_=pb[:].rearrange("p (h d) -> p h d", d=64))),
               reads=[bpb], writes=[b_vm])
        pi = 0
        for kind in range(2):
            dstT = qdT if kind == 0 else kdT; bdst = b_qd if kind == 0 else b_kd
            for h in range(4):
                wp = wing_p[pi % 2]; bwp = b_wp[pi % 2]; pi += 1
                gA = 5 + 8 * kind + h; gB = gA + 4
                dma("pool", lambda e: [e.dma_start(out=wp[:, :, 0:128], in_=wing_d[gA].rearrange("(k p) n -> p k n", p=128)),
                                       e.dma_start(out=wp[:, :, 128:256], in_=wing_d[gB].rearrange("(k p) n -> p k n", p=128))],
                    writes=[bwp])
                for ch in range(4):
                    cs = slice(ch * 512, (ch + 1) * 512)
                    pa = PB[2 * (ch % 2)]; bpa = b_pb[2 * (ch % 2)]; pbb = PB[2 * (ch % 2) + 1]; bpbb = b_pb[2 * (ch % 2) + 1]
                    for kc in range(8):
                        op("pe", lambda e: e.matmul(pa[:], lhsT=wp[:, kc, 0:128], rhs=hT[:, kc, cs], start=(kc == 0), stop=(kc == 7)),
                           reads=[bwp, b_hT], writes=[bpa])
                    for kc in range(8):
                        op("pe", lambda e: e.matmul(pbb[:], lhsT=wp[:, kc, 128:256], rhs=hT[:, kc, cs], start=(kc == 0), stop=(kc == 7)),
                           reads=[bwp, b_hT], writes=[bpbb])
                    op("dve", lambda e: e.tensor_tensor(out=tf[0][:], in0=pa[:], in1=Cd[:, cs], op=ALU.mult),
                       reads=[bpa, b_td], writes=[b_tf[0]])
                    op("dve", lambda e: e.tensor_tensor(out=tf[1][:], in0=pbb[:], in1=Sd[:, cs], op=ALU.mult),
                       reads=[bpbb, b_td], writes=[b_tf[1]])
                    op("pool", lambda e: e.tensor_tensor(out=dstT[:, h, cs], in0=tf[0][:], in1=tf[1][:], op=ALU.add),
                       reads=[b_tf[0], b_tf[1]], writes=[bdst])
        for t in range(16):
            pb = PB[6 + t % 2]; bpb = b_pb[6 + t % 2]
            for kc in range(8):
                op("pe", lambda e: e.matmul(pb[:], lhsT=hT[:, kc, t * 128:(t + 1) * 128], rhs=wvd[:, kc, :],
                                            start=(kc == 0), stop=(kc == 7)), reads=[b_hT, b_wvd], writes=[bpb])
            src = pb[:].rearrange("p (h j d) -> p h j d", h=4, j=2)
            if t % 2:
                op("act", lambda e: e.activation(out=vd[:, t, :, :, 0:64], in_=src, func=AF.Copy), reads=[bpb], writes=[b_vd])
            else:
                op("dve", lambda e: e.tensor_copy(out=vd[:, t, :, :, 0:64], in_=src), reads=[bpb], writes=[b_vd])
        A.close()

        Bs = Scope()
        wo = Bs.T("wo", [64, 16, D], BF16); b_wo = Bs.B("wo")
        dma("pool", lambda e: e.dma_start(out=wo[:], in_=wo_d.rearrange("(i p) n -> p i n", p=64)), writes=[b_wo])
        mixT = Bs.T("mixT", [64, 16, 512], BF16); b_mix = Bs.B("mix")
        kTh = [Bs.T("kTh%d" % i, [96, S], BF16) for i in range(2)]; b_kTh = [Bs.B("kTh0"), Bs.B("kTh1")]
        pt = [Bs.T("pt%d" % i, [128, 512], BF16) for i in range(3)]; b_pt = [Bs.B("pt%d" % i) for i in range(3)]
        qh = [Bs.T("qh%d" % i, [96, 512], BF16) for i in range(2)]; b_qh = [Bs.B("qh0"), Bs.B("qh1")]
        tg = [Bs.T("tg%d" % i, [128, 512], F32) for i in range(5)]; b_tg = [Bs.B("tg%d" % i) for i in range(5)]
        rd = Bs.T("rd", [128, 512], F32); b_rd = Bs.B("rd")
        sqb = [Bs.T("sqb%d" % i, [64, 512], BF16) for i in range(2)]; b_sqb = [Bs.B("sqb0"), Bs.B("sqb1")]
        xr = Bs.T("xr", [128, D], F32); b_xr = Bs.B("xr")
        tys = [Bs.T("ty%d" % i, [128, D], F32) for i in range(2)]; b_tys = [Bs.B("ty0"), Bs.B("ty1")]
        xm = Bs.T("xm", [128, D], F32); b_xm = Bs.B("xm")
        ssB = Bs.T("ssB", [128, 8], F32); b_ssB = Bs.B("ssB")
        h2f = Bs.T("h2f", [128, 8, 128], F32); b_h2f = Bs.B("h2f")
        junkB = h2f[:].rearrange("p k t -> p (k t)"); b_junkB = b_h2f
        h2b = Bs.T("h2b", [128, 8, 128], BF16); b_h2b = Bs.B("h2b")
        rt = Bs.T("rt", [128, 6, NE], F32); b_rt = Bs.B("rt")
        gts = Bs.T("gts", [32, 128], F32); b_gts = Bs.B("gts")
        b_xmid = Bs.B("xmid_d"); b_h2d = Bs.B("h2T_d"); b_gtd = Bs.B("GT_d")
        pti = [0]; sbi = [0]

        def attn_map(kT_ap_fn, q_ap_fn, scale, pv_list, c_, kreads, qreads, vreads):
            nkt = 4 * c_ + 4

            def issue_S(kt):
                lo = max(0, kt * 128 - c_ * 512); n = 512 - lo
                sb_ = PB[sbi[0] % 2]; bsb = b_pb[sbi[0] % 2]; sbi[0] += 1
                op("pe", lambda e: e.matmul(sb_[:, 0:n], lhsT=kT_ap_fn(kt), rhs=q_ap_fn(lo, 512), start=True, stop=True),
                   reads=kreads + qreads, writes=[bsb])
                p_ = pt[pti[0] % 3]; bp_ = b_pt[pti[0] % 3]; pti[0] += 1
                op("act", lambda e: e.activation(out=p_[:, 0:n], in_=sb_[:, 0:n], func=AF.Exp, scale=scale),
                   reads=[bsb], writes=[bp_])
                if kt >= 4 * c_:
                    op("pool", lambda e: e.tensor_tensor(out=p_[:, 0:128], in0=p_[:, 0:128], in1=maskT[:], op=ALU.mult),
                       reads=[bp_, b_mask], writes=[bp_])
                return (p_, bp_, lo, n)

            pend = issue_S(0)
            for kt in range(nkt):
                nxt = issue_S(kt + 1) if kt + 1 < nkt else None
                p_, bp_, lo, n = pend
                for (acc, bacc, v_fn) in pv_list:
                    op("pe", lambda e: e.matmul(acc[:, lo:512], lhsT=v_fn(kt), rhs=p_[:, 0:n],
                                                start=(kt == 0), stop=(kt == nkt - 1)),
                       reads=[bp_] + vreads, writes=[bacc])
                pend = nxt

        def normalize(acc, bacc, dst, bdst, tmp, btmp, pbn=None, bpbn=None):
            pbn = PB[7] if pbn is None else pbn; bpbn = b_pb[7] if bpbn is None else bpbn
            op("dve", lambda e: e.reciprocal(out=rd[64:65, :], in_=acc[64:65, :]), reads=[bacc], writes=[b_rd])
            op("pe", lambda e: e.matmul(pbn[0:64, :], lhsT=ones_f[64:65, 0:64], rhs=rd[64:65, :], start=True, stop=True),
               reads=[b_rd, b_ones], writes=[bpbn])
            op("act", lambda e: e.activation(out=tmp[0:64, :], in_=pbn[0:64, :], func=AF.Copy), reads=[bpbn], writes=[btmp])
            op("dve", lambda e: e.tensor_tensor(out=dst, in0=acc[0:64, :], in1=tmp[0:64, :], op=ALU.mult),
               reads=[bacc, btmp], writes=[bdst])

        for c_ in range(4):
            q0 = c_ * 512
            qs = slice(q0, q0 + 512)
            nk = (c_ + 1) * 512
            def mla_prep(h):
                kt_ = kTh[h % 2]; bkt = b_kTh[h % 2]
                for k2 in range(c_ + 1):
                    pk = PB[4 + k2 % 2]; bpk = b_pb[4 + k2 % 2]
                    op("pe", lambda e: e.matmul(pk[0:64, :], lhsT=wkvk[:, h, :], rhs=ckvnT[:, k2 * 512:(k2 + 1) * 512],
                                                start=True, stop=True), reads=[b_w, b_ckvn], writes=[bpk])
                    op("act", lambda e: e.activation(out=kt_[0:64, k2 * 512:(k2 + 1) * 512], in_=pk[0:64, :], func=AF.Copy),
                       reads=[bpk], writes=[bkt])
                op("pool", lambda e: e.tensor_copy(out=kt_[64:96, 0:nk], in_=krT[64:96, 0:nk]), reads=[b_kr], writes=[bkt])
                for (wq_, pbi) in ((wqA, 6), (wqB, 7)):
                    for kc in range(2):
                        op("pe", lambda e: e.matmul(PB[pbi][0:96, :], lhsT=wq_[:, kc, h, :], rhs=cqnT[:, kc, qs],
                                                    start=(kc == 0), stop=(kc == 1)), reads=[b_w, b_cqn], writes=[b_pb[pbi]])
                q_ = qh[h % 2]; bq_ = b_qh[h % 2]
                op("dve", lambda e: e.tensor_tensor(out=tg[0][0:96, :], in0=PB[6][0:96, :], in1=Cm[:, qs], op=ALU.mult),
                   reads=[b_pb[6], b_tm], writes=[b_tg[0]])
                op("dve", lambda e: e.tensor_tensor(out=tg[1][0:96, :], in0=PB[7][0:96, :], in1=Sm[:, qs], op=ALU.mult),
                   reads=[b_pb[7], b_tm], writes=[b_tg[1]])
                op("dve", lambda e: e.tensor_tensor(out=q_[:], in0=tg[0][0:96, :], in1=tg[1][0:96, :], op=ALU.add),
                   reads=[b_tg[0], b_tg[1]], writes=[bq_])

            mla_prep(0)
            for h in range(8):
                kt_ = kTh[h % 2]; bkt = b_kTh[h % 2]
                q_ = qh[h % 2]; bq_ = b_qh[h % 2]
                if h + 1 < 8:
                    mla_prep(h + 1)
                acc = PB[2 + h % 2]; bacc = b_pb[2 + h % 2]
                attn_map(lambda kt: kt_[:, kt * 128:(kt + 1) * 128], lambda lo, hi: q_[:, lo:hi], SC_M,
                         [(acc[0:65, :], bacc, lambda kt: vm[:, kt, h, :])], c_, [bkt], [bq_], [b_vm])
                normalize(acc, bacc, mixT[:, h, :], b_mix, tg[2], b_tg[2], pbn=PB[4 + h % 2], bpbn=b_pb[4 + h % 2])
            for h in range(4):
                for j in range(2):
                    js = slice(j * 64, (j + 1) * 64)
                    accs = [(PB[2 + 2 * j + hf][0:65, :], b_pb[2 + 2 * j + hf], (lambda kt, hf=hf: vd[:, kt, h, hf, :]))
                            for hf in range(2)]
                    attn_map(lambda kt: kdT[js, h, kt * 128:(kt + 1) * 128], lambda lo, hi: qdT[js, h, q0 + lo:q0 + hi], SC_D,
                             accs, c_, [b_kd], [b_qd], [b_vd])
                for j in range(2):
                    for hf in range(2):
                        normalize(PB[2 + 2 * j + hf], b_pb[2 + 2 * j + hf], tg[2 * j + hf][0:64, :], b_tg[2 * j + hf],
                                  tg[4], b_tg[4])
                for hf in range(2):
                    op("dve", lambda e: e.scalar_tensor_tensor(out=tg[hf][0:64, :], in0=tg[2 + hf][0:64, :], scalar=lamc[0:64, 3:4],
                                                               in1=tg[hf][0:64, :], op0=ALU.mult, op1=ALU.add),
                       reads=[b_tg[2 + hf], b_tg[hf], b_lam], writes=[b_tg[hf]])
                    op("act", lambda e: e.activation(out=sqb[hf][:], in_=tg[hf][0:64, :], func=AF.Square),
                       reads=[b_tg[hf]], writes=[b_sqb[hf]])
                for hf in range(2):
                    op("pe", lambda e: e.matmul(PB[7][0:64, :], lhsT=ones_b[0:64, 0:64], rhs=sqb[hf][:], start=(hf == 0), stop=(hf == 1)),
                       reads=[b_ones, b_sqb[hf]], writes=[b_pb[7]])
                rstd_bc(PB[7], b_pb[7], 128.0, 1e-5, tg[4], b_tg[4], rows=64)
                for hf in range(2):
                    op("dve", lambda e: e.tensor_scalar(out=tg[2 + hf][0:64, :], in0=tg[hf][0:64, :], scalar1=gsub[:, hf:hf + 1],
                                                        scalar2=0.8, op0=ALU.mult, op1=ALU.mult),
                       reads=[b_tg[hf], b_g], writes=[b_tg[2 + hf]])
                    op("dve", lambda e: e.tensor_tensor(out=mixT[:, 8 + 2 * h + hf, :], in0=tg[2 + hf][0:64, :], in1=tg[4][0:64, :],
                                                        op=ALU.mult), reads=[b_tg[2 + hf], b_tg[4]], writes=[b_mix])
            for tt in range(4):
                r0 = tok0 + q0 + tt * 128
                yb = 4 if tt % 2 == 0 else 2
                ty = tys[tt % 2]; b_ty = b_tys[tt % 2]
                dma("sp", lambda e: e.dma_start(out=xr[:], in_=x_d[r0:r0 + 128, :]), writes=[b_xr])
                for cg in range(2):
                    for i in range(16):
                        op("pe", lambda e: e.matmul(PB[yb + cg][:], lhsT=mixT[:, i, tt * 128:(tt + 1) * 128],
                                                    rhs=wo[:, i, cg * 512:(cg + 1) * 512], start=(i == 0), stop=(i == 15)),
                           reads=[b_mix, b_wo], writes=[b_pb[yb + cg]])
                for cg in range(2):
                    op("act", lambda e: e.activation(out=ty[:, cg * 512:(cg + 1) * 512], in_=PB[yb + cg][:], func=AF.Copy),
                       reads=[b_pb[yb + cg]], writes=[b_ty])
                r = rstd_col("act", ty[:], ssB, D, EPS, [b_ty], b_ssB, junkB, b_junkB)
                op("dve", lambda e: e.scalar_tensor_tensor(out=ty[:], in0=ty[:], scalar=r, in1=GM[:, b, :],
                                                           op0=ALU.mult, op1=ALU.mult), reads=[b_ty, b_ssB, b_mod], writes=[b_ty])
                op("dve", lambda e: e.tensor_tensor(out=xm[:], in0=ty[:], in1=xr[:], op=ALU.add),
                   reads=[b_ty, b_xr], writes=[b_xm])
                if stage == "xm":
                    dma("sp", lambda e: e.dma_start(out=out_d[r0:r0 + 128, :], in_=xm[:]), reads=[b_xm], writes=[b_xmid])
                else:
                    dma("sp", lambda e: e.dma_start(out=xmid_d[r0:r0 + 128, :], in_=xm[:]), reads=[b_xm], writes=[b_xmid])
                r2 = rstd_col("act", xm[:], ssB, D, EPS, [b_xm], b_ssB, junkB, b_junkB)
                op("act", lambda e: e.activation(out=ty[:], in_=xm[:], func=AF.Copy, scale=r2), reads=[b_xm, b_ssB], writes=[b_ty])
                for kc in range(8):
                    pbk = PB[6 + kc // 4]; bpbk = b_pb[6 + kc // 4]
                    op("pe", lambda e: e.transpose(out=pbk[:, (kc % 4) * 128:(kc % 4 + 1) * 128], in_=ty[:, kc * 128:(kc + 1) * 128],
                                                   identity=ident_f[:]), reads=[b_ty, b_id], writes=[bpbk])
                for kc in range(8):
                    pbk = PB[6 + kc // 4]; bpbk = b_pb[6 + kc // 4]
                    src = pbk[:, (kc % 4) * 128:(kc % 4 + 1) * 128]
                    if kc % 2:
                        op("dve", lambda e: e.tensor_scalar(out=h2f[:, kc, :], in0=src, scalar1=FS[:, b, kc:kc + 1],
                                                            scalar2=FH[:, b, kc:kc + 1], op0=ALU.mult, op1=ALU.add),
                           reads=[bpbk, b_mod], writes=[b_h2f])
                    else:
                        op("act", lambda e: e.activation(out=h2f[:, kc, :], in_=src, func=AF.Identity,
                                                         bias=FH[:, b, kc:kc + 1], scale=FS[:, b, kc:kc + 1]),
                           reads=[bpbk, b_mod], writes=[b_h2f])
                op("act", lambda e: e.activation(out=h2b[:], in_=h2f[:], func=AF.Copy), reads=[b_h2f], writes=[b_h2b])
                dma("sp", lambda e: e.dma_start(out=h2T_d[:, :, r0:r0 + 128].rearrange("k p t -> p k t"), in_=h2b[:]),
                    reads=[b_h2b], writes=[b_h2d])
                for kc in range(8):
                    op("pe", lambda e: e.matmul(PB[0][:, 0:NE], lhsT=h2f[:, kc, :], rhs=wr[:, kc, :], start=(kc == 0), stop=(kc == 7)),
                       reads=[b_h2f, b_wr], writes=[b_pb[0]])
                lg = rt[:, 0, :]; ex = rt[:, 1, :]; mk = rt[:, 2, :]; em = rt[:, 3, :]; gg = rt[:, 4, :]; m8 = rt[:, 5, 0:8]
                op("dve", lambda e: e.tensor_tensor(out=lg, in0=PB[0][:, 0:NE], in1=brr[:], op=ALU.add),
                   reads=[b_pb[0], b_wr], writes=[b_rt])
                op("dve", lambda e: e.max(out=m8, in_=lg), reads=[b_rt], writes=[b_rt])
                op("dve", lambda e: e.tensor_scalar(out=mk, in0=lg, scalar1=rt[:, 5, 3:4], scalar2=1.0, op0=ALU.is_ge, op1=ALU.mult),
                   reads=[b_rt], writes=[b_rt])
                op("dve", lambda e: e.tensor_scalar(out=rt[:, 5, 8:9], in0=rt[:, 5, 0:1], scalar1=-1.0, scalar2=0.0, op0=ALU.mult, op1=ALU.add),
                   reads=[b_rt], writes=[b_rt])
                op("act", lambda e: e.activation(out=ex, in_=lg, func=AF.Exp, bias=rt[:, 5, 8:9], scale=1.0),
                   reads=[b_rt], writes=[b_rt])
                op("dve", lambda e: e.memset(rt[:, 5, 9:10], 0.0), writes=[b_rt])
                op("dve", lambda e: e.scalar_tensor_tensor(out=em, in0=ex, scalar=1.0, in1=mk, op0=ALU.mult, op1=ALU.mult,
                                                           accum_out=rt[:, 5, 9:10]), reads=[b_rt], writes=[b_rt])
                op("dve", lambda e: e.reciprocal(out=rt[:, 5, 10:11], in_=rt[:, 5, 9:10]), reads=[b_rt], writes=[b_rt])
                op("dve", lambda e: e.tensor_scalar(out=gg, in0=em, scalar1=rt[:, 5, 10:11], scalar2=1.0, op0=ALU.mult, op1=ALU.mult),
                   reads=[b_rt], writes=[b_rt])
                op("pe", lambda e: e.transpose(out=PB[1][0:32, 0:128], in_=gg, identity=ident_f[:]),
                   reads=[b_rt, b_id], writes=[b_pb[1]])
                op("dve", lambda e: e.tensor_copy(out=gts[:], in_=PB[1][0:32, 0:128]), reads=[b_pb[1]], writes=[b_gts])
                dma("sp", lambda e: e.dma_start(out=GT_d[:, r0:r0 + 128], in_=gts[:]), reads=[b_gts], writes=[b_gtd])
        Bs.close()
        AB.close()
    P0.close()

    if stage == "xm":
        fence(c, [b_xmid])
        c.wait_all("sp", [b_xmid])
        print("instructions", c.n_ins, "waits", c.n_wait, "dsems", c.n_dsem)
        return nc

    TG = 1024
    M = Scope()
    yacc = M.T("yacc", [128, 8, TG], F32); b_yacc = M.B("yacc")
    h2g = M.T("h2g", [128, 8, TG], BF16); b_h2g = M.B("h2g")
    wgu = [M.T("wgu%d" % i, [128, 8, 2 * D], BF16) for i in range(2)]; b_wgu = [M.B("wgu0"), M.B("wgu1")]
    wdn = M.T("wdn", [128, 8, D], BF16); b_wdn = M.B("wdn")
    gtg = M.T("gtg", [32, TG], F32); b_gtg = M.B("gtg")
    bdn = M.T("bdn", [32, D], F32); bguc = M.T("bguc", [128, NE, 16], F32); b_mw = M.B("moew")
    gbc = [M.T("gbc%d" % i, [128, TG], F32) for i in range(2)]; b_gbc = [M.B("gbc0"), M.B("gbc1")]
    tA = [M.T("tA%d" % i, [128, 512], F32) for i in range(2)]; b_tA = [M.B("tA0"), M.B("tA1")]
    tS = [M.T("tS%d" % i, [128, 512], F32) for i in range(2)]; b_tS = [M.B("tS0"), M.B("tS1")]
    tU = [M.T("tU%d" % i, [128, 512], F32) for i in range(2)]; b_tU = [M.B("tU0"), M.B("tU1")]
    actT = [M.T("actT%d" % i, [128, 8, 512], BF16) for i in range(2)]; b_act = [M.B("act0"), M.B("act1")]
    GFt = M.T("GFt", [128, NB, D], F32); b_gft2 = M.B("GFt")
    tyF = M.T("tyF", [128, D], F32); b_tyF = M.B("tyF")
    xmt = M.T("xmt", [128, D], F32); b_xmt = M.B("xmt")
    ot = M.T("ot", [128, D], F32); b_ot = M.B("ot")
    ssF = M.T("ssF", [128, 8], F32); b_ssF = M.B("ssF")
    b_out = Buf("out")
    dma("sp", lambda e: [e.dma_start(out=bdn[:], in_=bdn_d), e.dma_start(out=bguc[:], in_=bguc_d)], writes=[b_mw])
    dma("sp", lambda e: e.dma_start(out=GFt[:], in_=gf_d), writes=[b_gft2])
    units = [(g_, e_) for g_ in range(NT // TG) for e_ in range(NE)]

    def load_wgu(u):
        ex_ = units[u][1]
        dma("pool", lambda e: e.dma_start(out=wgu[u % 2][:], in_=wgu_d[ex_].rearrange("(k p) n -> p k n", p=128)),
            writes=[b_wgu[u % 2]])

    def load_wdn(u):
        ex_ = units[u][1]
        dma("pool", lambda e: e.dma_start(out=wdn[:], in_=wdn_d[ex_].rearrange("(k p) n -> p k n", p=128)), writes=[b_wdn])

    load_wgu(0); load_wdn(0)
    for g in range(NT // TG):
        ts = slice(g * TG, (g + 1) * TG)
        dma("sp", lambda e: e.dma_start(out=h2g[:], in_=h2T_d[:, :, ts].rearrange("k p t -> p k t")), writes=[b_h2g])
        dma("sp", lambda e: e.dma_start(out=gtg[:], in_=GT_d[:, ts]), writes=[b_gtg])
        for dc in range(8):
            for tc in range(2):
                pbk = PB[4 + (2 * dc + tc) % 4]; bpbk = b_pb[4 + (2 * dc + tc) % 4]
                op("pe", lambda e: e.matmul(pbk[:], lhsT=bdn[:, dc * 128:(dc + 1) * 128], rhs=gtg[:, tc * 512:(tc + 1) * 512],
                                            start=True, stop=True), reads=[b_mw, b_gtg], writes=[bpbk])
                op("act", lambda e: e.activation(out=yacc[:, dc, tc * 512:(tc + 1) * 512], in_=pbk[:], func=AF.Copy),
                   reads=[bpbk], writes=[b_yacc])
        for ex in range(NE):
            u = g * NE + ex
            wi = u % 2
            w_ = wgu[wi]; bw_ = b_wgu[wi]
            if u + 1 < len(units):
                load_wgu(u + 1)
            gb_ = gbc[u % 2]; bgb_ = b_gbc[u % 2]
            dma("sp", lambda e: e.dma_start(out=gb_[:], in_=GT_d[ex:ex + 1, ts].partition_broadcast(128)), writes=[bgb_])
            for tc in range(2):
                tcs = slice(tc * 512, (tc + 1) * 512)
                at = actT[tc]; bat = b_act[tc]
                for fc in range(8):
                    pa = PB[2 * (fc % 2)]; bpa = b_pb[2 * (fc % 2)]; pu = PB[2 * (fc % 2) + 1]; bpu = b_pb[2 * (fc % 2) + 1]
                    for kc in range(8):
                        op("pe", lambda e: e.matmul(pa[:], lhsT=w_[:, kc, fc * 128:(fc + 1) * 128], rhs=h2g[:, kc, tcs],
                                                    start=(kc == 0), stop=(kc == 7)), reads=[bw_, b_h2g], writes=[bpa])
                    for kc in range(8):
                        op("pe", lambda e: e.matmul(pu[:], lhsT=w_[:, kc, D + fc * 128:D + (fc + 1) * 128], rhs=h2g[:, kc, tcs],
                                                    start=(kc == 0), stop=(kc == 7)), reads=[bw_, b_h2g], writes=[bpu])
                    a_ = tA[fc % 2]; ba_ = b_tA[fc % 2]; s_ = tS[fc % 2]; bs_ = b_tS[fc % 2]; u_ = tU[fc % 2]; bu_ = b_tU[fc % 2]
                    op("dve", lambda e: e.tensor_scalar(out=a_[:], in0=pa[:], scalar1=bguc[:, ex, fc:fc + 1], scalar2=7.0,
                                                        op0=ALU.add, op1=ALU.min), reads=[bpa, b_mw], writes=[ba_])
                    op("act", lambda e: e.activation(out=s_[:], in_=a_[:], func=AF.Sigmoid, scale=1.702), reads=[ba_], writes=[bs_])
                    op("act", lambda e: e.activation(out=u_[:], in_=pu[:], func=AF.Identity, bias=bguc[:, ex, 8 + fc:9 + fc], scale=1.0),
                       reads=[bpu, b_mw], writes=[bu_])
                    op("dve", lambda e: e.tensor_scalar(out=u_[:], in0=u_[:], scalar1=-7.0, scalar2=7.0,
                                                        op0=ALU.max, op1=ALU.min), reads=[bu_], writes=[bu_])
                    op("dve", lambda e: e.tensor_tensor(out=a_[:], in0=a_[:], in1=s_[:], op=ALU.mult), reads=[ba_, bs_], writes=[ba_])
                    op("dve", lambda e: e.scalar_tensor_tensor(out=a_[:], in0=u_[:], scalar=1.0, in1=a_[:], op0=ALU.add, op1=ALU.mult),
                       reads=[ba_, bu_], writes=[ba_])
                    op("dve", lambda e: e.tensor_tensor(out=at[:, fc, :], in0=a_[:], in1=gb_[:, tcs], op=ALU.mult),
                       reads=[ba_, bgb_], writes=[bat])
            for tc in range(2):
                tcs = slice(tc * 512, (tc + 1) * 512)
                at = actT[tc]; bat = b_act[tc]
                for dc in range(8):
                    py = PB[4 + dc % 4]; bpy = b_pb[4 + dc % 4]
                    for fc in range(8):
                        op("pe", lambda e: e.matmul(py[:], lhsT=wdn[:, fc, dc * 128:(dc + 1) * 128], rhs=at[:, fc, :],
                                                    start=(fc == 0), stop=(fc == 7)), reads=[b_wdn, bat], writes=[bpy])
                    op("dve", lambda e: e.tensor_tensor(out=yacc[:, dc, tcs], in0=py[:], in1=yacc[:, dc, tcs], op=ALU.add),
                       reads=[bpy, b_yacc], writes=[b_yacc])
            if u + 1 < len(units):
                load_wdn(u + 1)
        for tt in range(TG // 128):
            r0 = g * TG + tt * 128
            bb = r0 // S
            dma("sp", lambda e: e.dma_start(out=xmt[:], in_=xmid_d[r0:r0 + 128, :]), writes=[b_xmt])
            for dc in range(8):
                pbk = PB[dc // 4]; bpbk = b_pb[dc // 4]
                op("pe", lambda e: e.transpose(out=pbk[:, (dc % 4) * 128:(dc % 4 + 1) * 128], in_=yacc[:, dc, tt * 128:(tt + 1) * 128],
                                               identity=ident_f[:]), reads=[b_yacc, b_id], writes=[bpbk])
            for cg in range(2):
                op("act", lambda e: e.activation(out=tyF[:, cg * 512:(cg + 1) * 512], in_=PB[cg][:], func=AF.Copy),
                   reads=[b_pb[cg]], writes=[b_tyF])
            r = rstd_col("act", tyF[:], ssF, D, EPS, [b_tyF], b_ssF, ot[:], b_ot)
            op("dve", lambda e: e.scalar_tensor_tensor(out=tyF[:], in0=tyF[:], scalar=r, in1=GFt[:, bb, :],
                                                       op0=ALU.mult, op1=ALU.mult), reads=[b_tyF, b_ssF, b_gft2], writes=[b_tyF])
            op("dve", lambda e: e.tensor_tensor(out=ot[:], in0=tyF[:], in1=xmt[:], op=ALU.add),
               reads=[b_tyF, b_xmt], writes=[b_ot])
            dma("sp", lambda e: e.dma_start(out=out_d[r0:r0 + 128, :], in_=ot[:]), reads=[b_ot], writes=[b_out])
    fence(c, [b_out])
    M.close()
    c.wait_all("sp", [b_out])
    print("instructions", c.n_ins, "waits", c.n_wait, "dsems", c.n_dsem)
    return nc


def fence(c, bufs):
    deps = []
    for b in bufs:
        deps.append(b.last_w)
        deps.extend(b.readers)
    for e in c.ENGS:
        c._need(e, deps)


_NC_CACHE = {}


def kernel(**inputs):
    stage = inputs.pop("_stage", "full")
    inp = {k: np.asarray(v) for k, v in inputs.items()}
    maps = host_prep(inp)
    if stage not in _NC_CACHE:
        _NC_CACHE[stage] = build(stage)
    nc = _NC_CACHE[stage]
    ncr = NCORES if stage == "full" else 1
    maps = [{k: m[k] for k in nc._in_names} for m in maps[:ncr]]
    import os
    if os.environ.get("KTRACE"):
        res = run_bass_kernel_spmd(nc, maps, core_ids=list(range(ncr)), trace=True)
        print("KTRACE exec_time_ns", res.exec_time_ns)
    else:
        res = run_bass_kernel_spmd(nc, maps, core_ids=list(range(ncr)))
    out = np.concatenate([r["out"] for r in res.results], axis=0)
    if ncr < NCORES:
        out = np.concatenate([out, np.zeros(((NCORES - ncr) * NT, D), np.float32)], 0)
    return out.reshape(16, S, D).astype(np.float32)
```

```python
import math
import numpy as np
import concourse.bass as bass
import concourse.mybir as mybir
from concourse.bass_utils import run_bass_kernel_spmd

F32 = mybir.dt.float32
BF16 = mybir.dt.bfloat16
I32 = mybir.dt.int32
ALU = mybir.AluOpType
AF = mybir.ActivationFunctionType
AX = mybir.AxisListType

NCORES = 8
D = 1024
S = 2048
NB = 2
NT = NB * S
NE = 32
PI = math.pi
CW1 = 6.28125
CW2 = 2 * math.pi - 6.28125
PI_LO = 3.1415925


class Buf:
    __slots__ = ("name", "last_w", "readers", "dsem", "dcnt")

    def __init__(self, name):
        self.name = name
        self.last_w = None
        self.readers = []
        self.dsem = None
        self.dcnt = 0


class Ctx:
    ENGS = ("pe", "act", "dve", "pool", "sp")

    def __init__(self, nc):
        self.nc = nc
        self.eng = {"pe": nc.tensor, "act": nc.scalar, "dve": nc.vector,
                    "pool": nc.gpsimd, "sp": nc.sync}
        self.sems = {}
        self.cnt = {}
        for e in self.ENGS:
            self.sems[e] = nc.alloc_semaphore("s_" + e)
            self.cnt[e] = 0
        self.waited = {}
        self.n_dsem = 0
        self.n_wait = 0
        self.n_ins = 0

    def sb(self, name, shape, dt):
        return self.nc.alloc_sbuf_tensor("sb_" + name, list(shape), dt)

    def ps(self, name, shape, dt=F32):
        return self.nc.alloc_psum_tensor("ps_" + name, list(shape), dt)

    def _need(self, eng, deps):
        best = {}
        for d in deps:
            if d is None:
                continue
            k, v = d
            if k == "pe" and eng == "pe":
                continue
            if best.get(k, 0) < v:
                best[k] = v
        for k, v in best.items():
            if self.waited.get((eng, k), 0) >= v:
                continue
            self.eng[eng].wait_ge(self.sems[k], v)
            self.waited[(eng, k)] = v
            self.n_wait += 1

    @staticmethod
    def _deps(reads, writes):
        deps = []
        for b in reads:
            deps.append(b.last_w)
        for b in writes:
            deps.append(b.last_w)
            deps.extend(b.readers)
        return deps

    def op(self, eng, fn, reads=(), writes=()):
        self._need(eng, self._deps(reads, writes))
        ins = fn(self.eng[eng])
        self.cnt[eng] += 1
        ins.then_inc(self.sems[eng], 1)
        self.n_ins += 1
        tok = (eng, self.cnt[eng])
        for b in reads:
            b.readers.append(tok)
            if len(b.readers) > 10:
                best = {}
                for k, v in b.readers:
                    if best.get(k, 0) < v:
                        best[k] = v
                b.readers = list(best.items())
        for b in writes:
            b.last_w = tok
            b.readers = []
        return ins

    def _dsem(self, b):
        if b.dsem is None:
            b.dsem = "d%d_%s" % (self.n_dsem, b.name)
            self.n_dsem += 1
            self.sems[b.dsem] = self.nc.alloc_semaphore(b.dsem)
        return b.dsem

    def dma(self, eng, fn, reads=(), writes=(), owner=None):
        self._need(eng, self._deps(reads, writes))
        own = owner if owner is not None else writes[0]
        k = self._dsem(own)
        res = fn(self.eng[eng])
        if not isinstance(res, (list, tuple)):
            res = [res]
        for ins in res:
            ins.then_inc(self.sems[k], 16)
            own.dcnt += 16
            self.n_ins += 1
        tok = (k, own.dcnt)
        for b in reads:
            b.readers.append(tok)
        for b in writes:
            b.last_w = tok
            b.readers = []
        return tok

    def wait_all(self, eng, bufs):
        self._need(eng, [b.last_w for b in bufs])


def _col(v, p=128):
    return np.ascontiguousarray(v.reshape(-1, p).T)


def _rope_perm(n_half):
    i = np.arange(2 * n_half)
    return (i + n_half) % (2 * n_half)


def host_prep(inp):
    f = np.float32
    x = inp["x"]; c = inp["c"]; pos = inp["positions"]
    w_ada = np.ascontiguousarray(inp["w_ada"][0]); b_ada = inp["b_ada"][0]
    w_in = inp["w_in"][0]
    cq = w_in[:, 0:256]; ckv = w_in[:, 256:384]; kr = w_in[:, 384:416]
    qd = w_in[:, 416:928]; kd = w_in[:, 928:1440]; vd = w_in[:, 1440:1952]
    pm = _rope_perm(16)
    z64 = np.zeros((D, 64), f); z32 = np.zeros((D, 32), f)
    groups = [cq[:, 0:128], cq[:, 128:256], ckv,
              np.concatenate([z64, kr, z32], 1), np.concatenate([z64, kr[:, pm], z32], 1)]
    pd = _rope_perm(32)
    def permd(w):
        w4 = w.reshape(D, 4, 2, 64)
        return np.ascontiguousarray(w4[:, :, :, pd]).reshape(D, 512)
    qdp = permd(qd); kdp = permd(kd)
    for h in range(4):
        groups.append(qd[:, h * 128:(h + 1) * 128])
    for h in range(4):
        groups.append(qdp[:, h * 128:(h + 1) * 128])
    for h in range(4):
        groups.append(kd[:, h * 128:(h + 1) * 128])
    for h in range(4):
        groups.append(kdp[:, h * 128:(h + 1) * 128])
    w_in_g = np.ascontiguousarray(np.stack(groups, 0))
    wq = inp["w_q_b"][0].reshape(256, 8, 96)
    wqA = np.ascontiguousarray(wq)
    wqB = np.zeros_like(wq)
    wqB[:, :, 64:96] = wq[:, :, 64:96][:, :, pm]
    wkv = inp["w_kv_b"][0].reshape(128, 8, 128)
    wkvk = np.ascontiguousarray(wkv[:, :, 0:64])
    wkvv = np.ascontiguousarray(wkv[:, :, 64:128]).reshape(128, 512)
    invf_d = (10000.0 ** (-np.arange(0, 64, 2, dtype=f) / f(64))).astype(f)
    invf_m = (10000.0 ** (-np.arange(0, 32, 2, dtype=f) / f(32))).astype(f)
    cst = np.zeros((128, 8), f)
    p = np.arange(128)
    cst[:, 0] = invf_d[p % 32]
    cst[:, 1] = np.where((p % 64) < 32, -1.0, 1.0)
    cst[64:96, 2] = invf_m[(p[64:96] - 64) % 16]
    cst[:, 3] = 1.0
    cst[64:80, 3] = -1.0
    cst[:, 4] = -PI * cst[:, 1]
    cst[:, 5] = -PI * cst[:, 3]
    cst[:, 6] = -PI
    ident = np.eye(128, dtype=f)
    maskT = (p[None, :] >= p[:, None]).astype(f)
    lam = np.stack([inp["lambda_q1"][0], inp["lambda_k1"][0], inp["lambda_q2"][0], inp["lambda_k2"][0]], 0)
    lam_rep = np.ascontiguousarray(np.broadcast_to(lam[None], (128, 4, 64))).astype(f)
    shared = dict(
        w_ada=w_ada, bada_col=_col(b_ada),
        bada_rep=np.ascontiguousarray(np.broadcast_to(
            np.concatenate([b_ada[2048:3072], b_ada[5120:6144]])[None], (128, 2048))).astype(f),
        gpre_col=_col(inp["g_mix_pre"][0]), gfpre_col=_col(inp["g_ffn_pre"][0]),
        gpost_rep=np.ascontiguousarray(np.broadcast_to(np.concatenate(
            [inp["g_mix_post"][0], inp["g_ffn_post"][0]])[None], (128, 2048))).astype(f),
        w_in_g=w_in_g, w_vd=np.ascontiguousarray(vd),
        wqA=wqA, wqB=wqB, gqa_col=_col(inp["g_q_a"][0]),
        wkvk=wkvk, wkvv=wkvv, gkva_col=_col(inp["g_kv_a"][0]),
        lam_rep=lam_rep, gsub_col=_col(inp["g_subln"][0], 64),
        w_o=np.ascontiguousarray(inp["w_o"][0]),
        w_r=np.ascontiguousarray(inp["w_router"][0]),
        br_rep=np.ascontiguousarray(np.broadcast_to(inp["b_router"][0][None], (128, NE))).astype(f),
        w_gu=np.ascontiguousarray(inp["w_gate_up"][0]),
        bgu_col=np.ascontiguousarray(inp["b_gate_up"][0].reshape(NE, 16, 128).transpose(2, 0, 1)),
        w_dn=np.ascontiguousarray(inp["w_down"][0]),
        b_dn=np.ascontiguousarray(inp["b_down"][0]),
        cst=cst, ident=ident, maskT=maskT,
    )
    maps = []
    for i in range(NCORES):
        bs = slice(i * NB, (i + 1) * NB)
        m = dict(shared)
        m["x"] = np.ascontiguousarray(x[bs].reshape(NT, D))
        cc = c[bs]
        m["ccol"] = np.ascontiguousarray(cc.reshape(NB, 8, 128).transpose(2, 1, 0))
        m["crep"] = np.ascontiguousarray(np.broadcast_to(
            m["ccol"][:, :, :, None], (128, 8, NB, 128))).astype(f)
        m["posr"] = np.ascontiguousarray(np.broadcast_to(pos[bs][:, None, :], (NB, 128, S))).astype(np.int32)
        maps.append(m)
    return maps


def build(stage="full"):
    nc = bass.Bass("TRN2", target_bir_lowering=False)
    c = Ctx(nc)

    names = []

    def din(name, shape, dt=F32):
        names.append(name)
        return nc.dram_tensor(name, list(shape), dt, kind="ExternalInput").ap()

    x_d = din("x", [NT, D]); ccol_d = din("ccol", [128, 8, NB]); crep_d = din("crep", [128, 8, NB, 128])
    posr_d = din("posr", [NB, 128, S], I32)
    wada_d = din("w_ada", [D, 6 * D]); badac_d = din("bada_col", [128, 48]); badar_d = din("bada_rep", [128, 2048])
    gpre_d = din("gpre_col", [128, 8]); gfpre_d = din("gfpre_col", [128, 8]); gpost_d = din("gpost_rep", [128, 2048])
    wing_d = din("w_in_g", [21, D, 128]); wvd_d = din("w_vd", [D, 512])
    wqA_d = din("wqA", [256, 8, 96]); wqB_d = din("wqB", [256, 8, 96]); gqa_d = din("gqa_col", [128, 2])
    wkvk_d = din("wkvk", [128, 8, 64]); wkvv_d = din("wkvv", [128, 512]); gkva_d = din("gkva_col", [128, 1])
    lam_d = din("lam_rep", [128, 4, 64]); gsub_d = din("gsub_col", [64, 2])
    wo_d = din("w_o", [D, D]); wr_d = din("w_r", [D, NE]); brr_d = din("br_rep", [128, NE])
    if stage == "full":
        wgu_d = din("w_gu", [NE, D, 2 * D]); bguc_d = din("bgu_col", [128, NE, 16])
        wdn_d = din("w_dn", [NE, D, D]); bdn_d = din("b_dn", [NE, D])
    nc._in_names = names
    cst_d = din("cst", [128, 8]); ident_d = din("ident", [128, 128]); maskT_d = din("maskT", [128, 128])
    out_d = nc.dram_tensor("out", [NT, D], F32, kind="ExternalOutput").ap()
    xmid_d = nc.dram_tensor("xmid", [NT, D], F32, kind="Internal").ap()
    h2T_d = nc.dram_tensor("h2T", [8, 128, NT], BF16, kind="Internal").ap()
    GT_d = nc.dram_tensor("GTd", [NE, NT], F32, kind="Internal").ap()
    gf_d = nc.dram_tensor("gfd", [128, NB, D], F32, kind="Internal").ap()
    b_gfd = Buf("gfd")

    op = c.op; dma = c.dma

    cst = c.sb("cst", [128, 8], F32); b_cst = Buf("cst")
    ident_f = c.sb("ident_f", [128, 128], F32); ident_b = c.sb("ident_b", [128, 128], BF16); b_id = Buf("ident")
    maskT = c.sb("maskT", [128, 128], BF16); b_mask = Buf("maskT")
    ones_b = c.sb("ones_b", [128, 128], BF16); ones_f = c.sb("ones_f", [128, 128], F32); b_ones = Buf("ones")
    dma("sp", lambda e: e.dma_start(out=cst[:], in_=cst_d), writes=[b_cst])
    dma("sp", lambda e: e.dma_start(out=ident_f[:], in_=ident_d), writes=[b_id])
    b_idb = Buf("identb")
    dma("pool", lambda e: [e.dma_start(out=ident_b[:], in_=ident_d)], writes=[b_idb])
    dma("pool", lambda e: e.dma_start(out=maskT[:], in_=maskT_d), writes=[b_mask])
    op("dve", lambda e: e.memset(ones_b[:], 1.0), writes=[b_ones])
    op("dve", lambda e: e.memset(ones_f[:], 1.0), writes=[b_ones])

    from contextlib import ExitStack
    uid = [0]

    class Scope:
        def __init__(self):
            self.es = ExitStack(); self.bufs = []
        def T(self, name, shape, dt):
            uid[0] += 1
            return self.es.enter_context(nc.sbuf_tensor("t_%s_%d" % (name, uid[0]), list(shape), dt))
        def B(self, name):
            b = Buf(name); self.bufs.append(b); return b
        def close(self):
            fence(c, self.bufs)
            self.es.close()

    def bf(ap):
        return ap.bitcast(BF16)

    MS = c.sb("MS", [128, NB, 8], F32); MH = c.sb("MH", [128, NB, 8], F32)
    FS = c.sb("FS", [128, NB, 8], F32); FH = c.sb("FH", [128, NB, 8], F32)
    b_mod = Buf("mod")
    lamc = c.sb("lamc", [128, 4], F32); b_lam = Buf("lam")
    gqa = c.sb("gqa", [128, 2], F32); gkva = c.sb("gkva", [128, 1], F32); gsub = c.sb("gsub", [64, 2], F32)
    b_g = Buf("gains")
    dma("sp", lambda e: [e.dma_start(out=gqa[:], in_=gqa_d), e.dma_start(out=gkva[:], in_=gkva_d),
                         e.dma_start(out=gsub[:], in_=gsub_d)], writes=[b_g])

    P0 = Scope()
    GM = P0.T("GM", [128, NB, D], F32)
    PB = [c.ps("pb%d" % i, [128, 512], F32) for i in range(8)]
    b_pb = [Buf("pb%d" % i) for i in range(8)]

    with nc.sbuf_tensor("t_ccol", [128, 8, NB], F32) as ccol, \
         nc.sbuf_tensor("t_crep", [128, 8, NB, 128], F32) as crep, \
         nc.sbuf_tensor("t_wa0", [128, 8, 512], F32) as wa0, \
         nc.sbuf_tensor("t_wa1", [128, 8, 512], F32) as wa1, \
         nc.sbuf_tensor("t_badac", [128, 48], F32) as badac, \
         nc.sbuf_tensor("t_badar", [128, 2048], F32) as badar, \
         nc.sbuf_tensor("t_gpre", [128, 8], F32) as gpre, \
         nc.sbuf_tensor("t_gfpre", [128, 8], F32) as gfpre, \
         nc.sbuf_tensor("t_gpost", [128, 2048], F32) as gpost, \
         nc.sbuf_tensor("t_modc", [128, 48, NB], F32) as modc, \
         nc.sbuf_tensor("t_lamt", [128, 4, 64], F32) as lamt, \
         nc.sbuf_tensor("t_lamj", [128, 64], F32) as lamj, \
         nc.sbuf_tensor("t_GF", [128, NB, D], F32) as GF:
        b_cc = Buf("cc"); b_wa = [Buf("wa0"), Buf("wa1")]; b_misc = Buf("misc0"); b_modc = Buf("modc")
        wa = [wa0, wa1]
        dma("sp", lambda e: [e.dma_start(out=ccol[:], in_=ccol_d), e.dma_start(out=crep[:], in_=crep_d)],
            writes=[b_cc])
        dma("sp", lambda e: [e.dma_start(out=badac[:], in_=badac_d), e.dma_start(out=badar[:], in_=badar_d),
                             e.dma_start(out=gpre[:], in_=gpre_d), e.dma_start(out=gfpre[:], in_=gfpre_d),
                             e.dma_start(out=gpost[:], in_=gpost_d), e.dma_start(out=lamt[:], in_=lam_d)],
            writes=[b_misc])
        op("act", lambda e: e.activation(out=ccol[:], in_=ccol[:], func=AF.Silu), reads=[b_cc], writes=[b_cc])
        op("act", lambda e: e.activation(out=crep[:], in_=crep[:], func=AF.Silu), reads=[b_cc], writes=[b_cc])
        op("dve", lambda e: e.memset(lamc[:], 0.0), writes=[b_lam])
        for j in range(2):
            op("dve", lambda e: e.scalar_tensor_tensor(out=lamj[:], in0=lamt[:, 2 * j, :], scalar=1.0,
                                                       in1=lamt[:, 2 * j + 1, :], op0=ALU.mult, op1=ALU.mult,
                                                       accum_out=lamc[:, j:j + 1]),
               reads=[b_misc], writes=[b_lam, b_misc])
        op("act", lambda e: e.activation(out=lamc[:, 0:2], in_=lamc[:, 0:2], func=AF.Exp), reads=[b_lam], writes=[b_lam])
        op("dve", lambda e: e.tensor_tensor(out=lamc[:, 2:3], in0=lamc[:, 0:1], in1=lamc[:, 1:2], op=ALU.subtract),
           reads=[b_lam], writes=[b_lam])
        op("dve", lambda e: e.tensor_scalar(out=lamc[:, 3:4], in0=lamc[:, 2:3], scalar1=0.2, scalar2=-1.0,
                                            op0=ALU.add, op1=ALU.mult), reads=[b_lam], writes=[b_lam])
        for sl in range(12):
            w = wa[sl % 2]; bw = b_wa[sl % 2]
            dma("sp", lambda e: e.dma_start(out=w[:], in_=wada_d[:, sl * 512:(sl + 1) * 512]
                                            .rearrange("(k p) n -> p k n", p=128)), writes=[bw])
            if sl in (4, 5, 10, 11):
                gi = 0 if sl < 6 else 1
                half = sl % 2 if sl < 6 else (sl - 10)
                dst = GM if gi == 0 else GF
                for b in range(NB):
                    pbk = PB[b]; bpb = b_pb[b]
                    for kc in range(8):
                        op("pe", lambda e: e.matmul(pbk[:], lhsT=crep[:, kc, b, :], rhs=w[:, kc, :],
                                                    start=(kc == 0), stop=(kc == 7)),
                           reads=[b_cc, bw], writes=[bpb])
                    cs = slice(half * 512, (half + 1) * 512)
                    rs = slice(gi * 1024 + half * 512, gi * 1024 + (half + 1) * 512)
                    op("dve", lambda e: e.tensor_tensor(out=dst[:, b, cs], in0=pbk[:], in1=badar[:, rs], op=ALU.add),
                       reads=[bpb, b_misc], writes=[b_mod])
                    op("dve", lambda e: e.tensor_tensor(out=dst[:, b, cs], in0=dst[:, b, cs], in1=gpost[:, rs],
                                                        op=ALU.mult), reads=[b_mod, b_misc], writes=[b_mod])
            else:
                pbk = PB[2 + sl % 2]; bpb = b_pb[2 + sl % 2]
                for jj in range(4):
                    for kc in range(8):
                        op("pe", lambda e: e.matmul(pbk[:, jj * NB:(jj + 1) * NB], lhsT=w[:, kc, jj * 128:(jj + 1) * 128],
                                                    rhs=ccol[:, kc, :], start=(kc == 0), stop=(kc == 7)),
                           reads=[b_cc, bw], writes=[bpb])
                op("dve", lambda e: e.tensor_copy(out=modc[:, sl * 4:(sl + 1) * 4, :],
                                                  in_=pbk[:, 0:4 * NB].rearrange("p (j b) -> p j b", b=NB)),
                   reads=[bpb], writes=[b_modc])
        for b in range(NB):
            op("dve", lambda e: e.tensor_tensor(out=MH[:, b, :], in0=modc[:, 0:8, b], in1=badac[:, 0:8], op=ALU.add),
               reads=[b_modc, b_misc], writes=[b_mod])
            op("dve", lambda e: e.tensor_tensor(out=FH[:, b, :], in0=modc[:, 24:32, b], in1=badac[:, 24:32], op=ALU.add),
               reads=[b_modc, b_misc], writes=[b_mod])
            for (dst, j0, gg) in ((MS, 8, gpre), (FS, 32, gfpre)):
                op("dve", lambda e: e.scalar_tensor_tensor(out=dst[:, b, :], in0=modc[:, j0:j0 + 8, b], scalar=1.0,
                                                           in1=badac[:, j0:j0 + 8], op0=ALU.add, op1=ALU.add),
                   reads=[b_modc, b_misc], writes=[b_mod])
                op("dve", lambda e: e.tensor_tensor(out=dst[:, b, :], in0=dst[:, b, :], in1=gg[:], op=ALU.mult),
                   reads=[b_mod, b_misc], writes=[b_mod])
        b_gft = Buf("gft")
        op("dve", lambda e: e.tensor_copy(out=GF[:, :, 0:1], in_=GF[:, :, 0:1]), reads=[b_mod], writes=[b_gft])
        dma("sp", lambda e: e.dma_start(out=gf_d, in_=GF[:]), reads=[b_gft, b_mod], writes=[b_gfd])
        fence(c, [b_cc, b_misc, b_modc, b_gft, b_gfd] + b_wa)


    wqA = P0.T("wqA", [128, 2, 8, 96], BF16); wqB = P0.T("wqB", [128, 2, 8, 96], BF16)
    wkvk = P0.T("wkvk", [128, 8, 64], BF16); wkvv = P0.T("wkvv", [128, 512], BF16)
    wr = P0.T("wr", [128, 8, NE], F32); brr = P0.T("brr", [128, NE], F32)
    b_w = P0.B("attw")
    dma("pool", lambda e: [e.dma_start(out=wqA[:], in_=wqA_d.rearrange("(k p) h n -> p k h n", p=128)),
                           e.dma_start(out=wqB[:], in_=wqB_d.rearrange("(k p) h n -> p k h n", p=128)),
                           e.dma_start(out=wkvk[:], in_=wkvk_d), e.dma_start(out=wkvv[:], in_=wkvv_d)],
        writes=[b_w])
    b_wr = P0.B("wr")
    dma("sp", lambda e: [e.dma_start(out=wr[:], in_=wr_d.rearrange("(k p) n -> p k n", p=128)),
                         e.dma_start(out=brr[:], in_=brr_d)], writes=[b_wr])
    EPS = 1e-6
    SC_M = 96.0 ** -0.5
    SC_D = 64.0 ** -0.5

    def rstd_col(eng_sq, src, ss, n, eps, reads, bss, junk, bjunk):
        op("dve", lambda e: e.memset(ss[:, 0:1], 0.0), writes=[bss])
        op("act", lambda e: e.activation(out=junk, in_=src, func=AF.Square, accum_out=ss[:, 0:1]),
           reads=reads + [bss], writes=[bss, bjunk])
        op("dve", lambda e: e.tensor_scalar(out=ss[:, 1:2], in0=ss[:, 0:1], scalar1=1.0 / n, scalar2=eps,
                                            op0=ALU.mult, op1=ALU.add), reads=[bss], writes=[bss])
        op("dve", lambda e: e.reciprocal(out=ss[:, 2:3], in_=ss[:, 1:2]), reads=[bss], writes=[bss])
        op("act", lambda e: e.activation(out=ss[:, 3:4], in_=ss[:, 2:3], func=AF.Sqrt), reads=[bss], writes=[bss])
        return ss[:, 3:4]

    def rstd_bc(pbank, bpbank, n, eps, dst, bdst, rows=128):
        op("dve", lambda e: e.tensor_scalar(out=dst[0:rows, :], in0=pbank[0:rows, :], scalar1=1.0 / n, scalar2=eps,
                                            op0=ALU.mult, op1=ALU.add), reads=[bpbank], writes=[bdst])
        op("dve", lambda e: e.reciprocal(out=dst[0:rows, :], in_=dst[0:rows, :]), reads=[bdst], writes=[bdst])
        op("act", lambda e: e.activation(out=dst[0:rows, :], in_=dst[0:rows, :], func=AF.Sqrt), reads=[bdst], writes=[bdst])

    for b in range(NB):
        tok0 = b * S
        AB = Scope()
        cqnT = AB.T("cqnT", [128, 2, S], BF16); b_cqn = AB.B("cqn")
        ckvnT = AB.T("ckvnT", [128, S], BF16); b_ckvn = AB.B("ckvn")
        krT = AB.T("krT", [96, S], BF16); b_kr = AB.B("kr")
        vm = AB.T("vm", [128, 16, 8, 65], BF16); b_vm = AB.B("vm")
        qdT = AB.T("qdT", [128, 4, S], BF16); b_qd = AB.B("qd")
        kdT = AB.T("kdT", [128, 4, S], BF16); b_kd = AB.B("kd")
        vd = AB.T("vd", [128, 16, 4, 2, 65], BF16); b_vd = AB.B("vd")
        Cm = AB.T("Cm", [96, S], BF16); Sm = AB.T("Sm", [96, S], BF16); b_tm = AB.B("tabm")
        op("pool", lambda e: e.memset(vm[:], 1.0), writes=[b_vm])
        op("pool", lambda e: e.memset(vd[:], 1.0), writes=[b_vd])
        op("pool", lambda e: e.memset(krT[:], 0.0), writes=[b_kr])

        A = Scope()
        hT = A.T("hT", [128, 8, S], BF16); b_hT = A.B("hT")
        wing_m = A.T("wing_m", [128, 8, 640], BF16); b_wm = A.B("wing_m")
        wing_p = [A.T("wing_p%d" % i, [128, 8, 256], BF16) for i in range(2)]; b_wp = [A.B("wp0"), A.B("wp1")]
        wvd = A.T("wvd", [128, 8, 512], BF16); b_wvd = A.B("wvd")
        Cd = A.T("Cd", [128, S], BF16); Sd = A.T("Sd", [128, S], BF16); b_td = A.B("tabd")
        posi = A.T("posi", [128, 512], I32); posf = A.T("posf", [128, 512], F32); angt = A.T("angt", [128, 512], F32)
        angi = A.T("angi", [128, 512], I32); angk = A.T("angk", [128, 512], F32)
        b_pos = A.B("pos"); b_ang = A.B("ang"); b_angi = A.B("angi"); b_angk = A.B("angk")
        xt = [A.T("xt%d" % i, [128, D], F32) for i in range(2)]; b_xt = [A.B("xt0"), A.B("xt1")]
        xn = [A.T("xn%d" % i, [128, D], BF16) for i in range(2)]; b_xn = [A.B("xn0"), A.B("xn1")]
        ssA = A.T("ssA", [128, 8], F32); b_ssA = A.B("ssA")
        tb = [A.T("tb%d" % i, [128, 512], BF16) for i in range(2)]; b_tb = [A.B("tb0"), A.B("tb1")]
        tf = [A.T("tf%d" % i, [128, 512], F32) for i in range(3)]; b_tf = [A.B("tf%d" % i) for i in range(3)]

        for g in range(5):
            dma("pool", lambda e: e.dma_start(out=wing_m[:, :, g * 128:(g + 1) * 128],
                                              in_=wing_d[g].rearrange("(k p) n -> p k n", p=128)), writes=[b_wm])
        dma("pool", lambda e: e.dma_start(out=wvd[:], in_=wvd_d.rearrange("(k p) n -> p k n", p=128)), writes=[b_wvd])
        for ch in range(4):
            cs = slice(ch * 512, (ch + 1) * 512)
            dma("sp", lambda e: e.dma_start(out=posi[:], in_=posr_d[b][:, cs]), writes=[b_pos])
            op("dve", lambda e: e.tensor_copy(out=posf[:], in_=posi[:]), reads=[b_pos], writes=[b_pos])
            for (Ct, St, rows, ci, si, bt) in ((Cd, Sd, 128, 0, 1, b_td), (Cm, Sm, 96, 2, 3, b_tm)):
                for (dstT, shift, use_sgn) in ((Ct, 0.5 * PI, False), (St, 0.0, True)):
                    R = slice(0, rows)
                    op("dve", lambda e: e.tensor_scalar(out=angt[R, :], in0=posf[R, :], scalar1=cst[R, ci:ci + 1],
                                                        scalar2=shift, op0=ALU.mult, op1=ALU.add),
                       reads=[b_pos, b_cst], writes=[b_ang])
                    op("dve", lambda e: e.tensor_scalar(out=angi[R, :], in0=angt[R, :], scalar1=1.0 / (2 * PI), scalar2=0.0,
                                                        op0=ALU.mult, op1=ALU.add), reads=[b_ang], writes=[b_angi])
                    op("dve", lambda e: e.tensor_copy(out=angk[R, :], in_=angi[R, :]), reads=[b_angi], writes=[b_angk])
                    op("dve", lambda e: e.scalar_tensor_tensor(out=angt[R, :], in0=angk[R, :], scalar=-CW1, in1=angt[R, :],
                                                               op0=ALU.mult, op1=ALU.add), reads=[b_angk, b_ang], writes=[b_ang])
                    op("dve", lambda e: e.scalar_tensor_tensor(out=angt[R, :], in0=angk[R, :], scalar=-CW2, in1=angt[R, :],
                                                               op0=ALU.mult, op1=ALU.add), reads=[b_angk, b_ang], writes=[b_ang])
                    op("dve", lambda e: e.tensor_scalar(out=angk[R, :], in0=angt[R, :], scalar1=PI, scalar2=2 * PI,
                                                        op0=ALU.is_gt, op1=ALU.mult), reads=[b_ang], writes=[b_angk])
                    op("dve", lambda e: e.tensor_tensor(out=angt[R, :], in0=angt[R, :], in1=angk[R, :], op=ALU.subtract),
                       reads=[b_ang, b_angk], writes=[b_ang])
                    op("dve", lambda e: e.tensor_scalar(out=angt[R, :], in0=angt[R, :], scalar1=-PI_LO, scalar2=PI_LO,
                                                        op0=ALU.max, op1=ALU.min), reads=[b_ang], writes=[b_ang])
                    if use_sgn:
                        op("act", lambda e: e.activation(out=dstT[R, cs], in_=angt[R, :], func=AF.Sin, scale=cst[R, si:si + 1]),
                           reads=[b_ang, b_cst], writes=[bt])
                    else:
                        op("act", lambda e: e.activation(out=dstT[R, cs], in_=angt[R, :], func=AF.Sin),
                           reads=[b_ang, b_cst], writes=[bt])
        for t in range(16):
            xi = xt[t % 2]; bxi = b_xt[t % 2]; xni = xn[t % 2]; bxni = b_xn[t % 2]
            dma("sp", lambda e: e.dma_start(out=xi[:], in_=x_d[tok0 + t * 128: tok0 + (t + 1) * 128, :]), writes=[bxi])
            r = rstd_col("act", xi[:], ssA, D, EPS, [bxi], b_ssA, xni[:], bxni)
            op("act", lambda e: e.activation(out=xni[:], in_=xi[:], func=AF.Copy, scale=r), reads=[bxi, b_ssA], writes=[bxni])
            pT = bf(PB[6][:]).rearrange("p (k t) -> p k t", t=128)
            for kc in range(8):
                op("pe", lambda e: e.transpose(out=pT[:, kc, :], in_=xni[:, kc * 128:(kc + 1) * 128], identity=ident_b[:]),
                   reads=[bxni, b_idb], writes=[b_pb[6]])
            for kc in range(8):
                dst = hT[:, kc, t * 128:(t + 1) * 128]
                if kc % 2 == 0:
                    op("dve", lambda e: e.tensor_scalar(out=dst, in0=pT[:, kc, :], scalar1=MS[:, b, kc:kc + 1],
                                                        scalar2=MH[:, b, kc:kc + 1], op0=ALU.mult, op1=ALU.add),
                       reads=[b_pb[6], b_mod], writes=[b_hT])
                else:
                    op("act", lambda e: e.activation(out=dst, in_=pT[:, kc, :], func=AF.Identity,
                                                     bias=MH[:, b, kc:kc + 1], scale=MS[:, b, kc:kc + 1]),
                       reads=[b_pb[6], b_mod], writes=[b_hT])
        for ch in range(4):
            cs = slice(ch * 512, (ch + 1) * 512)
            for g in range(5):
                M = 128 if g < 3 else 96
                for kc in range(8):
                    op("pe", lambda e: e.matmul(PB[g][0:M, :], lhsT=wing_m[:, kc, g * 128:g * 128 + M], rhs=hT[:, kc, cs],
                                                start=(kc == 0), stop=(kc == 7)),
                       reads=[b_wm, b_hT], writes=[b_pb[g]])
            for i in range(2):
                op("act", lambda e: e.activation(out=tb[i][:], in_=PB[i][:], func=AF.Square), reads=[b_pb[i]], writes=[b_tb[i]])
            for i in range(2):
                op("pe", lambda e: e.matmul(PB[5][:], lhsT=ones_b[:], rhs=tb[i][:], start=(i == 0), stop=(i == 1)),
                   reads=[b_ones, b_tb[i]], writes=[b_pb[5]])
            rstd_bc(PB[5], b_pb[5], 256.0, EPS, tf[0], b_tf[0])
            for i in range(2):
                op("dve", lambda e: e.scalar_tensor_tensor(out=cqnT[:, i, cs], in0=PB[i][:], scalar=gqa[:, i:i + 1],
                                                           in1=tf[0][:], op0=ALU.mult, op1=ALU.mult),
                   reads=[b_pb[i], b_g, b_tf[0]], writes=[b_cqn])
            op("act", lambda e: e.activation(out=tb[0][:], in_=PB[2][:], func=AF.Square), reads=[b_pb[2]], writes=[b_tb[0]])
            op("pe", lambda e: e.matmul(PB[5][:], lhsT=ones_b[:], rhs=tb[0][:], start=True, stop=True),
               reads=[b_ones, b_tb[0]], writes=[b_pb[5]])
            rstd_bc(PB[5], b_pb[5], 128.0, EPS, tf[1], b_tf[1])
            op("dve", lambda e: e.scalar_tensor_tensor(out=ckvnT[:, cs], in0=PB[2][:], scalar=gkva[:, 0:1],
                                                       in1=tf[1][:], op0=ALU.mult, op1=ALU.mult),
               reads=[b_pb[2], b_g, b_tf[1]], writes=[b_ckvn])
            op("dve", lambda e: e.tensor_tensor(out=tf[2][64:96, :], in0=PB[3][64:96, :], in1=Cm[64:96, cs], op=ALU.mult),
               reads=[b_pb[3], b_tm], writes=[b_tf[2]])
            op("dve", lambda e: e.tensor_tensor(out=tf[0][64:96, :], in0=PB[4][64:96, :], in1=Sm[64:96, cs], op=ALU.mult),
               reads=[b_pb[4], b_tm], writes=[b_tf[0]])
            op("pool", lambda e: e.tensor_tensor(out=krT[64:96, cs], in0=tf[2][64:96, :], in1=tf[0][64:96, :], op=ALU.add),
               reads=[b_tf[2], b_tf[0]], writes=[b_kr])
        for t in range(16):
            pb = PB[6 + t % 2]; bpb = b_pb[6 + t % 2]
            op("pe", lambda e: e.matmul(pb[:], lhsT=ckvnT[:, t * 128:(t + 1) * 128], rhs=wkvv[:], start=True, stop=True),
               reads=[b_ckvn, b_w], writes=[bpb])
            op("act" if t % 2 else "dve",
               (lambda e: e.activation(out=vm[:, t, :, 0:64], in_=pb[:].rearrange("p (h d) -> p h d", d=64), func=AF.Copy))
               if t % 2 else
               (lambda e: e.tensor_copy(out=vm[:, t, :, 0:64], in_=pb[:].rearrange("p (h d) -> p h d", d=64))),
               reads=[bpb], writes=[b_vm])
        pi = 0
        for kind in range(2):
            dstT = qdT if kind == 0 else kdT; bdst = b_qd if kind == 0 else b_kd
            for h in range(4):
                wp = wing_p[pi % 2]; bwp = b_wp[pi % 2]; pi += 1
                gA = 5 + 8 * kind + h; gB = gA + 4
                dma("pool", lambda e: [e.dma_start(out=wp[:, :, 0:128], in_=wing_d[gA].rearrange("(k p) n -> p k n", p=128)),
                                       e.dma_start(out=wp[:, :, 128:256], in_=wing_d[gB].rearrange("(k p) n -> p k n", p=128))],
                    writes=[bwp])
                for ch in range(4):
                    cs = slice(ch * 512, (ch + 1) * 512)
                    pa = PB[2 * (ch % 2)]; bpa = b_pb[2 * (ch % 2)]; pbb = PB[2 * (ch % 2) + 1]; bpbb = b_pb[2 * (ch % 2) + 1]
                    for kc in range(8):
                        op("pe", lambda e: e.matmul(pa[:], lhsT=wp[:, kc, 0:128], rhs=hT[:, kc, cs], start=(kc == 0), stop=(kc == 7)),
                           reads=[bwp, b_hT], writes=[bpa])
                    for kc in range(8):
                        op("pe", lambda e: e.matmul(pbb[:], lhsT=wp[:, kc, 128:256], rhs=hT[:, kc, cs], start=(kc == 0), stop=(kc == 7)),
                           reads=[bwp, b_hT], writes=[bpbb])
                    op("dve", lambda e: e.tensor_tensor(out=tf[0][:], in0=pa[:], in1=Cd[:, cs], op=ALU.mult),
                       reads=[bpa, b_td], writes=[b_tf[0]])
                    op("dve", lambda e: e.tensor_tensor(out=tf[1][:], in0=pbb[:], in1=Sd[:, cs], op=ALU.mult),
                       reads=[bpbb, b_td], writes=[b_tf[1]])
                    op("pool", lambda e: e.tensor_tensor(out=dstT[:, h, cs], in0=tf[0][:], in1=tf[1][:], op=ALU.add),
                       reads=[b_tf[0], b_tf[1]], writes=[bdst])
        for t in range(16):
            pb = PB[6 + t % 2]; bpb = b_pb[6 + t % 2]
            for kc in range(8):
                op("pe", lambda e: e.matmul(pb[:], lhsT=hT[:, kc, t * 128:(t + 1) * 128], rhs=wvd[:, kc, :],
                                            start=(kc == 0), stop=(kc == 7)), reads=[b_hT, b_wvd], writes=[bpb])
            src = pb[:].rearrange("p (h j d) -> p h j d", h=4, j=2)
            if t % 2:
                op("act", lambda e: e.activation(out=vd[:, t, :, :, 0:64], in_=src, func=AF.Copy), reads=[bpb], writes=[b_vd])
            else:
                op("dve", lambda e: e.tensor_copy(out=vd[:, t, :, :, 0:64], in_=src), reads=[bpb], writes=[b_vd])
        A.close()

        Bs = Scope()
        wo = Bs.T("wo", [64, 16, D], BF16); b_wo = Bs.B("wo")
        dma("pool", lambda e: e.dma_start(out=wo[:], in_=wo_d.rearrange("(i p) n -> p i n", p=64)), writes=[b_wo])
        mixT = Bs.T("mixT", [64, 16, 512], BF16); b_mix = Bs.B("mix")
        kTh = [Bs.T("kTh%d" % i, [96, S], BF16) for i in range(2)]; b_kTh = [Bs.B("kTh0"), Bs.B("kTh1")]
        pt = [Bs.T("pt%d" % i, [128, 512], BF16) for i in range(3)]; b_pt = [Bs.B("pt%d" % i) for i in range(3)]
        qh = [Bs.T("qh%d" % i, [96, 512], BF16) for i in range(2)]; b_qh = [Bs.B("qh0"), Bs.B("qh1")]
        tg = [Bs.T("tg%d" % i, [128, 512], F32) for i in range(5)]; b_tg = [Bs.B("tg%d" % i) for i in range(5)]
        rd = Bs.T("rd", [128, 512], F32); b_rd = Bs.B("rd")
        sqb = [Bs.T("sqb%d" % i, [64, 512], BF16) for i in range(2)]; b_sqb = [Bs.B("sqb0"), Bs.B("sqb1")]
        xr = Bs.T("xr", [128, D], F32); b_xr = Bs.B("xr")
        ty = Bs.T("ty", [128, D], F32); b_ty = Bs.B("ty")
        xm = Bs.T("xm", [128, D], F32); b_xm = Bs.B("xm")
        ssB = Bs.T("ssB", [128, 8], F32); b_ssB = Bs.B("ssB")
        h2f = Bs.T("h2f", [128, 8, 128], F32); b_h2f = Bs.B("h2f")
        junkB = h2f[:].rearrange("p k t -> p (k t)"); b_junkB = b_h2f
        h2b = Bs.T("h2b", [128, 8, 128], BF16); b_h2b = Bs.B("h2b")
        rt = Bs.T("rt", [128, 6, NE], F32); b_rt = Bs.B("rt")
        gts = Bs.T("gts", [32, 128], F32); b_gts = Bs.B("gts")
        b_xmid = Bs.B("xmid_d"); b_h2d = Bs.B("h2T_d"); b_gtd = Bs.B("GT_d")
        pti = [0]; sbi = [0]
        deferred = []

        def run_deferred(n):
            for _ in range(n):
                if deferred:
                    deferred.pop(0)[0]()

        def flush_readers(wbanks):
            last = -1
            for i_, (_cl, banks) in enumerate(deferred):
                if banks & wbanks:
                    last = i_
            run_deferred(last + 1)


        def attn_map(kT_ap_fn, q_ap_fn, scale, pv_list, c_, kreads, qreads, vreads, wbanks=frozenset()):
            nkt = 4 * c_ + 4
            flush_readers(wbanks)

            def issue_S(kt):
                lo = max(0, kt * 128 - c_ * 512); n = 512 - lo
                sb_ = PB[sbi[0] % 2]; bsb = b_pb[sbi[0] % 2]; sbi[0] += 1
                op("pe", lambda e: e.matmul(sb_[:, 0:n], lhsT=kT_ap_fn(kt), rhs=q_ap_fn(lo, 512), start=True, stop=True),
                   reads=kreads + qreads, writes=[bsb])
                p_ = pt[pti[0] % 3]; bp_ = b_pt[pti[0] % 3]; pti[0] += 1
                op("act", lambda e: e.activation(out=p_[:, 0:n], in_=sb_[:, 0:n], func=AF.Exp, scale=scale),
                   reads=[bsb], writes=[bp_])
                if kt >= 4 * c_:
                    op("pool", lambda e: e.tensor_tensor(out=p_[:, 0:128], in0=p_[:, 0:128], in1=maskT[:], op=ALU.mult),
                       reads=[bp_, b_mask], writes=[bp_])
                return (p_, bp_, lo, n)

            pend = issue_S(0)
            for kt in range(nkt):
                nxt = issue_S(kt + 1) if kt + 1 < nkt else None
                p_, bp_, lo, n = pend
                for (acc, bacc, v_fn) in pv_list:
                    op("pe", lambda e: e.matmul(acc[:, lo:512], lhsT=v_fn(kt), rhs=p_[:, 0:n],
                                                start=(kt == 0), stop=(kt == nkt - 1)),
                       reads=[bp_] + vreads, writes=[bacc])
                pend = nxt
                run_deferred(2)

        def normalize(acc, bacc, dst, bdst, tmp, btmp, pbn=None, bpbn=None, bank=None):
            pbn = PB[7] if pbn is None else pbn; bpbn = b_pb[7] if bpbn is None else bpbn
            bk = frozenset([bank])
            deferred.append((lambda: op("dve", lambda e: e.reciprocal(out=rd[64:65, :], in_=acc[64:65, :]), reads=[bacc], writes=[b_rd]), bk))
            deferred.append((lambda: op("pe", lambda e: e.matmul(pbn[0:64, :], lhsT=ones_f[64:65, 0:64], rhs=rd[64:65, :], start=True, stop=True),
                                       reads=[b_rd, b_ones], writes=[bpbn]), bk))
            deferred.append((lambda: op("act", lambda e: e.activation(out=tmp[0:64, :], in_=pbn[0:64, :], func=AF.Copy), reads=[bpbn], writes=[btmp]), bk))
            deferred.append((lambda: op("dve", lambda e: e.tensor_tensor(out=dst, in0=acc[0:64, :], in1=tmp[0:64, :], op=ALU.mult),
                                       reads=[bacc, btmp], writes=[bdst]), bk))

        def diff_post(h):
            for hf in range(2):
                deferred.append((lambda hf=hf: op("dve", lambda e: e.scalar_tensor_tensor(out=tg[hf][0:64, :], in0=tg[2 + hf][0:64, :], scalar=lamc[0:64, 3:4],
                                                                                         in1=tg[hf][0:64, :], op0=ALU.mult, op1=ALU.add),
                                                 reads=[b_tg[2 + hf], b_tg[hf], b_lam], writes=[b_tg[hf]]), frozenset()))
                deferred.append((lambda hf=hf: op("act", lambda e: e.activation(out=sqb[hf][:], in_=tg[hf][0:64, :], func=AF.Square),
                                                 reads=[b_tg[hf]], writes=[b_sqb[hf]]), frozenset()))
            for hf in range(2):
                deferred.append((lambda hf=hf: op("pe", lambda e: e.matmul(PB[7][0:64, :], lhsT=ones_b[0:64, 0:64], rhs=sqb[hf][:], start=(hf == 0), stop=(hf == 1)),
                                                 reads=[b_ones, b_sqb[hf]], writes=[b_pb[7]]), frozenset()))
            deferred.append((lambda: op("dve", lambda e: e.tensor_scalar(out=tg[4][0:64, :], in0=PB[7][0:64, :], scalar1=1.0 / 128.0, scalar2=1e-5,
                                                                        op0=ALU.mult, op1=ALU.add), reads=[b_pb[7]], writes=[b_tg[4]]), frozenset()))
            deferred.append((lambda: op("dve", lambda e: e.reciprocal(out=tg[4][0:64, :], in_=tg[4][0:64, :]), reads=[b_tg[4]], writes=[b_tg[4]]), frozenset()))
            deferred.append((lambda: op("act", lambda e: e.activation(out=tg[4][0:64, :], in_=tg[4][0:64, :], func=AF.Sqrt), reads=[b_tg[4]], writes=[b_tg[4]]), frozenset()))
            for hf in range(2):
                deferred.append((lambda hf=hf: op("dve", lambda e: e.tensor_scalar(out=tg[2 + hf][0:64, :], in0=tg[hf][0:64, :], scalar1=gsub[:, hf:hf + 1],
                                                                                  scalar2=0.8, op0=ALU.mult, op1=ALU.mult),
                                                 reads=[b_tg[hf], b_g], writes=[b_tg[2 + hf]]), frozenset()))
                deferred.append((lambda hf=hf: op("dve", lambda e: e.tensor_tensor(out=mixT[:, 8 + 2 * h + hf, :], in0=tg[2 + hf][0:64, :], in1=tg[4][0:64, :],
                                                                                  op=ALU.mult), reads=[b_tg[2 + hf], b_tg[4]], writes=[b_mix]), frozenset()))

        for c_ in range(4):
            q0 = c_ * 512
            qs = slice(q0, q0 + 512)
            nk = (c_ + 1) * 512
            def mla_prep(h):
                kt_ = kTh[h % 2]; bkt = b_kTh[h % 2]
                for k2 in range(c_ + 1):
                    pk = PB[4 + k2 % 2]; bpk = b_pb[4 + k2 % 2]
                    op("pe", lambda e: e.matmul(pk[0:64, :], lhsT=wkvk[:, h, :], rhs=ckvnT[:, k2 * 512:(k2 + 1) * 512],
                                                start=True, stop=True), reads=[b_w, b_ckvn], writes=[bpk])
                    op("act", lambda e: e.activation(out=kt_[0:64, k2 * 512:(k2 + 1) * 512], in_=pk[0:64, :], func=AF.Copy),
                       reads=[bpk], writes=[bkt])
                op("pool", lambda e: e.tensor_copy(out=kt_[64:96, 0:nk], in_=krT[64:96, 0:nk]), reads=[b_kr], writes=[bkt])
                for (wq_, pbi) in ((wqA, 6), (wqB, 7)):
                    for kc in range(2):
                        op("pe", lambda e: e.matmul(PB[pbi][0:96, :], lhsT=wq_[:, kc, h, :], rhs=cqnT[:, kc, qs],
                                                    start=(kc == 0), stop=(kc == 1)), reads=[b_w, b_cqn], writes=[b_pb[pbi]])
                q_ = qh[h % 2]; bq_ = b_qh[h % 2]
                op("dve", lambda e: e.tensor_tensor(out=tg[0][0:96, :], in0=PB[6][0:96, :], in1=Cm[:, qs], op=ALU.mult),
                   reads=[b_pb[6], b_tm], writes=[b_tg[0]])
                op("dve", lambda e: e.tensor_tensor(out=tg[1][0:96, :], in0=PB[7][0:96, :], in1=Sm[:, qs], op=ALU.mult),
                   reads=[b_pb[7], b_tm], writes=[b_tg[1]])
                op("dve", lambda e: e.tensor_tensor(out=q_[:], in0=tg[0][0:96, :], in1=tg[1][0:96, :], op=ALU.add),
                   reads=[b_tg[0], b_tg[1]], writes=[bq_])

            mla_prep(0)
            for h in range(8):
                kt_ = kTh[h % 2]; bkt = b_kTh[h % 2]
                q_ = qh[h % 2]; bq_ = b_qh[h % 2]
                if h + 1 < 8:
                    mla_prep(h + 1)
                acc = PB[2 + h % 2]; bacc = b_pb[2 + h % 2]
                attn_map(lambda kt: kt_[:, kt * 128:(kt + 1) * 128], lambda lo, hi: q_[:, lo:hi], SC_M,
                         [(acc[0:65, :], bacc, lambda kt: vm[:, kt, h, :])], c_, [bkt], [bq_], [b_vm], wbanks=frozenset([2 + h % 2]))
                normalize(acc, bacc, mixT[:, h, :], b_mix, tg[2], b_tg[2], pbn=PB[4 + h % 2], bpbn=b_pb[4 + h % 2], bank=2 + h % 2)
            for h in range(4):
                for j in range(2):
                    js = slice(j * 64, (j + 1) * 64)
                    accs = [(PB[2 + 2 * j + hf][0:65, :], b_pb[2 + 2 * j + hf], (lambda kt, hf=hf: vd[:, kt, h, hf, :]))
                            for hf in range(2)]
                    attn_map(lambda kt: kdT[js, h, kt * 128:(kt + 1) * 128], lambda lo, hi: qdT[js, h, q0 + lo:q0 + hi], SC_D,
                             accs, c_, [b_kd], [b_qd], [b_vd], wbanks=frozenset([2 + 2 * j, 3 + 2 * j]))
                    for hf in range(2):
                        normalize(PB[2 + 2 * j + hf], b_pb[2 + 2 * j + hf], tg[2 * j + hf][0:64, :], b_tg[2 * j + hf],
                                  tg[4], b_tg[4], bank=2 + 2 * j + hf)
                diff_post(h)
            run_deferred(10000)
            for tt in range(4):
                r0 = tok0 + q0 + tt * 128
                dma("sp", lambda e: e.dma_start(out=xr[:], in_=x_d[r0:r0 + 128, :]), writes=[b_xr])
                for cg in range(2):
                    for i in range(16):
                        op("pe", lambda e: e.matmul(PB[4 + cg][:], lhsT=mixT[:, i, tt * 128:(tt + 1) * 128],
                                                    rhs=wo[:, i, cg * 512:(cg + 1) * 512], start=(i == 0), stop=(i == 15)),
                           reads=[b_mix, b_wo], writes=[b_pb[4 + cg]])
                for cg in range(2):
                    op("act", lambda e: e.activation(out=ty[:, cg * 512:(cg + 1) * 512], in_=PB[4 + cg][:], func=AF.Copy),
                       reads=[b_pb[4 + cg]], writes=[b_ty])
                r = rstd_col("act", ty[:], ssB, D, EPS, [b_ty], b_ssB, junkB, b_junkB)
                op("dve", lambda e: e.scalar_tensor_tensor(out=ty[:], in0=ty[:], scalar=r, in1=GM[:, b, :],
                                                           op0=ALU.mult, op1=ALU.mult), reads=[b_ty, b_ssB, b_mod], writes=[b_ty])
                op("dve", lambda e: e.tensor_tensor(out=xm[:], in0=ty[:], in1=xr[:], op=ALU.add),
                   reads=[b_ty, b_xr], writes=[b_xm])
                if stage == "xm":
                    dma("sp", lambda e: e.dma_start(out=out_d[r0:r0 + 128, :], in_=xm[:]), reads=[b_xm], writes=[b_xmid])
                else:
                    dma("sp", lambda e: e.dma_start(out=xmid_d[r0:r0 + 128, :], in_=xm[:]), reads=[b_xm], writes=[b_xmid])
                r2 = rstd_col("act", xm[:], ssB, D, EPS, [b_xm], b_ssB, junkB, b_junkB)
                op("act", lambda e: e.activation(out=ty[:], in_=xm[:], func=AF.Copy, scale=r2), reads=[b_xm, b_ssB], writes=[b_ty])
                for kc in range(8):
                    pbk = PB[6 + kc // 4]; bpbk = b_pb[6 + kc // 4]
                    op("pe", lambda e: e.transpose(out=pbk[:, (kc % 4) * 128:(kc % 4 + 1) * 128], in_=ty[:, kc * 128:(kc + 1) * 128],
                                                   identity=ident_f[:]), reads=[b_ty, b_id], writes=[bpbk])
                for kc in range(8):
                    pbk = PB[6 + kc // 4]; bpbk = b_pb[6 + kc // 4]
                    src = pbk[:, (kc % 4) * 128:(kc % 4 + 1) * 128]
                    if kc % 2:
                        op("dve", lambda e: e.tensor_scalar(out=h2f[:, kc, :], in0=src, scalar1=FS[:, b, kc:kc + 1],
                                                            scalar2=FH[:, b, kc:kc + 1], op0=ALU.mult, op1=ALU.add),
                           reads=[bpbk, b_mod], writes=[b_h2f])
                    else:
                        op("act", lambda e: e.activation(out=h2f[:, kc, :], in_=src, func=AF.Identity,
                                                         bias=FH[:, b, kc:kc + 1], scale=FS[:, b, kc:kc + 1]),
                           reads=[bpbk, b_mod], writes=[b_h2f])
                op("act", lambda e: e.activation(out=h2b[:], in_=h2f[:], func=AF.Copy), reads=[b_h2f], writes=[b_h2b])
                dma("sp", lambda e: e.dma_start(out=h2T_d[:, :, r0:r0 + 128].rearrange("k p t -> p k t"), in_=h2b[:]),
                    reads=[b_h2b], writes=[b_h2d])
                for kc in range(8):
                    op("pe", lambda e: e.matmul(PB[4][:, 0:NE], lhsT=h2f[:, kc, :], rhs=wr[:, kc, :], start=(kc == 0), stop=(kc == 7)),
                       reads=[b_h2f, b_wr], writes=[b_pb[4]])
                lg = rt[:, 0, :]; ex = rt[:, 1, :]; mk = rt[:, 2, :]; em = rt[:, 3, :]; gg = rt[:, 4, :]; m8 = rt[:, 5, 0:8]
                op("dve", lambda e: e.tensor_tensor(out=lg, in0=PB[4][:, 0:NE], in1=brr[:], op=ALU.add),
                   reads=[b_pb[4], b_wr], writes=[b_rt])
                op("dve", lambda e: e.max(out=m8, in_=lg), reads=[b_rt], writes=[b_rt])
                op("dve", lambda e: e.tensor_scalar(out=mk, in0=lg, scalar1=rt[:, 5, 3:4], scalar2=1.0, op0=ALU.is_ge, op1=ALU.mult),
                   reads=[b_rt], writes=[b_rt])
                op("dve", lambda e: e.tensor_scalar(out=rt[:, 5, 8:9], in0=rt[:, 5, 0:1], scalar1=-1.0, scalar2=0.0, op0=ALU.mult, op1=ALU.add),
                   reads=[b_rt], writes=[b_rt])
                op("act", lambda e: e.activation(out=ex, in_=lg, func=AF.Exp, bias=rt[:, 5, 8:9], scale=1.0),
                   reads=[b_rt], writes=[b_rt])
                op("dve", lambda e: e.memset(rt[:, 5, 9:10], 0.0), writes=[b_rt])
                op("dve", lambda e: e.scalar_tensor_tensor(out=em, in0=ex, scalar=1.0, in1=mk, op0=ALU.mult, op1=ALU.mult,
                                                           accum_out=rt[:, 5, 9:10]), reads=[b_rt], writes=[b_rt])
                op("dve", lambda e: e.reciprocal(out=rt[:, 5, 10:11], in_=rt[:, 5, 9:10]), reads=[b_rt], writes=[b_rt])
                op("dve", lambda e: e.tensor_scalar(out=gg, in0=em, scalar1=rt[:, 5, 10:11], scalar2=1.0, op0=ALU.mult, op1=ALU.mult),
                   reads=[b_rt], writes=[b_rt])
                op("pe", lambda e: e.transpose(out=PB[5][0:32, 0:128], in_=gg, identity=ident_f[:]),
                   reads=[b_rt, b_id], writes=[b_pb[5]])
                op("dve", lambda e: e.tensor_copy(out=gts[:], in_=PB[5][0:32, 0:128]), reads=[b_pb[5]], writes=[b_gts])
                dma("sp", lambda e: e.dma_start(out=GT_d[:, r0:r0 + 128], in_=gts[:]), reads=[b_gts], writes=[b_gtd])
        Bs.close()
        AB.close()
    P0.close()

    if stage == "xm":
        fence(c, [b_xmid])
        c.wait_all("sp", [b_xmid])
        print("instructions", c.n_ins, "waits", c.n_wait, "dsems", c.n_dsem)
        return nc

    TG = 1024
    M = Scope()
    yacc = M.T("yacc", [128, 8, TG], F32); b_yacc = M.B("yacc")
    h2g = M.T("h2g", [128, 8, TG], BF16); b_h2g = M.B("h2g")
    wgu = [M.T("wgu%d" % i, [128, 8, 2 * D], BF16) for i in range(2)]; b_wgu = [M.B("wgu0"), M.B("wgu1")]
    wdn = M.T("wdn", [128, 8, D], BF16); b_wdn = M.B("wdn")
    gtg = M.T("gtg", [32, TG], F32); b_gtg = M.B("gtg")
    bdn = M.T("bdn", [32, D], F32); bguc = M.T("bguc", [128, NE, 16], F32); b_mw = M.B("moew")
    gbc = [M.T("gbc%d" % i, [128, TG], F32) for i in range(2)]; b_gbc = [M.B("gbc0"), M.B("gbc1")]
    tA = [M.T("tA%d" % i, [128, 512], F32) for i in range(2)]; b_tA = [M.B("tA0"), M.B("tA1")]
    tS = [M.T("tS%d" % i, [128, 512], F32) for i in range(2)]; b_tS = [M.B("tS0"), M.B("tS1")]
    tU = [M.T("tU%d" % i, [128, 512], F32) for i in range(2)]; b_tU = [M.B("tU0"), M.B("tU1")]
    actT = [M.T("actT%d" % i, [128, 8, 512], BF16) for i in range(2)]; b_act = [M.B("act0"), M.B("act1")]
    GFt = M.T("GFt", [128, NB, D], F32); b_gft2 = M.B("GFt")
    tyF = M.T("tyF", [128, D], F32); b_tyF = M.B("tyF")
    xmt = M.T("xmt", [128, D], F32); b_xmt = M.B("xmt")
    ot = M.T("ot", [128, D], F32); b_ot = M.B("ot")
    ssF = M.T("ssF", [128, 8], F32); b_ssF = M.B("ssF")
    b_out = Buf("out")
    dma("sp", lambda e: [e.dma_start(out=bdn[:], in_=bdn_d), e.dma_start(out=bguc[:], in_=bguc_d)], writes=[b_mw])
    dma("sp", lambda e: e.dma_start(out=GFt[:], in_=gf_d), writes=[b_gft2])
    units = [(g_, e_) for g_ in range(NT // TG) for e_ in range(NE)]

    def load_wgu(u):
        ex_ = units[u][1]
        dma("pool", lambda e: e.dma_start(out=wgu[u % 2][:], in_=wgu_d[ex_].rearrange("(k p) n -> p k n", p=128)),
            writes=[b_wgu[u % 2]])

    def load_wdn(u):
        ex_ = units[u][1]
        dma("pool", lambda e: e.dma_start(out=wdn[:], in_=wdn_d[ex_].rearrange("(k p) n -> p k n", p=128)), writes=[b_wdn])

    load_wgu(0); load_wdn(0)
    for g in range(NT // TG):
        ts = slice(g * TG, (g + 1) * TG)
        dma("sp", lambda e: e.dma_start(out=h2g[:], in_=h2T_d[:, :, ts].rearrange("k p t -> p k t")), writes=[b_h2g])
        dma("sp", lambda e: e.dma_start(out=gtg[:], in_=GT_d[:, ts]), writes=[b_gtg])
        for dc in range(8):
            for tc in range(2):
                pbk = PB[4 + (2 * dc + tc) % 4]; bpbk = b_pb[4 + (2 * dc + tc) % 4]
                op("pe", lambda e: e.matmul(pbk[:], lhsT=bdn[:, dc * 128:(dc + 1) * 128], rhs=gtg[:, tc * 512:(tc + 1) * 512],
                                            start=True, stop=True), reads=[b_mw, b_gtg], writes=[bpbk])
                op("act", lambda e: e.activation(out=yacc[:, dc, tc * 512:(tc + 1) * 512], in_=pbk[:], func=AF.Copy),
                   reads=[bpbk], writes=[b_yacc])
        for ex in range(NE):
            u = g * NE + ex
            wi = u % 2
            w_ = wgu[wi]; bw_ = b_wgu[wi]
            if u + 1 < len(units):
                load_wgu(u + 1)
            gb_ = gbc[u % 2]; bgb_ = b_gbc[u % 2]
            dma("sp", lambda e: e.dma_start(out=gb_[:], in_=GT_d[ex:ex + 1, ts].partition_broadcast(128)), writes=[bgb_])
            for tc in range(2):
                tcs = slice(tc * 512, (tc + 1) * 512)
                at = actT[tc]; bat = b_act[tc]
                for fc in range(8):
                    pa = PB[2 * (fc % 2)]; bpa = b_pb[2 * (fc % 2)]; pu = PB[2 * (fc % 2) + 1]; bpu = b_pb[2 * (fc % 2) + 1]
                    for kc in range(8):
                        op("pe", lambda e: e.matmul(pa[:], lhsT=w_[:, kc, fc * 128:(fc + 1) * 128], rhs=h2g[:, kc, tcs],
                                                    start=(kc == 0), stop=(kc == 7)), reads=[bw_, b_h2g], writes=[bpa])
                    for kc in range(8):
                        op("pe", lambda e: e.matmul(pu[:], lhsT=w_[:, kc, D + fc * 128:D + (fc + 1) * 128], rhs=h2g[:, kc, tcs],
                                                    start=(kc == 0), stop=(kc == 7)), reads=[bw_, b_h2g], writes=[bpu])
                    a_ = tA[fc % 2]; ba_ = b_tA[fc % 2]; s_ = tS[fc % 2]; bs_ = b_tS[fc % 2]; u_ = tU[fc % 2]; bu_ = b_tU[fc % 2]
                    op("dve", lambda e: e.tensor_scalar(out=a_[:], in0=pa[:], scalar1=bguc[:, ex, fc:fc + 1], scalar2=7.0,
                                                        op0=ALU.add, op1=ALU.min), reads=[bpa, b_mw], writes=[ba_])
                    op("act", lambda e: e.activation(out=s_[:], in_=a_[:], func=AF.Sigmoid, scale=1.702), reads=[ba_], writes=[bs_])
                    op("act", lambda e: e.activation(out=u_[:], in_=pu[:], func=AF.Identity, bias=bguc[:, ex, 8 + fc:9 + fc], scale=1.0),
                       reads=[bpu, b_mw], writes=[bu_])
                    op("dve", lambda e: e.tensor_scalar(out=u_[:], in0=u_[:], scalar1=-7.0, scalar2=7.0,
                                                        op0=ALU.max, op1=ALU.min), reads=[bu_], writes=[bu_])
                    op("dve", lambda e: e.tensor_tensor(out=a_[:], in0=a_[:], in1=s_[:], op=ALU.mult), reads=[ba_, bs_], writes=[ba_])
                    op("dve", lambda e: e.scalar_tensor_tensor(out=a_[:], in0=u_[:], scalar=1.0, in1=a_[:], op0=ALU.add, op1=ALU.mult),
                       reads=[ba_, bu_], writes=[ba_])
                    op("dve", lambda e: e.tensor_tensor(out=at[:, fc, :], in0=a_[:], in1=gb_[:, tcs], op=ALU.mult),
                       reads=[ba_, bgb_], writes=[bat])
            for tc in range(2):
                tcs = slice(tc * 512, (tc + 1) * 512)
                at = actT[tc]; bat = b_act[tc]
                for dc in range(8):
                    py = PB[4 + dc % 4]; bpy = b_pb[4 + dc % 4]
                    for fc in range(8):
                        op("pe", lambda e: e.matmul(py[:], lhsT=wdn[:, fc, dc * 128:(dc + 1) * 128], rhs=at[:, fc, :],
                                                    start=(fc == 0), stop=(fc == 7)), reads=[b_wdn, bat], writes=[bpy])
                    op("dve", lambda e: e.tensor_tensor(out=yacc[:, dc, tcs], in0=py[:], in1=yacc[:, dc, tcs], op=ALU.add),
                       reads=[bpy, b_yacc], writes=[b_yacc])
            if u + 1 < len(units):
                load_wdn(u + 1)
        for tt in range(TG // 128):
            r0 = g * TG + tt * 128
            bb = r0 // S
            dma("sp", lambda e: e.dma_start(out=xmt[:], in_=xmid_d[r0:r0 + 128, :]), writes=[b_xmt])
            for dc in range(8):
                pbk = PB[dc // 4]; bpbk = b_pb[dc // 4]
                op("pe", lambda e: e.transpose(out=pbk[:, (dc % 4) * 128:(dc % 4 + 1) * 128], in_=yacc[:, dc, tt * 128:(tt + 1) * 128],
                                               identity=ident_f[:]), reads=[b_yacc, b_id], writes=[bpbk])
            for cg in range(2):
                op("act", lambda e: e.activation(out=tyF[:, cg * 512:(cg + 1) * 512], in_=PB[cg][:], func=AF.Copy),
                   reads=[b_pb[cg]], writes=[b_tyF])
            r = rstd_col("act", tyF[:], ssF, D, EPS, [b_tyF], b_ssF, ot[:], b_ot)
            op("dve", lambda e: e.scalar_tensor_tensor(out=tyF[:], in0=tyF[:], scalar=r, in1=GFt[:, bb, :],
                                                       op0=ALU.mult, op1=ALU.mult), reads=[b_tyF, b_ssF, b_gft2], writes=[b_tyF])
            op("dve", lambda e: e.tensor_tensor(out=ot[:], in0=tyF[:], in1=xmt[:], op=ALU.add),
               reads=[b_tyF, b_xmt], writes=[b_ot])
            dma("sp", lambda e: e.dma_start(out=out_d[r0:r0 + 128, :], in_=ot[:]), reads=[b_ot], writes=[b_out])
    fence(c, [b_out])
    M.close()
    c.wait_all("sp", [b_out])
    print("instructions", c.n_ins, "waits", c.n_wait, "dsems", c.n_dsem)
    return nc


def fence(c, bufs):
    deps = []
    for b in bufs:
        deps.append(b.last_w)
        deps.extend(b.readers)
    for e in c.ENGS:
        c._need(e, deps)


_NC_CACHE = {}


def kernel(**inputs):
    stage = inputs.pop("_stage", "full")
    inp = {k: np.asarray(v) for k, v in inputs.items()}
    maps = host_prep(inp)
    if stage not in _NC_CACHE:
        _NC_CACHE[stage] = build(stage)
    nc = _NC_CACHE[stage]
    ncr = NCORES if stage == "full" else 1
    maps = [{k: m[k] for k in nc._in_names} for m in maps[:ncr]]
    import os
    if os.environ.get("KTRACE"):
        res = run_bass_kernel_spmd(nc, maps, core_ids=list(range(ncr)), trace=True)
        print("KTRACE exec_time_ns", res.exec_time_ns)
    else:
        res = run_bass_kernel_spmd(nc, maps, core_ids=list(range(ncr)))
    out = np.concatenate([r["out"] for r in res.results], axis=0)
    if ncr < NCORES:
        out = np.concatenate([out, np.zeros(((NCORES - ncr) * NT, D), np.float32)], 0)
    return out.reshape(16, S, D).astype(np.float32)
```
